# Optimizing a Trainium2 kernel written in Bass

```python
import math
import jax, jax.numpy as jnp
from jax import lax
import numpy as np

D_MODEL = 1024
BATCH = 2
SEQ = 8192
DEPTH = 1

DN_HEADS = 8
DN_DK = 128
DN_DV = 128
DN_CONV = 4
DN_CHUNK = 64
MLA_HEADS = 8
MLA_DH = 128
MLA_DV = 128
Q_LORA = 256
KV_LORA = 256
IDX_HEADS = 8
IDX_DIM = 64
IDX_TOPK_MAX = 256
Q_BLOCK = 128
PEER_KEYS = 128
PEER_EXPERTS = PEER_KEYS * PEER_KEYS
PEER_HEADS = 8
PEER_DQ = 256
PEER_TOPK = 16
PEER_BLOCK = 128
EPS = 1e-6
IN_SPLITS = (DN_HEADS * DN_DK, DN_HEADS * DN_DK, DN_HEADS * DN_DV, DN_HEADS * DN_DV, DN_HEADS, DN_HEADS, Q_LORA, KV_LORA, IDX_DIM, IDX_HEADS, D_MODEL, D_MODEL)
IN_COLS = sum(IN_SPLITS)

kernel_name = 'hybrid_deltanet_dsa_peer_block'


def rmsnorm(x, w):
    xf = x.astype(jnp.float32)
    y = xf * lax.rsqrt(jnp.mean(xf * xf, axis=-1, keepdims=True) + EPS)
    return (y * w.astype(jnp.float32)).astype(x.dtype)


def l2norm(x):
    xf = x.astype(jnp.float32)
    return xf * lax.rsqrt(jnp.sum(xf * xf, axis=-1, keepdims=True) + EPS)


def causal_conv(x, w):
    K = w.shape[0]
    L = x.shape[1]
    xp = jnp.pad(x, ((0, 0), (K - 1, 0), (0, 0)))
    return sum(xp[:, i:i + L] * w[i] for i in range(K))


def to_blocks(a, blk):
    B, L = a.shape[:2]
    a = a.reshape((B, L // blk, blk) + a.shape[2:])
    return jnp.moveaxis(a, 1, 0)


def from_blocks(a):
    a = jnp.moveaxis(a, 0, 1)
    return a.reshape((a.shape[0], a.shape[1] * a.shape[2]) + a.shape[3:])


def gated_delta_rule(q, k, v, g, beta):
    B, L, H, dk = q.shape
    dv = v.shape[-1]
    C = DN_CHUNK
    n = L // C
    f32 = jnp.float32

    def chunk(a):
        a = jnp.moveaxis(a.astype(f32), 2, 1)
        return a.reshape((B, H, n, C) + a.shape[3:])

    q, k, v, g, beta = map(chunk, (q, k, v, g, beta))
    q = q * (dk ** -0.5)
    g = jnp.cumsum(g, axis=-1)
    tril = jnp.tril(jnp.ones((C, C), bool))
    strict = jnp.tril(jnp.ones((C, C), bool), -1)
    decay = jnp.exp(jnp.where(tril, g[..., :, None] - g[..., None, :], -jnp.inf))
    kb = k * beta[..., None]
    A = jnp.where(strict, jnp.einsum('bhncd,bhnsd->bhncs', kb, k) * decay, 0.0)
    eye = jnp.eye(C, dtype=f32)
    rhs = jnp.concatenate([v * beta[..., None], kb * jnp.exp(g)[..., None]], axis=-1)
    sol = lax.linalg.triangular_solve(A + eye, rhs, left_side=True, lower=True)
    u, w = sol[..., :dv], sol[..., dv:]
    attn = jnp.where(tril, jnp.einsum('bhncd,bhnsd->bhncs', q, k) * decay, 0.0)
    q_dec = q * jnp.exp(g)[..., None]
    k_dec = k * jnp.exp(g[..., -1:] - g)[..., None]
    g_last = jnp.exp(g[..., -1])

    def step(S, xs):
        qd, kd, u_c, w_c, a_c, gl = xs
        v_new = u_c - jnp.einsum('bhcd,bhde->bhce', w_c, S)
        o = jnp.einsum('bhcd,bhde->bhce', qd, S) + jnp.einsum('bhcs,bhse->bhce', a_c, v_new)
        S = S * gl[..., None, None] + jnp.einsum('bhcd,bhce->bhde', kd, v_new)
        return S, o

    xs = tuple(jnp.moveaxis(a, 2, 0) for a in (q_dec, k_dec, u, w, attn, g_last))
    S0 = jnp.zeros((B, H, dk, dv), f32)
    _, o = lax.scan(step, S0, xs)
    o = jnp.moveaxis(o, 0, 2).reshape(B, H, L, dv)
    return jnp.moveaxis(o, 1, 2)


def dsa_branch(q_lat, c_kv, k_idx, w_idx, q_norm_w, kv_norm_w, idx_k_norm_w, w_uq, w_iq, w_uk, w_uv):
    B, L, _ = q_lat.shape
    f32 = jnp.float32
    topk = min(IDX_TOPK_MAX, L // 4)
    q_lat = rmsnorm(q_lat, q_norm_w)
    c_kv = rmsnorm(c_kv, kv_norm_w)
    k_idx = rmsnorm(k_idx, idx_k_norm_w).astype(f32)
    q = (q_lat @ w_uq).reshape(B, L, MLA_HEADS, MLA_DH)
    q_idx = (q_lat @ w_iq).reshape(B, L, IDX_HEADS, IDX_DIM).astype(f32)
    w_idx = w_idx.astype(f32) * (IDX_HEADS ** -0.5 * IDX_DIM ** -0.5)
    q_abs = jnp.einsum('blhd,chd->blhc', q, w_uk) * (MLA_DH ** -0.5)
    key_pos = jnp.arange(L)

    def block(xs):
        qa, qi, wi, t0 = xs
        tq = t0 + jnp.arange(Q_BLOCK)
        s = jax.nn.relu(jnp.einsum('bqhd,bkd->bqhk', qi, k_idx))
        score = jnp.einsum('bqhk,bqh->bqk', s, wi)
        score = jnp.where(key_pos[None, None, :] <= tq[None, :, None], score, -jnp.inf)
        _, idx = lax.top_k(score, topk)
        valid = idx <= tq[None, :, None]
        kv = jax.vmap(lambda ckv, i: ckv[i])(c_kv, idx)
        logits = jnp.einsum('bqhc,bqkc->bqhk', qa, kv).astype(f32)
        logits = jnp.where(valid[:, :, None, :], logits, -jnp.inf)
        p = jax.nn.softmax(logits, axis=-1).astype(kv.dtype)
        return jnp.einsum('bqhk,bqkc->bqhc', p, kv)

    nb = L // Q_BLOCK
    xs = (to_blocks(q_abs, Q_BLOCK), to_blocks(q_idx, Q_BLOCK), to_blocks(w_idx, Q_BLOCK),
          jnp.arange(nb, dtype=jnp.int32) * Q_BLOCK)
    o_lat = from_blocks(lax.map(block, xs))
    o = jnp.einsum('blhc,chd->blhd', o_lat, w_uv)
    return o.reshape(B, L, MLA_HEADS * MLA_DV)


def mixer(h, w_in, dn_conv_w, dn_a_log, dn_dt_bias, dn_onorm_w, q_norm_w, kv_norm_w, idx_k_norm_w,
          w_uq, w_iq, w_uk, w_uv, w_a_out, w_b_out, w_o):
    B, L, _ = h.shape
    f32 = jnp.float32
    offs = np.cumsum(IN_SPLITS)[:-1].tolist()
    q, k, v, z, b, a, q_lat, c_kv, k_idx, w_idx, g_a, g_b = jnp.split(h @ w_in, offs, axis=-1)
    qkv = jax.nn.silu(causal_conv(jnp.concatenate([q, k, v], axis=-1), dn_conv_w))
    q, k, v = jnp.split(qkv, [DN_HEADS * DN_DK, 2 * DN_HEADS * DN_DK], axis=-1)
    q = l2norm(q.reshape(B, L, DN_HEADS, DN_DK))
    k = l2norm(k.reshape(B, L, DN_HEADS, DN_DK))
    v = v.reshape(B, L, DN_HEADS, DN_DV)
    beta = jax.nn.sigmoid(b.astype(f32))
    g = -jnp.exp(dn_a_log.astype(f32)) * jax.nn.softplus(a.astype(f32) + dn_dt_bias.astype(f32))
    o = gated_delta_rule(q, k, v, g, beta).astype(h.dtype)
    o = rmsnorm(o, dn_onorm_w) * jax.nn.silu(z.reshape(B, L, DN_HEADS, DN_DV))
    y_a = o.reshape(B, L, DN_HEADS * DN_DV) @ w_a_out
    y_b = dsa_branch(q_lat, c_kv, k_idx, w_idx, q_norm_w, kv_norm_w, idx_k_norm_w, w_uq, w_iq, w_uk, w_uv) @ w_b_out
    m = jax.nn.sigmoid(g_a) * y_a + jax.nn.sigmoid(g_b) * y_b
    return m @ w_o


def peer(h, w_q, sub_keys, u_emb, v_emb):
    B, L, _ = h.shape
    f32 = jnp.float32
    q = (h @ w_q).reshape(B, L, PEER_HEADS, 2, PEER_DQ // 2)
    s = jnp.einsum('blhpd,hpnd->blhpn', q, sub_keys).astype(f32)
    sv, si = lax.top_k(s, PEER_TOPK)
    cand = sv[..., 0, :, None] + sv[..., 1, None, :]
    cand_idx = si[..., 0, :, None] * PEER_KEYS + si[..., 1, None, :]
    top_v, top_j = lax.top_k(cand.reshape(B, L, PEER_HEADS, PEER_TOPK * PEER_TOPK), PEER_TOPK)
    experts = jnp.take_along_axis(cand_idx.reshape(B, L, PEER_HEADS, PEER_TOPK * PEER_TOPK), top_j, axis=-1)
    gates = jax.nn.softmax(top_v, axis=-1).astype(h.dtype)
    E = PEER_HEADS * PEER_TOPK
    experts = experts.reshape(B, L, E)
    gates = gates.reshape(B, L, E)

    def block(xs):
        hb, eb, gb = xs
        act = jax.nn.gelu(jnp.einsum('bted,btd->bte', u_emb[eb], hb), approximate=False) * gb
        return jnp.einsum('bte,bted->btd', act, v_emb[eb])

    xs = (to_blocks(h, PEER_BLOCK), to_blocks(experts, PEER_BLOCK), to_blocks(gates, PEER_BLOCK))
    return from_blocks(lax.map(block, xs))


def setup_inputs(seed: int = 0) -> dict:
    key = jax.random.key(seed)
    ks = iter(jax.random.split(key, 40))

    def nrm(shape, scale):
        return jax.random.normal(next(ks), shape, jnp.float32) * scale

    def gain(shape):
        return 1.0 + nrm(shape, 0.02)

    Dp = DEPTH
    D = D_MODEL
    x = nrm((BATCH, SEQ, D), 1.0)
    c = nrm((BATCH, D), 1.0)
    w_ada = nrm((Dp, D, 6 * D), D ** -0.5)
    b_ada = nrm((Dp, 6 * D), 0.02)
    norm1_w = gain((Dp, D))
    w_in = nrm((Dp, D, IN_COLS), D ** -0.5)
    dn_conv_w = nrm((Dp, DN_CONV, DN_HEADS * (2 * DN_DK + DN_DV)), DN_CONV ** -0.5)
    dn_a_log = jnp.log(jax.random.uniform(next(ks), (Dp, DN_HEADS), jnp.float32, 1.0, 16.0))
    dt = jnp.exp(jax.random.uniform(next(ks), (Dp, DN_HEADS), jnp.float32, math.log(1e-3), math.log(1e-1)))
    dn_dt_bias = dt + jnp.log(-jnp.expm1(-dt))
    dn_onorm_w = gain((Dp, DN_DV))
    q_norm_w = gain((Dp, Q_LORA))
    kv_norm_w = gain((Dp, KV_LORA))
    idx_k_norm_w = gain((Dp, IDX_DIM))
    w_uq = nrm((Dp, Q_LORA, MLA_HEADS * MLA_DH), Q_LORA ** -0.5)
    w_iq = nrm((Dp, Q_LORA, IDX_HEADS * IDX_DIM), Q_LORA ** -0.5)
    w_uk = nrm((Dp, KV_LORA, MLA_HEADS, MLA_DH), KV_LORA ** -0.5)
    w_uv = nrm((Dp, KV_LORA, MLA_HEADS, MLA_DV), KV_LORA ** -0.5)
    w_a_out = nrm((Dp, DN_HEADS * DN_DV, D), (DN_HEADS * DN_DV) ** -0.5)
    w_b_out = nrm((Dp, MLA_HEADS * MLA_DV, D), (MLA_HEADS * MLA_DV) ** -0.5)
    w_o = nrm((Dp, D, D), D ** -0.5)
    norm2_w = gain((Dp, D))
    peer_w_q = nrm((Dp, D, PEER_HEADS * PEER_DQ), D ** -0.5)
    peer_sub_keys = nrm((Dp, PEER_HEADS, 2, PEER_KEYS, PEER_DQ // 2), (PEER_DQ // 2) ** -0.5)
    peer_u = nrm((Dp, PEER_EXPERTS, D), D ** -0.5)
    peer_v = nrm((Dp, PEER_EXPERTS, D), 0.2)
    final_norm_w = gain((D,))
    return {'x': x, 'c': c, 'w_ada': w_ada, 'b_ada': b_ada, 'norm1_w': norm1_w, 'w_in': w_in,
            'dn_conv_w': dn_conv_w, 'dn_a_log': dn_a_log, 'dn_dt_bias': dn_dt_bias, 'dn_onorm_w': dn_onorm_w,
            'q_norm_w': q_norm_w, 'kv_norm_w': kv_norm_w, 'idx_k_norm_w': idx_k_norm_w,
            'w_uq': w_uq, 'w_iq': w_iq, 'w_uk': w_uk, 'w_uv': w_uv,
            'w_a_out': w_a_out, 'w_b_out': w_b_out, 'w_o': w_o, 'norm2_w': norm2_w,
            'peer_w_q': peer_w_q, 'peer_sub_keys': peer_sub_keys, 'peer_u': peer_u, 'peer_v': peer_v,
            'final_norm_w': final_norm_w}


def reference(x, c, w_ada, b_ada, norm1_w, w_in, dn_conv_w, dn_a_log, dn_dt_bias, dn_onorm_w,
              q_norm_w, kv_norm_w, idx_k_norm_w, w_uq, w_iq, w_uk, w_uv, w_a_out, w_b_out, w_o,
              norm2_w, peer_w_q, peer_sub_keys, peer_u, peer_v, final_norm_w):
    B = x.shape[0]
    for l in range(DEPTH):
        mod = (jax.nn.silu(c) @ w_ada[l] + b_ada[l]).reshape(B, 6, D_MODEL)
        sh1, sc1, gt1, sh2, sc2, gt2 = [mod[:, i, None, :] for i in range(6)]
        h = rmsnorm(x, norm1_w[l]) * (1.0 + sc1) + sh1
        x = x + gt1 * mixer(h, w_in[l], dn_conv_w[l], dn_a_log[l], dn_dt_bias[l], dn_onorm_w[l],
                            q_norm_w[l], kv_norm_w[l], idx_k_norm_w[l], w_uq[l], w_iq[l], w_uk[l], w_uv[l],
                            w_a_out[l], w_b_out[l], w_o[l])
        h = rmsnorm(x, norm2_w[l]) * (1.0 + sc2) + sh2
        x = x + gt2 * peer(h, peer_w_q[l], peer_sub_keys[l], peer_u[l], peer_v[l])
    return rmsnorm(x, final_norm_w)
```

```python
from contextlib import ExitStack
import numpy as np
import concourse.bass as bass
import concourse.mybir as mybir
from concourse.bass_utils import run_bass_kernel_spmd

F32 = mybir.dt.float32
BF16 = mybir.dt.bfloat16
I32 = mybir.dt.int32
U32 = mybir.dt.uint32
AF = mybir.ActivationFunctionType
ALU = mybir.AluOpType
AX = mybir.AxisListType
ENGS = ("pe", "act", "dve", "pool", "sp")
EPS = 1e-6
NEG = -30000.0

L = 8192
D = 1024
NCH = 64
NOWN = 16
DEBUG = {}


class Buf:
    __slots__ = ("name", "lw", "rd", "dsem", "dcnt")

    def __init__(self, name):
        self.name = name
        self.lw = None
        self.rd = {}
        self.dsem = None
        self.dcnt = 0


def rr(*gens):
    gens = list(gens)
    while gens:
        for g in list(gens):
            try:
                next(g)
            except StopIteration:
                gens.remove(g)


def bufs(name, n):
    return [Buf("%s%d" % (name, i)) for i in range(n)]


class Prog:
    def __init__(self, nc, stack):
        self.nc = nc
        self.gstack = stack
        self.stack = stack
        self.q = {e: [] for e in ENGS}
        self.cnt = {e: 0 for e in ENGS}
        self.sem = {e: stack.enter_context(nc.semaphore("s_" + e)) for e in ENGS}
        self.seen = {e: {} for e in ENGS}
        self.dtoks = {}
        self.out_tokens = []
        self.nid = 0
        self.rec = None

    def sb(self, name, shape, dt):
        return self.stack.enter_context(self.nc.sbuf_tensor(name, list(shape), dt))

    def ps(self, name, shape, dt):
        return self.stack.enter_context(self.nc.psum_tensor(name, list(shape), dt))

    def newsem(self, name):
        self.nid += 1
        return self.gstack.enter_context(self.nc.semaphore("%s_%d" % (name, self.nid)))

    def _need(self, eng, tok, waits):
        if tok is None:
            return
        sem, val, weng = tok
        if weng == "pe" and eng == "pe":
            return
        key = id(sem)
        if self.seen[eng].get(key, 0) >= val:
            return
        if waits.get(key, (None, 0))[1] < val:
            waits[key] = (sem, val)

    def _deps(self, eng, reads, writes):
        waits = {}
        for b in reads:
            self._need(eng, b.lw, waits)
        for b in writes:
            self._need(eng, b.lw, waits)
            for r in b.rd.values():
                self._need(eng, r, waits)
        for key, (sem, val) in waits.items():
            self.seen[eng][key] = val
        return list(waits.values())

    def _commit(self, tok, reads, writes):
        k = id(tok[0])
        for b in reads:
            o = b.rd.get(k)
            if o is None or o[1] < tok[1]:
                b.rd[k] = tok
        for b in writes:
            b.lw = tok
            b.rd = {}

    def record(self, f):
        self.rec = []
        f()
        r, self.rec = self.rec, None
        return r

    def replay(self, items):
        for kind, a, kw in items:
            (self.op if kind == "op" else self.dma)(*a, **kw)

    def op(self, eng, fn, reads=(), writes=()):
        if self.rec is not None:
            self.rec.append(("op", (eng, fn), dict(reads=list(reads), writes=list(writes))))
            return
        waits = self._deps(eng, reads, writes)
        self.cnt[eng] += 1
        sem = self.sem[eng]
        self.q[eng].append((waits, fn, sem, 1))
        self._commit((sem, self.cnt[eng], eng), reads, writes)

    def dma(self, eng, fn, reads=(), writes=(), track=None, is_out=False):
        if self.rec is not None:
            self.rec.append(("dma", (eng, fn), dict(reads=list(reads), writes=list(writes), track=track, is_out=is_out)))
            return
        waits = self._deps(eng, reads, writes)
        tb = track if track is not None else (writes[0] if writes else reads[0])
        if tb.dsem is None:
            tb.dsem = self.newsem("d")
        tb.dcnt += 1
        val = 16 * tb.dcnt
        self.q[eng].append((waits, fn, tb.dsem, 16))
        tok = (tb.dsem, val, "dma")
        self.dtoks[id(tb.dsem)] = (tb.dsem, val)
        self._commit(tok, reads, writes)
        if is_out:
            self.out_tokens.append(tok)

    def flush(self, final=False):
        q = self.q
        self.q = {e: [] for e in ENGS}
        bar = {}
        for e in ENGS:
            w = []
            for f in ENGS:
                if f != e and self.cnt[f] > 0 and self.seen[e].get(id(self.sem[f]), 0) < self.cnt[f]:
                    w.append((self.sem[f], self.cnt[f]))
                    self.seen[e][id(self.sem[f])] = self.cnt[f]
            for k, (s, v) in self.dtoks.items():
                if self.seen[e].get(k, 0) < v:
                    w.append((s, v))
                    self.seen[e][k] = v
            bar[e] = w

        def run(e, name):
            for waits, fn, sem, inc in q[name]:
                for (ws, wv) in waits:
                    e.wait_ge(ws, wv)
                fn(e).then_inc(sem, inc)
            for (ws, wv) in bar[name]:
                e.wait_ge(ws, wv)

        with self.nc.Block() as block:
            @block.sync
            def _(e):
                run(e, "sp")

            @block.scalar
            def _(e):
                run(e, "act")

            @block.vector
            def _(e):
                run(e, "dve")

            @block.gpsimd
            def _(e):
                run(e, "pool")

            @block.tensor
            def _(e):
                run(e, "pe")


C_Q, C_K, C_V, C_Z, C_B, C_A, C_QL, C_KV, C_KI, C_WI, C_GA, C_GB = (
    0, 1024, 2048, 3072, 4096, 4104, 4112, 4368, 4624, 4688, 4696, 5720)


def build(phases=("p2", "p3a", "p3b")):
    nc = bass.Bass("TRN2", target_bir_lowering=False)
    dr = {}

    def din(name, shape, dt=F32):
        dr[name] = nc.dram_tensor(name, list(shape), dt, kind="ExternalInput").ap()

    din("xf", [L, D]); din("xo", [NOWN * 128, D]); din("cT", [128, 8])
    din("sel", [128, 4]); din("cmask", [128, 512])
    din("w_ada", [D, 6 * D]); din("b_ada", [6 * D]); din("norm1_w", [D]); din("w_in", [D, 6744])
    din("dn_conv_w", [128, 24, 4]); din("dn_a_log", [8]); din("dn_dt_bias", [8]); din("dn_onorm_w", [128])
    din("q_norm_w", [256]); din("kv_norm_w", [256]); din("idx_k_norm_w", [64])
    din("w_uq", [256, 1024]); din("w_iq", [256, 512]); din("w_uk", [256, 1024]); din("w_uv", [256, 1024])
    din("w_a_out", [D, D]); din("w_b_out", [D, D]); din("w_o", [D, D]); din("norm2_w", [D])
    din("peer_w_q", [D, 2048]); din("peer_sub_keys", [16, 128, 128]); din("peer_u", [16384, D]); din("peer_v", [16384, D])
    din("final_norm_w", [D])
    out_d = nc.dram_tensor("out", [NOWN * 128, D], F32, kind="ExternalOutput").ap()
    ckv_tm_d = nc.dram_tensor("ckv_tm_d", [L, 256], BF16).ap()
    ckvT_d = nc.dram_tensor("ckvT_d", [256, L], BF16).ap()
    kidxT_d = nc.dram_tensor("kidxT_d", [64, L], BF16).ap()
    uv_d = nc.dram_tensor("uv_d", [16384, 2 * D], BF16).ap()
    wb_d = nc.dram_tensor("wb_d", [16, 128, 8, 512], BF16).ap()
    oown_d = nc.dram_tensor("oown_d", [NOWN * 128, D], F32,
                            kind=("ExternalOutput" if "dbg_o" in DEBUG else "Internal")).ap()
    yb_d = nc.dram_tensor("yb_d", [NOWN * 128, D], F32,
                          kind=("ExternalOutput" if "dbg_yb" in DEBUG else "Internal")).ap()

    with ExitStack() as gst:
        P = Prog(nc, gst)
        ident = P.sb("ident", [128, 128], BF16); b_ident = Buf("ident")
        identf = P.sb("identf", [128, 128], F32); b_identf = Buf("identf")
        onesf = P.sb("onesf", [128, 128], F32); b_onesf = Buf("onesf")
        trile = P.sb("trile", [128, 128], F32); b_trile = Buf("trile")
        slm = P.sb("slm", [128, 128], F32); b_slm = Buf("slm")
        negs = P.sb("negs", [128, 128], F32); b_negs = Buf("negs")
        neg2 = P.sb("neg2", [128, 128], F32); b_neg2 = Buf("neg2")
        bdm = P.sb("bdm", [128, 128], F32); b_bdm = Buf("bdm")
        offm = P.sb("offm", [128, 128], F32); b_offm = Buf("offm")
        offtm = P.sb("offtm", [128, 128], F32); b_offtm = Buf("offtm")
        MR = {}
        mod_d = nc.dram_tensor("mod_d", [6 * D], F32).ap()
        selt = P.sb("selt", [128, 4], F32); b_sel = Buf("selt")

        def pool_sel(out, in_, pattern, op, fill, base, cm, rd, wr):
            P.op("pool", lambda e: e.affine_select(out=out, in_=in_, pattern=pattern, compare_op=op, fill=fill,
                                                   base=base, channel_multiplier=cm), reads=rd, writes=wr)

        P.op("pool", lambda e: e.memset(onesf[:], 1.0), writes=[b_onesf])
        pool_sel(identf[:], onesf[:], [[-1, 128]], ALU.is_equal, 0.0, 0, 1, [b_onesf], [b_identf])
        P.op("dve", lambda e: e.tensor_copy(out=ident[:], in_=identf[:]), reads=[b_identf], writes=[b_ident])
        pool_sel(trile[:], onesf[:], [[1, 128]], ALU.is_ge, 0.0, 0, -1, [b_onesf], [b_trile])
        pool_sel(slm[:], onesf[:], [[-1, 128]], ALU.is_gt, 0.0, 0, 1, [b_onesf], [b_slm])
        zf = P.sb("zf", [128, 128], F32); b_zf = Buf("zf")
        P.op("pool", lambda e: e.memset(zf[:], 0.0), writes=[b_zf])
        pool_sel(negs[:], zf[:], [[-1, 128]], ALU.is_gt, NEG, 0, 1, [b_zf], [b_negs])
        pool_sel(neg2[:], zf[:], [[1, 128]], ALU.is_ge, NEG, 0, -1, [b_zf], [b_neg2])
        P.op("pool", lambda e: e.memset(bdm[:], 0.0), writes=[b_bdm])
        P.op("pool", lambda e: e.memset(bdm[0:64, 0:64], 1.0), reads=[b_bdm], writes=[b_bdm])
        P.op("pool", lambda e: e.memset(bdm[64:128, 64:128], 1.0), reads=[b_bdm], writes=[b_bdm])
        bdsl = P.sb("bdsl", [128, 128], F32); b_bdsl = Buf("bdsl")
        bdsu = P.sb("bdsu", [128, 128], F32); b_bdsu = Buf("bdsu")
        pool_sel(bdsl[:], bdm[:], [[-1, 128]], ALU.is_gt, 0.0, 0, 1, [b_bdm], [b_bdsl])
        pool_sel(bdsu[:], bdm[:], [[1, 128]], ALU.is_gt, 0.0, 0, -1, [b_bdm], [b_bdsu])
        P.op("pool", lambda e: e.memset(offm[:], 0.0), writes=[b_offm])
        P.op("pool", lambda e: e.memset(offm[64:128, 0:64], 1.0), reads=[b_offm], writes=[b_offm])
        P.op("pool", lambda e: e.memset(offtm[:], 0.0), writes=[b_offtm])
        P.op("pool", lambda e: e.memset(offtm[0:64, 64:128], 1.0), reads=[b_offtm], writes=[b_offtm])
        P.dma("sp", lambda e: e.dma_start(out=selt[:], in_=dr["sel"]), writes=[b_sel])

        with ExitStack() as st:
            P.stack = st
            modrow = P.sb("modrow_s", [128, 6, D], F32); b_mod = Buf("modrow_s")
            cTt = P.sb("cTt", [128, 8], F32); b_cT = Buf("cTt")
            scT = P.sb("scT", [128, 8], BF16); b_scT = Buf("scT")
            nrow = P.sb("nrow", [128, 2, D], F32); b_nrow = Buf("nrow")
            was = [P.sb("wa%d" % i, [128, 8, 512], BF16) for i in range(2)]; b_wa = bufs("wa", 2)
            pm = [P.ps("pm%d" % i, [128, 512], F32) for i in range(2)]; b_pm = bufs("pm", 2)
            P.dma("sp", lambda e: e.dma_start(out=cTt[:], in_=dr["cT"]), writes=[b_cT])
            P.dma("sp", lambda e: e.dma_start(out=modrow[:].rearrange("p a d -> p (a d)"),
                                              in_=dr["b_ada"].partition_broadcast(128)), writes=[b_mod])
            P.dma("sp", lambda e: e.dma_start(out=nrow[:, 0, :], in_=dr["norm1_w"].partition_broadcast(128)), writes=[b_nrow])
            P.dma("sp", lambda e: e.dma_start(out=nrow[:, 1, :], in_=dr["norm2_w"].partition_broadcast(128)), writes=[b_nrow])
            P.op("act", lambda e: e.activation(out=scT[:], in_=cTt[:], func=AF.Silu), reads=[b_cT], writes=[b_scT])
            wa_v = dr["w_ada"].rearrange("(k p) n -> p k n", p=128)
            for cc in range(12):
                s = cc % 2
                P.dma("pool", lambda e, s=s, cc=cc: e.dma_start(out=was[s][:], in_=wa_v[:, :, cc * 512:(cc + 1) * 512]),
                      writes=[b_wa[s]])
                for k in range(8):
                    P.op("pe", lambda e, s=s, k=k: e.matmul(pm[s][:], lhsT=scT[:, k:k + 1].to_broadcast([128, 128]),
                                                            rhs=was[s][:, k, :], start=(k == 0), stop=(k == 7)),
                         reads=[b_scT, b_wa[s]], writes=[b_pm[s]])
                a, o = cc // 2, (cc % 2) * 512
                P.op("dve", lambda e, s=s, a=a, o=o: e.tensor_tensor(out=modrow[:, a, o:o + 512], in0=modrow[:, a, o:o + 512],
                                                                  in1=pm[s][:], op=ALU.add),
                     reads=[b_pm[s], b_mod], writes=[b_mod])
            for (a, n) in ((1, 0), (4, 1)):
                P.op("dve", lambda e, a=a, n=n: e.scalar_tensor_tensor(out=modrow[:, a, :], in0=modrow[:, a, :], scalar=1.0,
                                                                      in1=nrow[:, n, :], op0=ALU.add, op1=ALU.mult),
                     reads=[b_mod, b_nrow], writes=[b_mod])
            P.dma("sp", lambda e: e.dma_start(out=mod_d.rearrange("(a n) -> a n", a=1), in_=modrow[0:1, :, :].rearrange("p a d -> p (a d)")),
                  reads=[b_mod], track=b_mod)
            P.flush()
        P.stack = gst

        def normmod(xt, bx, hb, bh, sq, bsq, st_, bst, arow, brow):
            P.op("pool", lambda e: e.memset(st_[:, 0:1], 0.0), writes=[bst])
            P.op("act", lambda e: e.activation(out=sq, in_=xt, func=AF.Square, accum_out=st_[:, 0:1]),
                 reads=[bx], writes=[bsq, bst])
            P.op("act", lambda e: e.activation(out=st_[:, 1:2], in_=st_[:, 0:1], func=AF.Ln, scale=1.0 / D, bias=EPS),
                 reads=[bst], writes=[bst])
            P.op("act", lambda e: e.activation(out=st_[:, 2:3], in_=st_[:, 1:2], func=AF.Exp, scale=-0.5), reads=[bst], writes=[bst])
            mr_, bmr_ = MR["t"], MR["b"]
            P.op("dve", lambda e: e.scalar_tensor_tensor(out=sq, in0=xt, scalar=st_[:, 2:3], in1=mr_[:, arow, :],
                                                          op0=ALU.mult, op1=ALU.mult),
                 reads=[bx, bst, bmr_, bsq], writes=[bsq])
            P.op("dve", lambda e: e.tensor_tensor(out=hb, in0=sq, in1=mr_[:, brow, :], op=ALU.add),
                 reads=[bsq, bmr_], writes=[bh])

        G_ = locals()
        if "p2" in phases:
            phase2(P, nc, dr, G_)
        if "p3a" in phases:
            phase3a(P, nc, dr, G_)
        if "p3b" in phases:
            phase3b(P, nc, dr, G_)
        if any(P.q[e] for e in ENGS):
            P.flush()
    return nc


def phase2(P, nc, dr, G):
    gst = P.stack
    ident, b_ident, identf, b_identf = G["ident"], G["b_ident"], G["identf"], G["b_identf"]
    onesf, b_onesf, trile, b_trile, slm, b_slm = G["onesf"], G["b_onesf"], G["trile"], G["b_trile"], G["slm"], G["b_slm"]
    negs, b_negs, neg2, b_neg2 = G["negs"], G["b_negs"], G["neg2"], G["b_neg2"]
    bdm, b_bdm, offm, b_offm, offtm, b_offtm = G["bdm"], G["b_bdm"], G["offm"], G["b_offm"], G["offtm"], G["b_offtm"]
    bdsl, b_bdsl, bdsu, b_bdsu = G["bdsl"], G["b_bdsl"], G["bdsu"], G["b_bdsu"]
    selt, b_sel, normmod = G["selt"], G["b_sel"], G["normmod"]
    ckv_tm_d, ckvT_d, kidxT_d, oown_d = G["ckv_tm_d"], G["ckvT_d"], G["kidxT_d"], G["oown_d"]
    uv_d = G["uv_d"]
    with ExitStack() as st:
        P.stack = st
        modrow = P.sb("modrow_2", [128, 2, D], F32); b_mod = Buf("modrow_2")
        P.dma("sp", lambda e: e.dma_start(out=modrow[:].rearrange("p a d -> p (a d)"), in_=G["mod_d"][0:2 * D].partition_broadcast(128)), writes=[b_mod])
        G["MR"]["t"], G["MR"]["b"] = modrow, b_mod
        wqkv = P.sb("wqkv", [128, 8, 3072], BF16); b_wqkv = Buf("wqkv")
        wkvba = P.sb("wkvba", [128, 8, 336], BF16); b_wkvba = Buf("wkvba")
        win_v = dr["w_in"].rearrange("(k p) n -> p k n", p=128)
        for k in range(8):
            P.dma("pool", lambda e, k=k: e.dma_start(out=wqkv[:, k, :], in_=win_v[:, k, 0:3072]), writes=[b_wqkv])
        P.dma("pool", lambda e: e.dma_start(out=wkvba[:, :, 0:320], in_=win_v[:, :, C_KV:C_KV + 320]), writes=[b_wkvba])
        P.dma("pool", lambda e: e.dma_start(out=wkvba[:, :, 320:336], in_=win_v[:, :, C_B:C_B + 16]), writes=[b_wkvba])
        convw = P.sb("convw", [128, 24, 4], F32); b_convw = Buf("convw")
        P.dma("sp", lambda e: e.dma_start(out=convw[:], in_=dr["dn_conv_w"]), writes=[b_convw])
        rows = P.sb("rows", [128, 16 + 256 + 64], F32); b_rows = Buf("rows")
        P.dma("sp", lambda e: e.dma_start(out=rows[:, 0:8], in_=dr["dn_a_log"].partition_broadcast(128)), writes=[b_rows])
        P.dma("sp", lambda e: e.dma_start(out=rows[:, 8:16], in_=dr["dn_dt_bias"].partition_broadcast(128)), writes=[b_rows])
        P.dma("sp", lambda e: e.dma_start(out=rows[:, 16:272], in_=dr["kv_norm_w"].partition_broadcast(128)), writes=[b_rows])
        P.dma("sp", lambda e: e.dma_start(out=rows[:, 272:336], in_=dr["idx_k_norm_w"].partition_broadcast(128)), writes=[b_rows])
        nea = P.sb("nea", [128, 8], F32); b_nea = Buf("nea")
        P.op("act", lambda e: e.activation(out=nea[:], in_=rows[:, 0:8], func=AF.Exp), reads=[b_rows], writes=[b_nea])
        P.op("dve", lambda e: e.tensor_scalar(out=nea[:], in0=nea[:], scalar1=-1.0, scalar2=None, op0=ALU.mult),
             reads=[b_nea], writes=[b_nea])

        xts = [P.sb("xt%d" % i, [128, D], F32) for i in range(1)] * 2; b_xt = bufs("xt", 1) * 2
        sqs = P.sb("sq", [128, D], F32); b_sq = Buf("sq")
        stt = [P.sb("stt%d" % i, [128, 4], F32) for i in range(1)] * 2; b_stt = bufs("stt", 1) * 2
        hbs = [P.sb("hb%d" % i, [128, D], BF16) for i in range(1)] * 2; b_hb = bufs("hb", 1) * 2
        hTg = [P.sb("hTg%d" % i, [128, 8, 512], BF16) for i in range(1)] * 2; b_hTg = bufs("hTg", 1) * 2
        kvba = [P.sb("kvba%d" % i, [128, 336], F32) for i in range(4)]; b_kvba = bufs("kvba", 4)
        hist = P.sb("hist", [128, 24, 3], F32); b_hist = bufs("hist", 24)
        pcs = [P.sb("pc%d" % i, [128, 515], F32) for i in range(2)]; b_pc = bufs("pc", 2)
        cacc = [P.sb("cacc%d" % i, [128, 512], F32) for i in range(2)]; b_cacc = bufs("cacc", 2)
        qkvs = P.sb("qkvs", [128, 24, 512], BF16); b_qkvs = bufs("qkvs", 24)
        S = P.sb("S", [128, 8, 128], F32); b_S = bufs("S", 8)
        Sb = P.sb("Sb", [128, 8, 128], BF16); b_Sb = bufs("Sb", 8)
        oacc = [P.sb("oacc%d" % i, [128, D], F32) for i in range(1)] * 2; b_oacc = bufs("oacc", 1) * 2
        tms = [P.sb("tm%d" % i, [128, 3, 8, 128], BF16) for i in range(2)]
        b_tms = [[bufs("tm%d_%d_" % (i, w), 8) for w in range(3)] for i in range(2)]
        scs2 = [P.sb("sc%d" % i, [128, 24, 8], F32) for i in range(2)]; b_scs2 = bufs("sc", 2)
        sqt = sqs[:, :].rearrange("p (a b) -> p a b", a=8); b_sqt = b_sq
        Rs = [P.sb("R%d" % i, [128, 8, 256], BF16) for i in range(2)]; b_Rs = [bufs("R%d_" % i, 8) for i in range(2)]
        Ks2s = [P.sb("Ks2_%d" % i, [128, 8, 128], BF16) for i in range(2)]; b_Ks2s = bufs("Ks2_", 2)
        Lgs = [P.sb("Lg%d" % i, [128, 8, 128], F32) for i in range(2)]; b_Qms = [bufs("Qm%d_" % i, 8) for i in range(2)]
        E = P.sb("E", [128, 8, 128], F32); b_E = bufs("E", 8); och = E; b_och = b_E
        ET = P.sb("ET", [128, 8, 128], F32); b_ET = bufs("ET", 2)
        Np = P.sb("Np", [128, 8, 128], F32); b_Np = bufs("Np", 8)
        NA = [P.sb("NA%d" % i, [128, 8, 128], F32) for i in range(2)]; b_NA = [bufs("NA%d_" % i, 8) for i in range(2)]
        NT = [P.sb("NT%d" % i, [128, 8, 128], F32) for i in range(2)]; b_NT = [bufs("NT%d_" % i, 8) for i in range(2)]
        Qb = P.sb("Qb", [128, 8, 128], BF16); b_Qb = bufs("Qb", 8)
        y1 = P.sb("y1", [128, 8, 256], BF16); b_y1 = bufs("y1", 8); R2 = y1; b_R2 = b_y1
        aT = P.sb("aT", [128, 8, 128], BF16); b_aT = bufs("aT", 8)
        o1 = Np; b_o1 = b_Np
        ckvn = P.sb("ckvn", [128, 320], BF16); b_ckvn = Buf("ckvn")
        ckvT_s = P.sb("ckvT_s", [128, 3, 128], BF16); b_ckvTs = Buf("ckvT_s")
        b_uvd = Buf("uvd")
        psA = P.ps("psA", [128, 512], F32); b_psA = Buf("psA")
        psB = P.ps("psB", [128, 512], F32); b_psB = Buf("psB")
        psT = P.ps("psT", [128, 8, 128], BF16); b_psT = Buf("psT")
        psH = [P.ps("psH%d" % i, [128, 4, 128], F32) for i in range(4)]; b_psH = bufs("psH", 4)
        psS = P.ps("psS", [128, 512], F32); b_psS = Buf("psS")

        P.op("pool", lambda e: e.memset(hist[:], 0.0), writes=b_hist)
        P.op("pool", lambda e: e.memset(S[:], 0.0), writes=b_S)
        P.op("pool", lambda e: e.memset(Sb[:], 0.0), writes=b_Sb)

        def mm(out, lhsT, rhs, start, stop, rd, wr):
            P.op("pe", lambda e: e.matmul(out, lhsT=lhsT, rhs=rhs, start=start, stop=stop), reads=rd, writes=wr)

        def tr(out, in_, idn, rd, wr):
            P.op("pe", lambda e: e.transpose(out=out, in_=in_, identity=idn), reads=rd, writes=wr)

        def front(g):
            hs = 0
            for cb in range(4):
                blk = 4 * g + cb
                xs = blk % 2
                P.dma("sp", lambda e, xs=xs, blk=blk: e.dma_start(out=xts[xs][:], in_=dr["xf"][blk * 128:(blk + 1) * 128, :]),
                      writes=[b_xt[xs]])
                normmod(xts[xs][:], b_xt[xs], hbs[xs][:], b_hb[xs], sqs[:], b_sq, stt[xs], b_stt[xs], 1, 0)
                for k in range(8):
                    tr(psT[:, k, :], hbs[xs][:, k * 128:(k + 1) * 128], ident[:], [b_hb[xs], b_ident], [b_psT])
                P.op("act", lambda e, hs=hs, cb=cb: e.copy(out=hTg[hs][:, :, cb * 128:(cb + 1) * 128], in_=psT[:]),
                     reads=[b_psT], writes=[b_hTg[hs]])
                for k in range(8):
                    mm(psS[:, 0:336], hTg[hs][:, k, cb * 128:(cb + 1) * 128], wkvba[:, k, :], k == 0, k == 7,
                       [b_hTg[hs], b_wkvba], [b_psS])
                P.op("act", lambda e, cb=cb: e.copy(out=kvba[cb][:], in_=psS[:, 0:336]), reads=[b_psS], writes=[b_kvba[cb]])

        front(0)
        for g in range(NCH // 4):
            hs = 0
            for cc in range(24):
                ps_, bps_ = (psA, b_psA) if cc % 2 == 0 else (psB, b_psB)
                p = cc % 2
                for k in range(8):
                    mm(ps_[:], wqkv[:, k, cc * 128:(cc + 1) * 128], hTg[hs][:, k, :], k == 0, k == 7,
                       [b_wqkv, b_hTg[hs]], [bps_])
                P.op("act", lambda e, p=p, ps_=ps_: e.copy(out=pcs[p][:, 3:515], in_=ps_[:]), reads=[bps_], writes=[b_pc[p]])
                P.op("dve", lambda e, p=p, cc=cc: e.tensor_copy(out=pcs[p][:, 0:3], in_=hist[:, cc, :]),
                     reads=[b_hist[cc], b_pc[p]], writes=[b_pc[p]])
                P.op("act", lambda e, ps_=ps_, cc=cc: e.copy(out=hist[:, cc, :], in_=ps_[:, 509:512]), reads=[bps_], writes=[b_hist[cc]])
                P.op("dve", lambda e, p=p, cc=cc: e.tensor_scalar(out=cacc[p][:], in0=pcs[p][:, 3:515], scalar1=convw[:, cc, 3:4],
                                                               scalar2=None, op0=ALU.mult),
                     reads=[b_pc[p], b_convw], writes=[b_cacc[p]])
                for i in range(3):
                    P.op("dve", lambda e, p=p, cc=cc, i=i: e.scalar_tensor_tensor(
                        out=cacc[p][:], in0=pcs[p][:, i:i + 512], scalar=convw[:, cc, i:i + 1], in1=cacc[p][:],
                        op0=ALU.mult, op1=ALU.add), reads=[b_pc[p], b_convw, b_cacc[p]], writes=[b_cacc[p]])
                P.op("act", lambda e, p=p, cc=cc: e.activation(out=qkvs[:, cc, :], in_=cacc[p][:], func=AF.Silu),
                     reads=[b_cacc[p]], writes=[b_qkvs[cc]])

            def S1(cb, par):
                tm, b_tm, sc, b_sc, R, b_R, Ks2, b_Ks2 = tms[par], b_tms[par], scs2[par], b_scs2[par], Rs[par], b_Rs[par], Ks2s[par], b_Ks2s[par]
                Lg, b_Qm = Lgs[par], b_Qms[par]
                Qm = Lg
                NoT, b_NoT, sol, b_sol, wT, b_wT, vn, b_vn = tm[:, 2, :, :], b_tm[2], R, b_R, tm[:, 1, :, :], b_tm[1], tm[:, 0, :, :], b_tm[0]
                blk = 4 * g + cb
                c0 = cb * 128
                kb = kvba[cb]; bkb = b_kvba[cb]
                P.op("pool", lambda e: e.memset(sc[:, 20, 0:2], 0.0), writes=[b_sc])
                P.op("act", lambda e, kb=kb: e.activation(out=sqt[:, 0:2, :].rearrange("p a b -> p (a b)"), in_=kb[:, 0:256],
                                                          func=AF.Square, accum_out=sc[:, 20, 0:1]),
                     reads=[bkb, b_sc], writes=[b_sqt, b_sc])
                P.op("act", lambda e, kb=kb: e.activation(out=sqt[:, 2, 0:64], in_=kb[:, 256:320],
                                                          func=AF.Square, accum_out=sc[:, 20, 1:2]),
                     reads=[bkb, b_sc], writes=[b_sqt, b_sc])
                P.op("act", lambda e: e.activation(out=sc[:, 20, 2:3], in_=sc[:, 20, 0:1], func=AF.Ln, scale=1.0 / 256, bias=EPS),
                     reads=[b_sc], writes=[b_sc])
                P.op("act", lambda e: e.activation(out=sc[:, 20, 3:4], in_=sc[:, 20, 1:2], func=AF.Ln, scale=1.0 / 64, bias=EPS),
                     reads=[b_sc], writes=[b_sc])
                P.op("act", lambda e: e.activation(out=sc[:, 20, 4:6], in_=sc[:, 20, 2:4], func=AF.Exp, scale=-0.5), reads=[b_sc], writes=[b_sc])
                P.op("dve", lambda e, kb=kb: e.scalar_tensor_tensor(out=ckvn[:, 0:256], in0=kb[:, 0:256], scalar=sc[:, 20, 4:5],
                                                                    in1=rows[:, 16:272], op0=ALU.mult, op1=ALU.mult),
                     reads=[bkb, b_sc, b_rows], writes=[b_ckvn])
                P.op("dve", lambda e, kb=kb: e.scalar_tensor_tensor(out=ckvn[:, 256:320], in0=kb[:, 256:320], scalar=sc[:, 20, 5:6],
                                                                    in1=rows[:, 272:336], op0=ALU.mult, op1=ALU.mult),
                     reads=[bkb, b_sc, b_rows, b_ckvn], writes=[b_ckvn])
                P.dma("sp", lambda e, blk=blk: e.dma_start(out=ckv_tm_d[blk * 128:(blk + 1) * 128, :], in_=ckvn[:, 0:256]),
                      reads=[b_ckvn], track=b_ckvn)
                for i in range(2):
                    tr(psT[:, i, :], ckvn[:, i * 128:(i + 1) * 128], ident[:], [b_ckvn, b_ident], [b_psT])
                tr(psT[0:64, 2, :], ckvn[:, 256:320], ident[:], [b_ckvn, b_ident], [b_psT])
                P.op("act", lambda e: e.copy(out=ckvT_s[:, 0:2, :], in_=psT[:, 0:2, :]), reads=[b_psT], writes=[b_ckvTs])
                P.op("act", lambda e: e.copy(out=ckvT_s[0:64, 2, :], in_=psT[0:64, 2, :]), reads=[b_psT, b_ckvTs], writes=[b_ckvTs])
                P.dma("sp", lambda e, blk=blk: e.dma_start(
                    out=ckvT_d[:, blk * 128:(blk + 1) * 128].rearrange("(a p) t -> p a t", p=128), in_=ckvT_s[:, 0:2, :]),
                    reads=[b_ckvTs], track=b_ckvTs)
                P.dma("sp", lambda e, blk=blk: e.dma_start(out=kidxT_d[:, blk * 128:(blk + 1) * 128], in_=ckvT_s[0:64, 2, :]),
                      reads=[b_ckvTs], track=b_ckvTs)

                for tb in range(2):
                    r0 = blk * 256
                    src = dr["peer_u" if tb == 0 else "peer_v"][r0:r0 + 256, :]
                    dst = uv_d[r0:r0 + 256, tb * D:(tb + 1) * D]
                    P.dma("pool", lambda e, src=src, dst=dst: e.dma_start(out=dst, in_=src), writes=[b_uvd], track=b_uvd)
                for w in range(3):
                    for h in range(8):
                        tr(psT[:, h, :], qkvs[:, 8 * w + h, c0:c0 + 128], ident[:], [b_qkvs[8 * w + h], b_ident], [b_psT])
                    P.op("act" if w != 1 else "dve",
                         (lambda e, w=w: e.copy(out=tm[:, w, :, :], in_=psT[:])) if w != 1 else
                         (lambda e, w=w: e.tensor_copy(out=tm[:, w, :, :], in_=psT[:])),
                         reads=[b_psT], writes=b_tm[w])
                def scop(eng, fn, extra_r=()):
                    P.op(eng, fn, reads=[b_sc] + list(extra_r), writes=[b_sc])
                scop("act", lambda e, kb=kb: e.activation(out=sc[:, 0, :], in_=kb[:, 320:328], func=AF.Exp, scale=-1.0), [bkb])
                scop("dve", lambda e: e.tensor_scalar(out=sc[:, 0, :], in0=sc[:, 0, :], scalar1=1.0, scalar2=None, op0=ALU.add))
                scop("dve", lambda e: e.reciprocal(out=sc[:, 0, :], in_=sc[:, 0, :]))
                scop("dve", lambda e, kb=kb: e.tensor_tensor(out=sc[:, 16, :], in0=kb[:, 328:336], in1=rows[:, 8:16], op=ALU.add),
                     [bkb, b_rows])
                scop("act", lambda e: e.activation(out=sc[:, 16, :], in_=sc[:, 16, :], func=AF.Exp))
                scop("act", lambda e: e.activation(out=sc[:, 16, :], in_=sc[:, 16, :], func=AF.Ln, bias=1.0))
                scop("dve", lambda e: e.tensor_tensor(out=sc[:, 1, :], in0=sc[:, 16, :], in1=nea[:], op=ALU.mult), [b_nea])
                mm(psS[:, 0:8], trile[:], sc[:, 1, :], True, True, [b_trile, b_sc], [b_psS])
                mm(psS[:, 8:16], onesf[:], sc[:, 1, :], True, True, [b_onesf, b_sc], [b_psS])
                scop("act", lambda e: e.copy(out=sc[:, 2:4, :].rearrange("p a b -> p (a b)"), in_=psS[:, 0:16]), [b_psS])
                scop("act", lambda e: e.activation(out=sc[:, 4, :], in_=sc[:, 2, :], func=AF.Exp))
                scop("dve", lambda e: e.tensor_tensor(out=sc[:, 16, :], in0=sc[:, 3, :], in1=sc[:, 2, :], op=ALU.subtract))
                scop("act", lambda e: e.activation(out=sc[:, 5, :], in_=sc[:, 16, :], func=AF.Exp))
                scop("act", lambda e: e.activation(out=sc[:, 6, :], in_=sc[:, 3, :], func=AF.Exp))
                for (w, row) in ((0, 7), (1, 8)):
                    P.op("act", lambda e, w=w: e.activation(out=sqt[:].rearrange("p a b -> p (a b)"),
                                                            in_=tm[:, w, :, :].rearrange("p a b -> p (a b)"), func=AF.Square),
                         reads=b_tm[w], writes=[b_sqt])
                    scop("dve", lambda e, row=row: e.reduce_sum(out=sc[:, row, :], in_=sqt[:], axis=AX.X), [b_sqt])
                scop("act", lambda e: e.activation(out=sc[:, 7:9, :], in_=sc[:, 7:9, :], func=AF.Ln, bias=EPS))
                scop("act", lambda e: e.activation(out=sc[:, 9, :], in_=sc[:, 8, :], func=AF.Exp, scale=-1.0))
                scop("act", lambda e: e.activation(out=sc[:, 10, :], in_=sc[:, 8, :], func=AF.Exp, scale=-0.5))
                scop("act", lambda e: e.activation(out=sc[:, 11, :], in_=sc[:, 7, :], func=AF.Exp, scale=-0.5))
                scop("dve", lambda e: e.tensor_scalar(out=sc[:, 11, :], in0=sc[:, 11, :], scalar1=128.0 ** -0.5, scalar2=None, op0=ALU.mult))
                scop("dve", lambda e: e.tensor_tensor(out=sc[:, 12, :], in0=sc[:, 11, :], in1=sc[:, 4, :], op=ALU.mult))
                scop("dve", lambda e: e.tensor_tensor(out=sc[:, 13, :], in0=sc[:, 10, :], in1=sc[:, 0, :], op=ALU.mult))
                scop("dve", lambda e: e.tensor_tensor(out=sc[:, 16, :], in0=sc[:, 9, :], in1=sc[:, 0, :], op=ALU.mult))
                scop("dve", lambda e: e.tensor_tensor(out=sc[:, 14, :], in0=sc[:, 16, :], in1=sc[:, 4, :], op=ALU.mult))
                scop("dve", lambda e: e.tensor_scalar(out=sc[:, 15, :], in0=sc[:, 16, :], scalar1=-1.0, scalar2=None, op0=ALU.mult))
                P.op("dve", lambda e: e.tensor_tensor(out=R[:, :, 0:128], in0=tm[:, 2, :, :],
                                                      in1=sc[:, 13, :].unsqueeze(2).to_broadcast([128, 8, 128]), op=ALU.mult),
                     reads=b_tm[2] + [b_sc], writes=b_R)
                P.op("dve", lambda e: e.tensor_tensor(out=R[:, :, 128:256], in0=tm[:, 1, :, :],
                                                      in1=sc[:, 14, :].unsqueeze(2).to_broadcast([128, 8, 128]), op=ALU.mult),
                     reads=b_tm[1] + [b_sc] + b_R, writes=b_R)
                P.op("dve", lambda e: e.tensor_tensor(out=Ks2[:], in0=tm[:, 1, :, :],
                                                       in1=sc[:, 5, :].unsqueeze(2).to_broadcast([128, 8, 128]), op=ALU.mult),
                     reads=b_tm[1] + [b_sc], writes=[b_Ks2])
                P.op("dve", lambda e: e.tensor_tensor(out=Lg[:], in0=slm[:].unsqueeze(1).to_broadcast([128, 8, 128]),
                                                       in1=sc[:, 1, :].unsqueeze(2).to_broadcast([128, 8, 128]), op=ALU.mult),
                     reads=[b_slm, b_sc], writes=b_Qm)

            def S234(cb, par):
                tm, b_tm, sc, b_sc, R, b_R, Ks2, b_Ks2 = tms[par], b_tms[par], scs2[par], b_scs2[par], Rs[par], b_Rs[par], Ks2s[par], b_Ks2s[par]
                Lg, b_Qm = Lgs[par], b_Qms[par]
                Qm = Lg
                NoT, b_NoT, sol, b_sol, wT, b_wT, vn, b_vn = tm[:, 2, :, :], b_tm[2], R, b_R, tm[:, 1, :, :], b_tm[1], tm[:, 0, :, :], b_tm[0]
                blk = 4 * g + cb
                c0 = cb * 128
                kb = kvba[cb]; bkb = b_kvba[cb]
                def b4(row, hh):
                    return sc[:, row, 4 * hh:4 * hh + 4].unsqueeze(2).to_broadcast([128, 4, 128])
                CB = (psA, psB); b_CB = (b_psA, b_psB)
                def g_(hh):
                    hs4 = slice(4 * hh, 4 * hh + 4)
                    for i in range(4):
                        h = 4 * hh + i
                        mm(psH[hh][:, i, :], trile[:], Lg[:, h, :], True, True, [b_trile, b_Qm[h]], [b_psH[hh]])
                    P.op("act", lambda e, hh=hh, hs4=hs4: e.activation(out=E[:, hs4, :], in_=psH[hh][:], func=AF.Exp),
                         reads=[b_psH[hh]], writes=b_E[hs4])
                    yield
                    for i in range(4):
                        h = 4 * hh + i
                        mm(psH[2 + hh][:, i, :], Lg[:, h, :], trile[:], True, False, [b_trile, b_Qm[h]], [b_psH[2 + hh]])
                        mm(psH[2 + hh][:, i, :], identf[:], neg2[:], False, True, [b_identf, b_neg2], [b_psH[2 + hh]])
                    P.op("act", lambda e, hh=hh, hs4=hs4: e.activation(out=ET[:, hs4, :], in_=psH[2 + hh][:], func=AF.Exp),
                         reads=[b_psH[2 + hh]], writes=[b_ET[hh]])
                    yield
                    P.op("dve", lambda e, hh=hh, hs4=hs4: e.tensor_tensor(out=E[:, hs4, :], in0=E[:, hs4, :], in1=b4(15, hh), op=ALU.mult),
                         reads=b_E[hs4] + [b_sc], writes=b_E[hs4])
                    yield
                rr(*[g_(v_) for v_ in range(2)])
                def g_(hh):
                    hs4 = slice(4 * hh, 4 * hh + 4)
                    for i in range(4):
                        h = 4 * hh + i
                        kT = qkvs[:, 8 + h, c0:c0 + 128]
                        mm(psH[hh][:, i, :], kT, kT, True, True, [b_qkvs[8 + h]], [b_psH[hh]])
                    P.op("dve", lambda e, hh=hh, hs4=hs4: e.tensor_tensor(out=Np[:, hs4, :], in0=psH[hh][:], in1=E[:, hs4, :], op=ALU.mult),
                         reads=[b_psH[hh]] + b_E[hs4], writes=b_Np[hs4])
                    yield
                    for i in range(4):
                        h = 4 * hh + i
                        tr(psH[2 + hh][:, i, :], Np[:, h, :], identf[:], [b_Np[h], b_identf], [b_psH[2 + hh]])
                    P.op("dve", lambda e, hh=hh, hs4=hs4: e.tensor_tensor(out=NT[0][:, hs4, :], in0=psH[2 + hh][:],
                                                                       in1=bdsu[:].unsqueeze(1).to_broadcast([128, 4, 128]), op=ALU.mult),
                         reads=[b_psH[2 + hh], b_bdsu], writes=b_NT[0][hs4])
                    yield
                    P.op("dve", lambda e, hh=hh, hs4=hs4: e.tensor_tensor(out=NoT[:, hs4, :], in0=psH[2 + hh][:],
                                                                       in1=offtm[:].unsqueeze(1).to_broadcast([128, 4, 128]), op=ALU.mult),
                         reads=[b_psH[2 + hh], b_offtm], writes=b_NoT[hs4])
                    yield
                    P.op("dve", lambda e, hs4=hs4: e.tensor_tensor(out=NA[0][:, hs4, :], in0=Np[:, hs4, :],
                                                                    in1=bdsl[:].unsqueeze(1).to_broadcast([128, 4, 128]), op=ALU.mult),
                         reads=b_Np[hs4] + [b_bdsl], writes=b_NA[0][hs4])
                    yield
                    P.op("dve", lambda e, hs4=hs4: e.tensor_tensor(out=Qm[:, hs4, :], in0=NT[0][:, hs4, :],
                                                                    in1=identf[:].unsqueeze(1).to_broadcast([128, 4, 128]), op=ALU.add),
                         reads=b_NT[0][hs4] + [b_identf], writes=b_Qm[hs4])
                    yield
                rr(*[g_(v_) for v_ in range(2)])
                cur = 0
                for lev in range(5):
                    nxt = 1 - cur
                    last = (lev == 4)
                    def g_(hh):
                        hs4 = slice(4 * hh, 4 * hh + 4)
                        for i in range(4):
                            h = 4 * hh + i
                            mm(psH[hh][:, i, :], NT[cur][:, h, :], NA[cur][:, h, :], True, True,
                               [b_NT[cur][h], b_NA[cur][h]], [b_psH[hh]])
                        P.op("act", lambda e, hh=hh, hs4=hs4, nxt=nxt: e.copy(out=NA[nxt][:, hs4, :], in_=psH[hh][:]),
                             reads=[b_psH[hh]], writes=b_NA[nxt][hs4])
                        yield
                        if not last:
                            for i in range(4):
                                h = 4 * hh + i
                                mm(psH[2 + hh][:, i, :], NA[cur][:, h, :], NT[cur][:, h, :], True, True,
                                   [b_NT[cur][h], b_NA[cur][h]], [b_psH[2 + hh]])
                            P.op("act", lambda e, hh=hh, hs4=hs4, nxt=nxt: e.copy(out=NT[nxt][:, hs4, :], in_=psH[2 + hh][:]),
                                 reads=[b_psH[2 + hh]], writes=b_NT[nxt][hs4])
                        for i in range(4):
                            h = 4 * hh + i
                            mm(CB[hh][:, i * 128:(i + 1) * 128], NA[nxt][:, h, :], Qm[:, h, :], True, True,
                               [b_NA[nxt][h], b_Qm[h]], [b_CB[hh]])
                        P.op("dve", lambda e, hh=hh, hs4=hs4: e.tensor_tensor(out=Qm[:, hs4, :], in0=CB[hh][:].rearrange("p (a b) -> p a b", a=4),
                                                                           in1=Qm[:, hs4, :], op=ALU.add),
                             reads=[b_CB[hh]] + b_Qm[hs4], writes=b_Qm[hs4])
                        yield
                    rr(*[g_(v_) for v_ in range(2)])
                    cur = nxt
                P.op("act", lambda e: e.copy(out=Qb[:], in_=Qm[:]), reads=b_Qm, writes=b_Qb)
                def g_(q):
                    hs2 = slice(2 * q, 2 * q + 2)
                    pb, bpb = psH[q], b_psH[q]
                    pbv = pb[:].rearrange("p a b -> p (a b)").rearrange("p (a b) -> p a b", a=2)
                    for i in range(2):
                        h = 2 * q + i
                        mm(pbv[:, i, :], Qb[:, h, :], R[:, h, :], True, True, [b_Qb[h], b_R[h]], [bpb])
                    P.op("act", lambda e, hs2=hs2, pbv=pbv: e.copy(out=y1[:, hs2, :], in_=pbv), reads=[bpb], writes=b_y1[hs2])
                    yield
                    for i in range(2):
                        h = 2 * q + i
                        mm(pbv[:, i, :], NoT[:, h, :], y1[:, h, :], True, True, [b_NoT[h], b_y1[h]], [bpb])
                    P.op("dve", lambda e, hs2=hs2, pbv=pbv: e.tensor_tensor(out=R2[:, hs2, :], in0=pbv, in1=R[:, hs2, :], op=ALU.add),
                         reads=[bpb] + b_R[hs2], writes=b_R2[hs2])
                    yield
                    for i in range(2):
                        h = 2 * q + i
                        mm(pbv[:, i, :], Qb[:, h, :], R2[:, h, :], True, True, [b_Qb[h], b_R2[h]], [bpb])
                    P.op("act", lambda e, hs2=hs2, pbv=pbv: e.copy(out=sol[:, hs2, :], in_=pbv), reads=[bpb], writes=b_sol[hs2])
                    yield
                rr(*[g_(v_) for v_ in range(4)])
                for h in range(8):
                    tr(psT[:, h, :], sol[:, h, 128:256], ident[:], [b_sol[h], b_ident], [b_psT])
                P.op("act", lambda e: e.copy(out=wT, in_=psT[:]), reads=[b_psT], writes=b_wT)
                def g_(hh):
                    hs4 = slice(4 * hh, 4 * hh + 4)
                    for i in range(4):
                        h = 4 * hh + i
                        mm(psH[hh][:, i, :], qkvs[:, 8 + h, c0:c0 + 128], qkvs[:, h, c0:c0 + 128], True, True,
                           [b_qkvs[8 + h], b_qkvs[h]], [b_psH[hh]])
                    P.op("dve", lambda e, hh=hh, hs4=hs4: e.tensor_tensor(out=aT[:, hs4, :], in0=psH[hh][:], in1=ET[:, hs4, :], op=ALU.mult),
                         reads=[b_psH[hh], b_ET[hh]], writes=b_aT[hs4])
                    yield
                rr(*[g_(v_) for v_ in range(2)])
                def g_(hh):
                    hs4 = slice(4 * hh, 4 * hh + 4)
                    for i in range(4):
                        h = 4 * hh + i
                        mm(psH[2 + hh][:, i, :], wT[:, h, :], Sb[:, h, :], True, True, [b_wT[h], b_Sb[h]], [b_psH[2 + hh]])
                    P.op("dve", lambda e, hh=hh, hs4=hs4: e.tensor_tensor(out=vn[:, hs4, :], in0=sol[:, hs4, 0:128], in1=psH[2 + hh][:],
                                                                       op=ALU.subtract),
                         reads=b_sol[hs4] + [b_psH[2 + hh]], writes=b_vn[hs4])
                    yield
                    for i in range(4):
                        h = 4 * hh + i
                        mm(CB[hh][:, i * 128:(i + 1) * 128], qkvs[:, h, c0:c0 + 128], Sb[:, h, :], True, True, [b_qkvs[h], b_Sb[h]], [b_CB[hh]])
                    P.op("dve", lambda e, hh=hh, hs4=hs4: e.tensor_tensor(out=o1[:, hs4, :], in0=CB[hh][:].rearrange("p (a b) -> p a b", a=4),
                                                                       in1=b4(12, hh), op=ALU.mult),
                         reads=[b_CB[hh], b_sc], writes=b_o1[hs4])
                    yield
                    for i in range(4):
                        h = 4 * hh + i
                        mm(psH[hh][:, i, :], aT[:, h, :], vn[:, h, :], True, True, [b_aT[h], b_vn[h]], [b_psH[hh]])
                    P.op("dve", lambda e, hh=hh, hs4=hs4: e.tensor_tensor(out=och[:, hs4, :], in0=psH[hh][:], in1=b4(11, hh), op=ALU.mult),
                         reads=[b_psH[hh], b_sc], writes=b_och[hs4])
                    yield
                    P.op("dve", lambda e, hs4=hs4: e.tensor_tensor(out=och[:, hs4, :], in0=och[:, hs4, :], in1=o1[:, hs4, :], op=ALU.add),
                         reads=b_och[hs4] + b_o1[hs4], writes=b_och[hs4])
                    yield
                    for i in range(4):
                        h = 4 * hh + i
                        mm(psH[2 + hh][:, i, :], Ks2[:, h, :], vn[:, h, :], True, True, [b_Ks2, b_vn[h]], [b_psH[2 + hh]])
                    P.op("dve", lambda e, hh=hh, hs4=hs4: e.tensor_tensor(out=S[:, hs4, :], in0=S[:, hs4, :], in1=b4(6, hh), op=ALU.mult),
                         reads=b_S[hs4] + [b_sc], writes=b_S[hs4])
                    yield
                    P.op("dve", lambda e, hh=hh, hs4=hs4: e.tensor_tensor(out=S[:, hs4, :], in0=S[:, hs4, :], in1=psH[2 + hh][:], op=ALU.add),
                         reads=b_S[hs4] + [b_psH[2 + hh]], writes=b_S[hs4])
                    yield
                    P.op("act", lambda e, hs4=hs4: e.copy(out=Sb[:, hs4, :], in_=S[:, hs4, :]), reads=b_S[hs4], writes=b_Sb[hs4])
                    yield
                rr(*[g_(v_) for v_ in range(2)])
                oa = 0
                och_f = och[:].rearrange("p a b -> p (a b)")
                if cb == 0:
                    P.op("dve", lambda e, oa=oa: e.tensor_scalar(out=oacc[oa][:], in0=och_f, scalar1=selt[:, 0:1], scalar2=None, op0=ALU.mult),
                         reads=b_och + [b_sel], writes=[b_oacc[oa]])
                else:
                    P.op("dve", lambda e, oa=oa, cb=cb: e.scalar_tensor_tensor(out=oacc[oa][:], in0=och_f, scalar=selt[:, cb:cb + 1],
                                                                              in1=oacc[oa][:], op0=ALU.mult, op1=ALU.add),
                         reads=b_och + [b_sel, b_oacc[oa]], writes=[b_oacc[oa]])
                if cb == 3:
                    P.dma("sp", lambda e, oa=oa, g=g: e.dma_start(out=oown_d[g * 128:(g + 1) * 128, :], in_=oacc[oa][:]),
                          reads=[b_oacc[oa]], track=b_oacc[oa], is_out=("dbg_o" in DEBUG))

            S1(0, (4 * g) % 2)
            for cb in range(4):
                par = (4 * g + cb) % 2
                la = P.record(lambda: S234(cb, par))
                if cb < 3:
                    lb = P.record(lambda: S1(cb + 1, 1 - par))
                else:
                    lb = P.record(lambda: front(g + 1)) if g + 1 < NCH // 4 else []
                na, nb = len(la), len(lb)
                ia = ib = 0
                cur = 0
                while ia < na or ib < nb:
                    want = 0 if (ib >= nb or (ia < na and ia * nb <= ib * na)) else 1
                    if cur == 0 and ia > 0 and ia < na and la[ia - 1][1][0] == "pe":
                        want = 0
                    elif cur == 1 and ib > 0 and ib < nb and lb[ib - 1][1][0] == "pe":
                        want = 1
                    if want == 0:
                        P.replay(la[ia:ia + 1]); ia += 1
                    else:
                        P.replay(lb[ib:ib + 1]); ib += 1
                    cur = want
        P.flush()
    P.stack = gst


def make_in_maps(inputs):
    f = lambda a: np.ascontiguousarray(np.asarray(a, dtype=np.float32))
    x = f(inputs["x"]); c = f(inputs["c"])
    shared = {
        "w_ada": f(inputs["w_ada"][0]), "b_ada": f(inputs["b_ada"][0]), "norm1_w": f(inputs["norm1_w"][0]),
        "w_in": f(inputs["w_in"][0]), "dn_conv_w": f(inputs["dn_conv_w"][0].reshape(4, 24, 128).transpose(2, 1, 0)), "dn_a_log": f(inputs["dn_a_log"][0]),
        "dn_dt_bias": f(inputs["dn_dt_bias"][0]), "dn_onorm_w": f(inputs["dn_onorm_w"][0]),
        "q_norm_w": f(inputs["q_norm_w"][0]), "kv_norm_w": f(inputs["kv_norm_w"][0]),
        "idx_k_norm_w": f(inputs["idx_k_norm_w"][0]), "w_uq": f(inputs["w_uq"][0]), "w_iq": f(inputs["w_iq"][0]),
        "w_uk": f(inputs["w_uk"][0].reshape(256, 1024)), "w_uv": f(inputs["w_uv"][0].reshape(256, 1024)),
        "w_a_out": f(inputs["w_a_out"][0]), "w_b_out": f(inputs["w_b_out"][0]), "w_o": f(inputs["w_o"][0]),
        "norm2_w": f(inputs["norm2_w"][0]), "peer_w_q": f(inputs["peer_w_q"][0]),
        "peer_sub_keys": f(inputs["peer_sub_keys"][0].reshape(16, 128, 128)),
        "peer_u": f(inputs["peer_u"][0]), "peer_v": f(inputs["peer_v"][0]), "final_norm_w": f(inputs["final_norm_w"]),
    }
    maps = []
    for k in range(8):
        b, r = k // 4, k % 4
        xb = x[b]
        xo = np.ascontiguousarray(xb.reshape(16, 4, 128, D)[:, r].reshape(NOWN * 128, D))
        sel = np.zeros((128, 4), np.float32); sel[:, r] = 1.0
        qp = np.arange(128)[:, None] + 128 * r
        kp = np.arange(512)[None, :]
        cm = np.where(kp <= qp, 0.0, -1e30).astype(np.float32)
        m = dict(shared)
        m.update({"xf": xb, "xo": xo, "cT": np.ascontiguousarray(c[b].reshape(8, 128).T), "sel": sel, "cmask": cm})
        maps.append(m)
    return maps


def kernel(**inputs):
    nc = build()
    maps = make_in_maps(inputs)
    res = run_bass_kernel_spmd(nc, maps, core_ids=list(range(8)))
    out = np.zeros((2, L, D), np.float32)
    for k in range(8):
        b, r = k // 4, k % 4
        o = np.asarray(res.results[k]["out"]).reshape(16, 128, D)
        out[b].reshape(16, 4, 128, D)[:, r] = o
    return out


def phase3a(P, nc, dr, G):
    gst = P.stack
    ident, b_ident, onesf, b_onesf = G["ident"], G["b_ident"], G["onesf"], G["b_onesf"]
    normmod = G["normmod"]
    ckv_tm_d, ckvT_d, kidxT_d, yb_d = G["ckv_tm_d"], G["ckvT_d"], G["kidxT_d"], G["yb_d"]
    with ExitStack() as st:
        P.stack = st
        modrow = P.sb("modrow_3", [128, 2, D], F32); b_mod = Buf("modrow_3")
        P.dma("sp", lambda e: e.dma_start(out=modrow[:].rearrange("p a d -> p (a d)"), in_=G["mod_d"][0:2 * D].partition_broadcast(128)), writes=[b_mod])
        G["MR"]["t"], G["MR"]["b"] = modrow, b_mod
        win_v = dr["w_in"].rearrange("(k p) n -> p k n", p=128)
        wql = P.sb("wql", [128, 8, 264], BF16); b_wql = Buf("wql")
        P.dma("pool", lambda e: e.dma_start(out=wql[:, :, 0:256], in_=win_v[:, :, C_QL:C_QL + 256]), writes=[b_wql])
        P.dma("pool", lambda e: e.dma_start(out=wql[:, :, 256:264], in_=win_v[:, :, C_WI:C_WI + 8]), writes=[b_wql])
        wuq = P.sb("wuq", [128, 2, 1024], BF16); b_wuq = Buf("wuq")
        wiq = P.sb("wiq", [128, 2, 512], BF16); b_wiq = Buf("wiq")
        wuk = P.sb("wuk", [128, 2, 1024], BF16); b_wuk = Buf("wuk")
        wuv = P.sb("wuv", [128, 2, 1024], BF16); b_wuv = Buf("wuv")
        wbs = P.sb("wbs", [128, 8, 512], BF16); b_wbs = Buf("wbs")
        wukT = P.sb("wukT", [128, 8, 256], BF16); b_wukT = Buf("wukT")
        for (t_, b_, nm) in ((wuq, b_wuq, "w_uq"), (wiq, b_wiq, "w_iq"), (wuk, b_wuk, "w_uk"), (wuv, b_wuv, "w_uv")):
            P.dma("pool", lambda e, t_=t_, nm=nm: e.dma_start(out=t_[:], in_=dr[nm].rearrange("(k p) n -> p k n", p=128)), writes=[b_])
        wb_d = G["wb_d"]
        b_wbd = Buf("wbd")
        v_ = dr["w_b_out"].rearrange("(k p) n -> p k n", p=128)
        for ci in (14, 15):
            P.dma("pool", lambda e, ci=ci, v_=v_: e.dma_start(out=wb_d[ci], in_=v_[:, :, (ci - 14) * 512:(ci - 13) * 512]), writes=[b_wbd], track=b_wbd)
        wsrc = [(win_v, C_Z), (win_v, C_Z + 512), (win_v, C_GA), (win_v, C_GA + 512), (win_v, C_GB), (win_v, C_GB + 512)]
        for nm_ in ("w_a_out", "w_o"):
            v_ = dr[nm_].rearrange("(k p) n -> p k n", p=128)
            wsrc += [(v_, 0), (v_, 512)]
        v_ = dr["peer_w_q"].rearrange("(k p) n -> p k n", p=128)
        wsrc += [(v_, 0), (v_, 512), (v_, 1024), (v_, 1536)]
        for ci, (v_, c0_) in enumerate(wsrc):
            P.dma("pool", lambda e, ci=ci, v_=v_, c0_=c0_: e.dma_start(out=wb_d[ci], in_=v_[:, :, c0_:c0_ + 512]), writes=[b_wbd], track=b_wbd)
        kidxT = P.sb("kidxT", [64, L], BF16); b_kidxT = Buf("kidxT")
        for i in range(4):
            P.dma("sp", lambda e, i=i: e.dma_start(out=kidxT[:, i * 2048:(i + 1) * 2048], in_=kidxT_d[:, i * 2048:(i + 1) * 2048]),
                  writes=[b_kidxT])
        qnrow = P.sb("qnrow", [128, 256], F32); b_qnrow = Buf("qnrow")
        P.dma("sp", lambda e: e.dma_start(out=qnrow[:], in_=dr["q_norm_w"].partition_broadcast(128)), writes=[b_qnrow])
        cmask = P.sb("cmaskt", [128, 512], F32); b_cmask = Buf("cmask")
        P.dma("sp", lambda e: e.dma_start(out=cmask[:], in_=dr["cmask"]), writes=[b_cmask])
        onesb = P.sb("onesb", [128, 128], BF16); b_onesb = Buf("onesb")
        P.op("dve", lambda e: e.tensor_copy(out=onesb[:], in_=onesf[:]), reads=[b_onesf], writes=[b_onesb])

        xt = P.sb("xt3", [128, D], F32); b_xt = Buf("xt3")
        sq = P.sb("sq3", [128, D], F32); b_sq = Buf("sq3")
        stt = P.sb("stt3", [128, 8], F32); b_stt = Buf("stt3")
        hb = P.sb("hb3", [128, D], BF16); b_hb = Buf("hb3")
        hT = P.sb("hT3", [128, 8, 128], BF16); b_hT = Buf("hT3")
        ql = P.sb("ql", [128, 264], F32); b_ql = Buf("ql")
        qln = P.sb("qln", [128, 256], BF16); b_qln = Buf("qln")
        wi = P.sb("wi", [128, 8], F32); b_wi = Buf("wi")
        qlT = P.sb("qlT", [128, 2, 128], BF16); b_qlT = Buf("qlT")
        qT = P.sb("qT", [128, 8, 128], BF16); b_qT = Buf("qT")
        qaTs = [P.sb("qaT%d" % i, [128, 2, 8, 128], BF16) for i in range(2)]; b_qaTs = bufs("qaT", 2)
        qiT = P.sb("qiT", [64, 8, 128], BF16); b_qiT = Buf("qiT")
        score = P.sb("score", [128, L], F32); b_score = Buf("score")
        wk = P.sb("wk", [128, L], F32); b_wk = Buf("wk")
        mk = wk[:, :].bitcast(BF16)
        rl = [P.sb("rl%d" % i, [128, 512], F32) for i in range(2)]; b_rl = bufs("rl", 2)
        KIT = 32
        pw = P.sb("pw", [128, KIT], F32); b_pw = Buf("pw")
        for k in range(KIT):
            P.op("pool", lambda e, k=k: e.memset(pw[:, k:k + 1], 2.0 ** -(k + 1)), reads=[b_pw], writes=[b_pw])
        bs = P.sb("bs", [128, 8], F32); b_bs = Buf("bs")
        steps = P.sb("steps", [128, 2, KIT], F32); b_steps = Buf("steps")
        cnt = P.sb("cnt", [128, KIT], F32); b_cnt = Buf("cnt")
        thr = P.sb("thr", [128, 1], F32); b_thr = Buf("thr")
        maskT = P.sb("maskT", [128, 64, 128], BF16); b_maskT = Buf("maskT")
        ckT = [P.sb("ckT%d" % i, [128, 2, 512], BF16) for i in range(2)]; b_ckT = bufs("ckT", 2)
        ckM = [P.sb("ckM%d" % i, [128, 4, 256], BF16) for i in range(2)]; b_ckM = bufs("ckM", 2)
        pe_ = [P.sb("pe%d" % i, [128, 512], BF16) for i in range(2)]; b_pe = bufs("pe", 2)
        pm_ = [P.sb("pmk%d" % i, [128, 4, 128], BF16) for i in range(2)]; b_pm = bufs("pmm", 2)
        rs_t = P.sb("rs_t", [128, 512], F32); rs = rs_t[:, :]; b_rs = Buf("rs_t")
        rlB = [P.sb("rlB%d" % i, [128, 512], F32) for i in range(2)]; b_rlB = bufs("rlB", 2)
        olT = wuk[:, :, :].rearrange("p a (h b) -> p a h b", h=8); b_olT = b_wuk
        oT = qT; b_oT = b_qT
        yb = xt; b_yb = b_xt
        psS = P.ps("ps3S", [128, 512], F32); b_psS = Buf("ps3S")
        psT = P.ps("ps3T", [128, 8, 128], BF16); b_psT = Buf("ps3T")
        psL = [P.ps("ps3L%d" % i, [128, 512], F32) for i in range(2)]; b_psL = bufs("ps3L", 2)
        psO = [P.ps("ps3O%d" % i, [128, 512], F32) for i in range(2)]; b_psO = bufs("ps3O", 2)
        psM = P.ps("ps3M", [128, 512], F32); b_psM = Buf("ps3M")
        psX = P.ps("ps3X", [128, 512], F32); b_psX = Buf("ps3X")
        psQ = [psS, psX]; b_psQ = [b_psS, b_psX]

        def mm(out, lhsT, rhs, start, stop, rd, wr):
            P.op("pe", lambda e: e.matmul(out, lhsT=lhsT, rhs=rhs, start=start, stop=stop), reads=rd, writes=wr)

        def tr(out, in_, idn, rd, wr):
            P.op("pe", lambda e: e.transpose(out=out, in_=in_, identity=idn), reads=rd, writes=wr)

        for h in range(8):
            for cc in range(2):
                tr(psT[:, cc, :], wuk[:, cc, h * 128:(h + 1) * 128], ident[:], [b_wuk, b_ident], [b_psT])
            P.op("act", lambda e, h=h: e.copy(out=wukT[:, h, :].rearrange("p (a b) -> p a b", a=2), in_=psT[:, 0:2, :]),
                 reads=[b_psT], writes=[b_wukT])

        def A1(j):
            NK = 4 * j + 4
            NKC = j + 1
            qaT, b_qaT = qaTs[j % 2], b_qaTs[j % 2]
            P.dma("sp", lambda e, j=j: e.dma_start(out=xt[:], in_=dr["xo"][j * 128:(j + 1) * 128, :]), writes=[b_xt])
            normmod(xt[:], b_xt, hb[:], b_hb, sq[:], b_sq, stt, b_stt, 1, 0)
            for k in range(8):
                tr(psT[:, k, :], hb[:, k * 128:(k + 1) * 128], ident[:], [b_hb, b_ident], [b_psT])
            P.op("act", lambda e: e.copy(out=hT[:], in_=psT[:]), reads=[b_psT], writes=[b_hT])
            for k in range(8):
                mm(psS[:, 0:264], hT[:, k, :], wql[:, k, :], k == 0, k == 7, [b_hT, b_wql], [b_psS])
            P.op("act", lambda e: e.copy(out=ql[:], in_=psS[:, 0:264]), reads=[b_psS], writes=[b_ql])
            P.op("pool", lambda e: e.memset(stt[:, 4:5], 0.0), writes=[b_stt])
            P.op("act", lambda e: e.activation(out=sq[:, 0:256], in_=ql[:, 0:256], func=AF.Square, accum_out=stt[:, 4:5]),
                 reads=[b_ql, b_stt], writes=[b_sq, b_stt])
            P.op("act", lambda e: e.activation(out=stt[:, 5:6], in_=stt[:, 4:5], func=AF.Ln, scale=1.0 / 256, bias=EPS),
                 reads=[b_stt], writes=[b_stt])
            P.op("act", lambda e: e.activation(out=stt[:, 6:7], in_=stt[:, 5:6], func=AF.Exp, scale=-0.5), reads=[b_stt], writes=[b_stt])
            P.op("dve", lambda e: e.scalar_tensor_tensor(out=qln[:], in0=ql[:, 0:256], scalar=stt[:, 6:7], in1=qnrow[:],
                                                          op0=ALU.mult, op1=ALU.mult), reads=[b_ql, b_stt, b_qnrow], writes=[b_qln])
            P.op("dve", lambda e: e.tensor_scalar(out=wi[:], in0=ql[:, 256:264], scalar1=(8.0 ** -0.5) * (64.0 ** -0.5), scalar2=None,
                                                   op0=ALU.mult), reads=[b_ql], writes=[b_wi])
            for cc in range(2):
                tr(psT[:, cc, :], qln[:, cc * 128:(cc + 1) * 128], ident[:], [b_qln, b_ident], [b_psT])
            P.op("act", lambda e: e.copy(out=qlT[:], in_=psT[:, 0:2, :]), reads=[b_psT], writes=[b_qlT])
            for h in range(8):
                po, bpo = psQ[h // 4], b_psQ[h // 4]
                for cc in range(2):
                    mm(po[:, (h % 4) * 128:(h % 4 + 1) * 128], wuq[:, cc, h * 128:(h + 1) * 128], qlT[:, cc, :], cc == 0, cc == 1,
                       [b_wuq, b_qlT], [bpo])
            for hh in range(2):
                P.op("act", lambda e, hh=hh: e.activation(out=qT[:, 4 * hh:4 * hh + 4, :].rearrange("p a b -> p (a b)"), in_=psQ[hh][:],
                                                          func=AF.Copy, scale=128.0 ** -0.5), reads=[b_psQ[hh]], writes=[b_qT])
            for cc in range(2):
                for h in range(8):
                    po, bpo = psQ[h // 4], b_psQ[h // 4]
                    mm(po[:, (h % 4) * 128:(h % 4 + 1) * 128], wukT[:, h, cc * 128:(cc + 1) * 128], qT[:, h, :], True, True,
                       [b_wukT, b_qT], [bpo])
                for hh in range(2):
                    P.op("act", lambda e, hh=hh, cc=cc: e.copy(out=qaT[:, cc, 4 * hh:4 * hh + 4, :].rearrange("p a b -> p (a b)"),
                                                               in_=psQ[hh][:]), reads=[b_psQ[hh]], writes=[b_qaT])
            for h in range(8):
                po, bpo = psQ[h // 4], b_psQ[h // 4]
                for cc in range(2):
                    mm(po[0:64, (h % 4) * 128:(h % 4 + 1) * 128], wiq[:, cc, h * 64:(h + 1) * 64], qlT[:, cc, :], cc == 0, cc == 1,
                       [b_wiq, b_qlT], [bpo])
            for hh in range(2):
                P.op("act", lambda e, hh=hh: e.copy(out=qiT[:, 4 * hh:4 * hh + 4, :].rearrange("p a b -> p (a b)"), in_=psQ[hh][0:64, :]),
                     reads=[b_psQ[hh]], writes=[b_qiT])
            n = 0
            for kc in range(NKC):
                sl = score[:, kc * 512:(kc + 1) * 512]
                for h in range(8):
                    p = n % 2; n += 1
                    mm(psQ[p][:], qiT[:, h, :], kidxT[:, kc * 512:(kc + 1) * 512], True, True, [b_qiT, b_kidxT], [b_psQ[p]])
                    P.op("act", lambda e, p=p: e.activation(out=rl[p][:], in_=psQ[p][:], func=AF.Relu), reads=[b_psQ[p]], writes=[b_rl[p]])
                    if h == 0:
                        P.op("dve", lambda e, p=p, sl=sl: e.tensor_scalar(out=sl, in0=rl[p][:], scalar1=wi[:, 0:1], scalar2=None, op0=ALU.mult),
                             reads=[b_rl[p], b_wi], writes=[b_score])
                    else:
                        P.op("dve", lambda e, p=p, sl=sl, h=h: e.scalar_tensor_tensor(out=sl, in0=rl[p][:], scalar=wi[:, h:h + 1], in1=sl,
                                                                                   op0=ALU.mult, op1=ALU.add),
                             reads=[b_rl[p], b_wi, b_score], writes=[b_score])
                if kc == NKC - 1:
                    P.op("dve", lambda e, NK=NK: e.reduce_max(out=bs[:, 0:1], in_=score[:, 0:NK * 128], axis=AX.X, apply_absolute_value=True),
                         reads=[b_score, b_bs], writes=[b_bs])
                    P.op("dve", lambda e, sl=sl: e.tensor_tensor(out=sl, in0=sl, in1=cmask[:], op=ALU.add),
                         reads=[b_score, b_cmask], writes=[b_score])
        def A2(j):
            NK = 4 * j + 4
            W = NK * 128
            P.op("dve", lambda e: e.tensor_scalar(out=bs[:, 1:2], in0=bs[:, 0:1], scalar1=2.0, scalar2=2.0, op0=ALU.mult, op1=ALU.add),
                 reads=[b_bs], writes=[b_bs])
            P.op("dve", lambda e: e.tensor_scalar(out=bs[:, 2:3], in0=bs[:, 0:1], scalar1=-1.0, scalar2=-1.0, op0=ALU.mult, op1=ALU.add),
                 reads=[b_bs], writes=[b_bs])
            P.op("dve", lambda e: e.tensor_scalar(out=steps[:, 0, :], in0=pw[:], scalar1=bs[:, 1:2], scalar2=None, op0=ALU.mult),
                 reads=[b_pw, b_bs], writes=[b_steps])
            P.op("dve", lambda e: e.memset(cnt[:], 0.0), reads=[b_cnt], writes=[b_cnt])
            P.op("dve", lambda e: e.tensor_tensor(out=bs[:, 3:4], in0=bs[:, 2:3], in1=steps[:, 0, 0:1], op=ALU.add),
                 reads=[b_bs, b_steps], writes=[b_bs])
            for k in range(KIT):
                P.op("dve", lambda e, k=k, W=W: e.tensor_scalar(out=mk[:, 0:W], in0=score[:, 0:W], scalar1=bs[:, 3:4], scalar2=0.0,
                                                               op0=ALU.is_gt, op1=ALU.add, accum_out=cnt[:, k:k + 1]),
                     reads=[b_score, b_bs, b_cnt], writes=[b_wk, b_cnt])
                P.op("dve", lambda e, k=k: e.tensor_scalar(out=bs[:, 5:6], in0=cnt[:, k:k + 1], scalar1=255.5, scalar2=None, op0=ALU.is_gt),
                     reads=[b_cnt, b_bs], writes=[b_bs])
                P.op("dve", lambda e, k=k: e.scalar_tensor_tensor(out=bs[:, 2:3], in0=bs[:, 5:6], scalar=steps[:, 0, k:k + 1], in1=bs[:, 2:3],
                                                                  op0=ALU.mult, op1=ALU.add), reads=[b_bs, b_steps], writes=[b_bs])
                if k + 1 < KIT:
                    P.op("dve", lambda e, k=k: e.tensor_tensor(out=bs[:, 3:4], in0=bs[:, 2:3], in1=steps[:, 0, k + 1:k + 2], op=ALU.add),
                         reads=[b_bs, b_steps], writes=[b_bs])
        def A3(j):
            NK = 4 * j + 4
            W = NK * 128
            P.op("dve", lambda e, W=W: e.tensor_scalar(out=mk[:, 0:W], in0=score[:, 0:W], scalar1=bs[:, 2:3], scalar2=None, op0=ALU.is_gt),
                 reads=[b_score, b_bs], writes=[b_wk])
            for kb0 in range(0, NK, 8):
                nb = min(8, NK - kb0)
                for i in range(nb):
                    tr(psT[:, i, :], mk[:, (kb0 + i) * 128:(kb0 + i + 1) * 128], ident[:], [b_wk, b_ident], [b_psT])
                P.op("act", lambda e, kb0=kb0, nb=nb: e.activation(out=maskT[:, kb0:kb0 + nb, :], in_=psT[:, 0:nb, :], func=AF.Identity,
                                                                  scale=30000.0, bias=-30000.0), reads=[b_psT], writes=[b_maskT])
        def B(j):
            NK = 4 * j + 4
            NKC = j + 1
            qaT, b_qaT = qaTs[j % 2], b_qaTs[j % 2]
            ld = 0
            for hh in range(2):
                for kc in range(NKC):
                    s_ = ld % 2; ld += 1
                    P.dma("sp", lambda e, s_=s_, kc=kc: e.dma_start(
                        out=ckT[s_][:], in_=ckvT_d[:, kc * 512:(kc + 1) * 512].rearrange("(a p) t -> p a t", p=128)), writes=[b_ckT[s_]])
                    P.dma("sp", lambda e, s_=s_, kc=kc: e.dma_start(
                        out=ckM[s_][:], in_=ckv_tm_d[kc * 512:(kc + 1) * 512, :].rearrange("(a p) c -> p a c", p=128)), writes=[b_ckM[s_]])
                    for i in range(4):
                        kb = kc * 4 + i
                        p = kb % 2
                        for cc in range(2):
                            mm(psL[p][:], ckT[s_][:, cc, i * 128:(i + 1) * 128], qaT[:, cc, 4 * hh:4 * hh + 4, :].rearrange("p a b -> p (a b)"),
                               cc == 0, False, [b_ckT[s_], b_qaT], [b_psL[p]])
                        mm(psL[p][:].rearrange("p (a b) -> p a b", a=4), ident[:], maskT[:, kb, :].unsqueeze(1).to_broadcast([128, 4, 128]),
                           False, True, [b_ident, b_maskT], [b_psL[p]])
                        P.op("act", lambda e, p=p: e.activation(out=pm_[p][:].rearrange("p a b -> p (a b)"), in_=psL[p][:], func=AF.Exp),
                             reads=[b_psL[p]], writes=[b_pm[p]])
                        pmf = pm_[p][:].rearrange("p a b -> p (a b)")
                        for cc in range(2):
                            mm(psO[cc][:], ckM[s_][:, i, cc * 128:(cc + 1) * 128], pmf, kb == 0, kb == NK - 1, [b_ckM[s_], b_pm[p]], [b_psO[cc]])
                        mm(psM[:], onesb[:], pmf, kb == 0, kb == NK - 1, [b_onesb, b_pm[p]], [b_psM])
                P.op("act", lambda e: e.activation(out=rs, in_=psM[:], func=AF.Ln), reads=[b_psM], writes=[b_rs])
                P.op("act", lambda e: e.activation(out=rs, in_=rs, func=AF.Exp, scale=-1.0), reads=[b_rs], writes=[b_rs])
                for cc in range(2):
                    P.op("act", lambda e, cc=cc: e.copy(out=rlB[cc][:], in_=psO[cc][:]), reads=[b_psO[cc]], writes=[b_rlB[cc]])
                    P.op("pool", lambda e, cc=cc, hh=hh: e.tensor_tensor(out=olT[:, cc, 4 * hh:4 * hh + 4, :].rearrange("p a b -> p (a b)"),
                                                                      in0=rlB[cc][:], in1=rs, op=ALU.mult),
                         reads=[b_rlB[cc], b_rs], writes=[b_olT])
            for h in range(8):
                po, bpo = psO[h // 4], b_psO[h // 4]
                for cc in range(2):
                    mm(po[:, (h % 4) * 128:(h % 4 + 1) * 128], wuv[:, cc, h * 128:(h + 1) * 128], olT[:, cc, h, :], cc == 0, cc == 1,
                       [b_wuv, b_olT], [bpo])
            for hh in range(2):
                P.op("act", lambda e, hh=hh: e.copy(out=oT[:, 4 * hh:4 * hh + 4, :].rearrange("p a b -> p (a b)"), in_=psO[hh][:]),
                     reads=[b_psO[hh]], writes=[b_oT])
            for half in range(2):
                P.dma("sp", lambda e, half=half: e.dma_start(out=wbs[:], in_=wb_d[14 + half]), reads=[b_wbd], writes=[b_wbs])
                for h in range(8):
                    mm(psL[half][:], oT[:, h, :], wbs[:, h, :], h == 0, h == 7, [b_oT, b_wbs], [b_psL[half]])
                P.op("act", lambda e, half=half: e.copy(out=yb[:, half * 512:(half + 1) * 512], in_=psL[half][:]),
                     reads=[b_psL[half]], writes=[b_yb])
            P.dma("sp", lambda e, j=j: e.dma_start(out=yb_d[j * 128:(j + 1) * 128, :], in_=yb[:]), reads=[b_yb], track=b_yb,
                  is_out=("dbg_yb" in DEBUG))
        def merge(la, lb):
            na, nb = len(la), len(lb)
            ia = ib = 0
            while ia < na or ib < nb:
                if ib >= nb or (ia < na and ia * nb <= ib * na):
                    P.replay(la[ia:ia + 1]); ia += 1
                else:
                    P.replay(lb[ib:ib + 1]); ib += 1

        A1(0); A2(0); A3(0)
        for j in range(NOWN):
            if j + 1 < NOWN:
                merge(P.record(lambda: (A1(j + 1), A2(j + 1))), P.record(lambda: B(j)))
                A3(j + 1)
            else:
                B(j)
        P.flush()
    P.stack = gst


def phase3b(P, nc, dr, G):
    gst = P.stack
    ident, b_ident, onesf, b_onesf = G["ident"], G["b_ident"], G["onesf"], G["b_onesf"]
    normmod = G["normmod"]
    oown_d, yb_d, out_d, uv_d = G["oown_d"], G["yb_d"], G["out_d"], G["uv_d"]
    with ExitStack() as st:
        P.stack = st
        modrow = P.sb("modrow_4", [128, 6, D], F32); b_mod = Buf("modrow_4")
        P.dma("sp", lambda e: e.dma_start(out=modrow[:].rearrange("p a d -> p (a d)"), in_=G["mod_d"][0:6 * D].partition_broadcast(128)), writes=[b_mod])
        G["MR"]["t"], G["MR"]["b"] = modrow, b_mod
        win_v = dr["w_in"].rearrange("(k p) n -> p k n", p=128)
        wb_d = G["wb_d"]
        widx = {"z": 0, "ga": 2, "gb": 4, "ao": 6, "wo": 8, "pq": 10}
        wst = [P.sb("wst%d" % i, [128, 8, 512], BF16) for i in range(3)]; b_wst = bufs("wst", 3)
        wctr = [0]

        def wload(name, half):
            s_ = wctr[0] % 3; wctr[0] += 1
            ci = widx[name] + half
            P.dma("sp", lambda e: e.dma_start(out=wst[s_][:], in_=wb_d[ci]), writes=[b_wst[s_]])
            return wst[s_], b_wst[s_]

        skn = P.sb("skn", [128, 16, 128], BF16); b_skn = Buf("skn")
        P.dma("pool", lambda e: e.dma_start(out=skn[:], in_=dr["peer_sub_keys"].rearrange("a n d -> n a d")), writes=[b_skn])
        skT = P.sb("skT", [128, 16, 128], BF16); b_skT = Buf("skT")
        rows = P.sb("rows3", [128, 128 + D], F32); b_rows = Buf("rows3")
        P.dma("sp", lambda e: e.dma_start(out=rows[:, 0:128], in_=dr["dn_onorm_w"].partition_broadcast(128)), writes=[b_rows])
        P.dma("sp", lambda e: e.dma_start(out=rows[:, 128:128 + D], in_=dr["final_norm_w"].partition_broadcast(128)), writes=[b_rows])
        iota = P.sb("iota", [128, 16], F32); b_iota = Buf("iota")
        thr16 = P.sb("thr16", [128, 16], F32); b_thr16 = Buf("thr16")
        for i in range(16):
            P.op("pool", lambda e, i=i: e.memset(iota[:, i:i + 1], float(i)), reads=[b_iota], writes=[b_iota])
            P.op("pool", lambda e, i=i: e.memset(thr16[:, i:i + 1], float(16 * (i + 1)) if i < 15 else 1.0e9), reads=[b_thr16], writes=[b_thr16])

        xt = P.sb("xt4", [128, D], F32); b_xt = Buf("xt4")
        sq = P.sb("sq4", [128, D], F32); b_sq = Buf("sq4")
        stt = P.sb("stt4", [128, 24], F32); b_stt = Buf("stt4")
        hb = P.sb("hb4", [128, D], BF16); b_hb = Buf("hb4")
        hT = P.sb("hT4", [128, 8, 128], BF16); b_hT = Buf("hT4")
        zs = P.sb("zs", [128, D], F32); b_zs = Buf("zs")
        ot = P.sb("ot", [128, D], F32); b_ot = Buf("ot")
        og = P.sb("og", [128, D], BF16); b_og = Buf("og")
        ogT = P.sb("ogT", [128, 8, 128], BF16); b_ogT = Buf("ogT")
        sg = P.sb("sg", [128, 512], F32); b_sg = Buf("sg")
        ybt = P.sb("ybt", [128, D], F32); b_ybt = Buf("ybt")
        mt = P.sb("mt", [128, D], F32); b_mt = Buf("mt")
        mb = P.sb("mb", [128, D], BF16); b_mb = Buf("mb")
        x1s = [P.sb("x1_%d" % i, [128, D], F32) for i in range(3)]; b_x1s = bufs("x1_", 3)
        h2f = P.sb("h2f", [128, D], F32); b_h2f = Buf("h2f")
        qpT = P.sb("qpT", [128, 16, 128], BF16); b_qpT = Buf("qpT")
        scss = [P.sb("scs%d" % i, [128, 16, 128], F32) for i in range(2)]; b_scss = bufs("scs", 2)
        scw = P.sb("scw", [128, 256], F32); b_scw = Buf("scw")
        sv = P.sb("sv", [128, 16, 16], F32); b_sv = Buf("sv")
        si = P.sb("si", [128, 16, 16], U32); b_si = Buf("si")
        sif = P.sb("sif", [128, 16, 16], F32); b_sif = Buf("sif")
        cand = P.sb("cand", [128, 8, 256], F32); b_cand = Buf("cand")
        cv = P.sb("cv", [128, 8, 16], F32); b_cv = Buf("cv")
        cp = P.sb("cp", [128, 8, 16], U32); b_cp = Buf("cp")
        ca = P.sb("ca", [128, 2, 8, 16], U32); b_ca = Buf("ca")
        caf = P.sb("caf", [128, 2, 8, 16], F32); b_caf = Buf("caf")
        oh = P.sb("oh", [128, 16, 16], F32); b_oh = Buf("oh")
        ij = P.sb("ij", [128, 2, 8, 16], F32); b_ij = Buf("ij")
        eidf = P.sb("eidf", [128, 128], F32); b_eidf = Buf("eidf")
        eids = [P.sb("eid%d" % i, [128, 128], I32) for i in range(2)]; b_eids = bufs("eid", 2)
        gtss = [P.sb("gts%d" % i, [128, 8, 16], F32) for i in range(2)]; b_gtss = bufs("gts", 2)
        act = P.sb("actp", [128, 128], F32); b_act = Buf("actp")
        coef = P.sb("coef", [128, 128], F32); b_coef = Buf("coef")
        NUG = 6
        ug = [P.sb("ug%d" % i, [128, 2 * D], BF16) for i in range(NUG)]; b_ug = bufs("ug", NUG)
        dg = [P.sb("dg%d" % i, [128, 128], BF16) for i in range(4)]; b_dg = bufs("dg", 4)
        jb = P.sb("jb", [128, D], BF16); b_jb = Buf("jb")
        h2bs = [P.sb("h2b%d" % i, [128, D], BF16) for i in range(3)]; b_h2bs = bufs("h2b", 3)
        sq_t = P.sb("sq_t", [128, D], F32); b_sq_t = Buf("sq_t")
        stts = P.sb("stts", [128, 24], F32); b_stts = Buf("stts")
        stt_t = P.sb("stt_t", [128, 4], F32); b_stt_t = Buf("stt_t")
        mt_t = P.sb("mt_t", [128, D], F32); b_mt_t = Buf("mt_t")
        acc = P.sb("acc", [128, D], F32); b_acc = Buf("acc")
        psA = [P.ps("ps4A%d" % i, [128, 512], F32) for i in range(4)]; b_psA = bufs("ps4A", 4)
        psB = [P.ps("ps4B%d" % i, [128, 512], F32) for i in range(2)]; b_psB = bufs("ps4B", 2)
        psT = P.ps("ps4T", [128, 8, 128], BF16); b_psT = Buf("ps4T")
        psS = P.ps("ps4S", [128, 512], F32); b_psS = Buf("ps4S")

        def mm(out, lhsT, rhs, start, stop, rd, wr):
            P.op("pe", lambda e: e.matmul(out, lhsT=lhsT, rhs=rhs, start=start, stop=stop), reads=rd, writes=wr)

        def tr(out, in_, idn, rd, wr):
            P.op("pe", lambda e: e.transpose(out=out, in_=in_, identity=idn), reads=rd, writes=wr)

        def transpose8(src, bsrc, dst, bdst):
            for k in range(8):
                tr(psT[:, k, :], src[:, k * 128:(k + 1) * 128], ident[:], [bsrc, b_ident], [b_psT])
            P.op("act", lambda e: e.copy(out=dst[:], in_=psT[:]), reads=[b_psT], writes=[bdst])

        for a in range(0, 16, 8):
            for i in range(8):
                tr(psT[:, i, :], skn[:, a + i, :], ident[:], [b_skn, b_ident], [b_psT])
            P.op("act", lambda e, a=a: e.copy(out=skT[:, a:a + 8, :], in_=psT[:]), reads=[b_psT], writes=[b_skT])

        def top16(src_ap, bsrc, vals, idxs, work, extra_w):
            P.op("dve", lambda e: e.max(out=vals[:, 0:8], in_=src_ap), reads=[bsrc] + extra_w, writes=extra_w)
            P.op("dve", lambda e: e.max_index(out=idxs[:, 0:8], in_max=vals[:, 0:8], in_values=src_ap), reads=[bsrc] + extra_w, writes=extra_w)
            P.op("dve", lambda e: e.match_replace(out=work, in_to_replace=vals[:, 0:8], in_values=src_ap, imm_value=-3.0e38),
                 reads=[bsrc] + extra_w, writes=[b_scw])
            P.op("dve", lambda e: e.max(out=vals[:, 8:16], in_=work), reads=[b_scw] + extra_w, writes=extra_w)
            P.op("dve", lambda e: e.max_index(out=idxs[:, 8:16], in_max=vals[:, 8:16], in_values=work), reads=[b_scw] + extra_w, writes=extra_w)

        def mixer(j):
            x1, b_x1, h2b, b_h2b = x1s[j % 3], b_x1s[j % 3], h2bs[j % 3], b_h2bs[j % 3]
            scs, b_scs = scss[j % 2], b_scss[j % 2]
            P.dma("sp", lambda e, j=j: e.dma_start(out=xt[:], in_=dr["xo"][j * 128:(j + 1) * 128, :]), writes=[b_xt])
            P.dma("sp", lambda e, j=j: e.dma_start(out=ot[:], in_=oown_d[j * 128:(j + 1) * 128, :]), writes=[b_ot])
            P.dma("sp", lambda e, j=j: e.dma_start(out=ybt[:], in_=yb_d[j * 128:(j + 1) * 128, :]), writes=[b_ybt])
            normmod(xt[:], b_xt, hb[:], b_hb, sq[:], b_sq, stt, b_stt, 1, 0)
            transpose8(hb, b_hb, hT, b_hT)
            for half in range(2):
                w_, bw_ = wload("z", half)
                for k in range(8):
                    mm(psA[half][:], hT[:, k, :], w_[:, k, :], k == 0, k == 7, [b_hT, bw_], [b_psA[half]])
                P.op("act", lambda e, half=half: e.activation(out=zs[:, half * 512:(half + 1) * 512], in_=psA[half][:], func=AF.Silu),
                     reads=[b_psA[half]], writes=[b_zs])
            P.op("act", lambda e: e.activation(out=sq[:], in_=ot[:], func=AF.Square), reads=[b_ot], writes=[b_sq])
            P.op("dve", lambda e: e.reduce_sum(out=stt[:, 8:16], in_=sq[:, :].rearrange("p (a b) -> p a b", a=8), axis=AX.X),
                 reads=[b_sq, b_stt], writes=[b_stt])
            P.op("act", lambda e: e.activation(out=stt[:, 8:16], in_=stt[:, 8:16], func=AF.Sqrt, scale=1.0 / 128, bias=EPS),
                 reads=[b_stt], writes=[b_stt])
            P.op("dve", lambda e: e.reciprocal(out=stt[:, 16:24], in_=stt[:, 8:16]), reads=[b_stt], writes=[b_stt])
            o3 = ot[:, :].rearrange("p (a b) -> p a b", a=8)
            P.op("dve", lambda e: e.tensor_tensor(out=o3, in0=o3, in1=stt[:, 16:24].unsqueeze(2).to_broadcast([128, 8, 128]), op=ALU.mult),
                 reads=[b_ot, b_stt], writes=[b_ot])
            P.op("dve", lambda e: e.tensor_tensor(out=o3, in0=o3, in1=rows[:, 0:128].unsqueeze(1).to_broadcast([128, 8, 128]), op=ALU.mult),
                 reads=[b_ot, b_rows], writes=[b_ot])
            P.op("dve", lambda e: e.tensor_tensor(out=og[:], in0=ot[:], in1=zs[:], op=ALU.mult), reads=[b_ot, b_zs], writes=[b_og])
            transpose8(og, b_og, ogT, b_ogT)
            for half in range(2):
                hs_ = slice(half * 512, (half + 1) * 512)
                w_, bw_ = wload("ao", half)
                for k in range(8):
                    mm(psA[0][:], ogT[:, k, :], w_[:, k, :], k == 0, k == 7, [b_ogT, bw_], [b_psA[0]])
                w_, bw_ = wload("ga", half)
                for k in range(8):
                    mm(psA[1][:], hT[:, k, :], w_[:, k, :], k == 0, k == 7, [b_hT, bw_], [b_psA[1]])
                P.op("act", lambda e: e.activation(out=sg[:], in_=psA[1][:], func=AF.Sigmoid), reads=[b_psA[1]], writes=[b_sg])
                P.op("dve", lambda e, hs_=hs_: e.tensor_tensor(out=mt[:, hs_], in0=psA[0][:], in1=sg[:], op=ALU.mult),
                     reads=[b_psA[0], b_sg], writes=[b_mt])
                w_, bw_ = wload("gb", half)
                for k in range(8):
                    mm(psA[2][:], hT[:, k, :], w_[:, k, :], k == 0, k == 7, [b_hT, bw_], [b_psA[2]])
                P.op("act", lambda e: e.activation(out=sg[:], in_=psA[2][:], func=AF.Sigmoid), reads=[b_psA[2]], writes=[b_sg])
                P.op("dve", lambda e, hs_=hs_: e.tensor_tensor(out=ybt[:, hs_], in0=ybt[:, hs_], in1=sg[:], op=ALU.mult),
                     reads=[b_ybt, b_sg], writes=[b_ybt])
                P.op("dve", lambda e, hs_=hs_: e.tensor_tensor(out=mb[:, hs_], in0=mt[:, hs_], in1=ybt[:, hs_], op=ALU.add),
                     reads=[b_mt, b_ybt], writes=[b_mb])
            transpose8(mb, b_mb, ogT, b_ogT)
            for half in range(2):
                hs_ = slice(half * 512, (half + 1) * 512)
                w_, bw_ = wload("wo", half)
                for k in range(8):
                    mm(psA[half][:], ogT[:, k, :], w_[:, k, :], k == 0, k == 7, [b_ogT, bw_], [b_psA[half]])
                P.op("dve", lambda e, half=half, hs_=hs_: e.tensor_tensor(out=mt[:, hs_], in0=psA[half][:], in1=modrow[:, 2, hs_], op=ALU.mult),
                     reads=[b_psA[half], b_mod], writes=[b_mt])
                P.op("dve", lambda e, hs_=hs_: e.tensor_tensor(out=x1[:, hs_], in0=mt[:, hs_], in1=xt[:, hs_], op=ALU.add),
                     reads=[b_mt, b_xt], writes=[b_x1])
            normmod(x1[:], b_x1, h2f[:], b_h2f, sq[:], b_sq, stt, b_stt, 4, 3)
            P.op("act", lambda e: e.copy(out=hb[:], in_=h2f[:]), reads=[b_h2f], writes=[b_hb])
            P.op("act", lambda e: e.copy(out=h2b[:], in_=h2f[:]), reads=[b_h2f], writes=[b_h2b])
            transpose8(hb, b_hb, hT, b_hT)
            for q4 in range(4):
                w_, bw_ = wload("pq", q4)
                for i in range(4):
                    for k in range(8):
                        mm(psA[i][:, 0:128], w_[:, k, i * 128:(i + 1) * 128], hT[:, k, :], k == 0, k == 7, [b_hT, bw_], [b_psA[i]])
                    P.op("act", lambda e, i=i, q4=q4: e.copy(out=qpT[:, 4 * q4 + i, :], in_=psA[i][:, 0:128]), reads=[b_psA[i]], writes=[b_qpT])
            for q4 in range(4):
                for i in range(4):
                    hp = 4 * q4 + i
                    mm(psA[q4][:, i * 128:(i + 1) * 128], qpT[:, hp, :], skT[:, hp, :], True, True, [b_qpT, b_skT], [b_psA[q4]])
                P.op("act", lambda e, q4=q4: e.copy(out=scs[:, 4 * q4:4 * q4 + 4, :].rearrange("p a b -> p (a b)"), in_=psA[q4][:]),
                     reads=[b_psA[q4]], writes=[b_scs])

        def select(j):
            par = j % 2
            eid, b_eid, gts, b_gts = eids[par], b_eids[par], gtss[par], b_gtss[par]
            scs, b_scs = scss[par], b_scss[par]
            for hp in range(16):
                top16(scs[:, hp, :], b_scs, sv[:, hp, :], si[:, hp, :], scw[:, 0:128], [b_sv, b_si])
            P.op("dve", lambda e: e.tensor_copy(out=sif[:], in_=si[:]), reads=[b_si], writes=[b_sif])
            for h in range(8):
                P.op("dve", lambda e, h=h: e.tensor_tensor(
                    out=cand[:, h, :].rearrange("p (a b) -> p a b", a=16),
                    in0=sv[:, 2 * h, :].unsqueeze(2).to_broadcast([128, 16, 16]),
                    in1=sv[:, 2 * h + 1, :].unsqueeze(1).to_broadcast([128, 16, 16]), op=ALU.add),
                    reads=[b_sv, b_cand], writes=[b_cand])
            for h in range(8):
                top16(cand[:, h, :], b_cand, cv[:, h, :], cp[:, h, :], scw[:, 0:256], [b_cv, b_cp])
            P.op("dve", lambda e: e.tensor_scalar(out=stts[:, 8:16], in0=cv[:, :, 0], scalar1=-1.0, scalar2=None, op0=ALU.mult),
                 reads=[b_cv, b_stts], writes=[b_stts])
            P.op("pool", lambda e: e.memset(stts[:, 16:24], 0.0), reads=[b_stts], writes=[b_stts])
            for h in range(8):
                P.op("act", lambda e, h=h: e.activation(out=gts[:, h, :], in_=cv[:, h, :], func=AF.Exp, bias=stts[:, 8 + h:9 + h],
                                                        accum_out=stts[:, 16 + h:17 + h]), reads=[b_cv, b_stts, b_gts], writes=[b_gts, b_stts])
            P.op("dve", lambda e: e.reciprocal(out=stts[:, 16:24], in_=stts[:, 16:24]), reads=[b_stts], writes=[b_stts])
            P.op("dve", lambda e: e.tensor_tensor(out=gts[:], in0=gts[:], in1=stts[:, 16:24].unsqueeze(2).to_broadcast([128, 8, 16]), op=ALU.mult),
                 reads=[b_gts, b_stts], writes=[b_gts])
            P.op("dve", lambda e: e.tensor_copy(out=caf[:, 0, :, :], in_=cp[:]), reads=[b_cp, b_caf], writes=[b_caf])
            tmp3 = cand[:, :, :].rearrange("p h (r k) -> p (h r) k", k=16)
            P.op("dve", lambda e: e.tensor_tensor(out=tmp3, in0=caf[:, 0, :, :].rearrange("p a b -> p (a b)").unsqueeze(2).to_broadcast([128, 128, 16]),
                                                  in1=thr16[:].unsqueeze(1).to_broadcast([128, 128, 16]), op=ALU.is_ge),
                 reads=[b_caf, b_thr16, b_cand], writes=[b_cand])
            P.op("dve", lambda e: e.reduce_sum(out=caf[:, 1, :, :].rearrange("p a b -> p (a b)"), in_=tmp3, axis=AX.X),
                 reads=[b_cand, b_caf], writes=[b_caf])
            P.op("dve", lambda e: e.scalar_tensor_tensor(out=caf[:, 0, :, :], in0=caf[:, 1, :, :], scalar=-16.0, in1=caf[:, 0, :, :],
                                                          op0=ALU.mult, op1=ALU.add), reads=[b_caf], writes=[b_caf])
            for t_ in range(2):
                for h in range(8):
                    P.op("dve", lambda e, t_=t_, h=h: e.tensor_tensor(
                        out=oh[:], in0=caf[:, 1 - t_, h, :].unsqueeze(2).to_broadcast([128, 16, 16]),
                        in1=iota[:].unsqueeze(1).to_broadcast([128, 16, 16]), op=ALU.is_equal), reads=[b_caf, b_iota, b_oh], writes=[b_oh])
                    P.op("dve", lambda e, t_=t_, h=h: e.tensor_tensor(
                        out=oh[:], in0=oh[:], in1=sif[:, 2 * h + t_, :].unsqueeze(1).to_broadcast([128, 16, 16]), op=ALU.mult),
                        reads=[b_oh, b_sif], writes=[b_oh])
                    P.op("dve", lambda e, t_=t_, h=h: e.reduce_sum(out=ij[:, t_, h, :], in_=oh[:], axis=AX.X), reads=[b_oh, b_ij], writes=[b_ij])
            P.op("dve", lambda e: e.scalar_tensor_tensor(out=eidf[:], in0=ij[:, 0, :, :].rearrange("p a b -> p (a b)"), scalar=128.0,
                                                          in1=ij[:, 1, :, :].rearrange("p a b -> p (a b)"), op0=ALU.mult, op1=ALU.add),
                 reads=[b_ij], writes=[b_eidf])
            P.op("dve", lambda e: e.tensor_copy(out=eid[:], in_=eidf[:]), reads=[b_eidf], writes=[b_eid])

        def slot(j, s):
            par = j % 2
            eid, b_eid, gts, b_gts, h2b, b_h2b = eids[par], b_eids[par], gtss[par], b_gtss[par], h2bs[j % 3], b_h2bs[j % 3]
            gflat = gts[:].rearrange("p a b -> p (a b)")
            if s == 0:
                P.op("pool", lambda e: e.memset(act[:], 0.0), writes=[b_act])
            if True:
                u_ = s % NUG
                d_ = s % 4
                P.dma("pool", lambda e, u_=u_, s=s: e.indirect_dma_start(
                    out=ug[u_][:], out_offset=None, in_=uv_d, in_offset=bass.IndirectOffsetOnAxis(ap=eid[:, s:s + 1], axis=0)),
                    reads=[b_eid], writes=[b_ug[u_]])
                P.op("dve", lambda e, u_=u_, s=s: e.scalar_tensor_tensor(out=jb[:], in0=ug[u_][:, 0:D], scalar=1.0, in1=h2b[:], op0=ALU.mult, op1=ALU.mult,
                                                                      accum_out=act[:, s:s + 1]), reads=[b_ug[u_], b_h2b, b_act, b_jb], writes=[b_jb, b_act])
                P.op("act", lambda e, s=s: e.activation(out=coef[:, s:s + 1], in_=act[:, s:s + 1], func=AF.Gelu), reads=[b_act, b_coef], writes=[b_coef])
                P.op("act", lambda e, s=s: e.activation(out=coef[:, s:s + 1], in_=coef[:, s:s + 1], func=AF.Copy, scale=gflat[:, s:s + 1]),
                     reads=[b_coef, b_gts], writes=[b_coef])
                P.op("act", lambda e, s=s, d_=d_: e.activation(out=dg[d_][:], in_=ident[:], func=AF.Copy, scale=coef[:, s:s + 1]),
                     reads=[b_coef, b_ident], writes=[b_dg[d_]])
                for half in range(2):
                    mm(psB[half][:], dg[d_][:], ug[u_][:, D + half * 512:D + (half + 1) * 512], s == 0, s == 127,
                       [b_dg[d_], b_ug[u_]], [b_psB[half]])

        def tail(j):
            x1, b_x1 = x1s[j % 3], b_x1s[j % 3]
            sq, b_sq, stt, b_stt, mt, b_mt = sq_t, b_sq_t, stt_t, b_stt_t, mt_t, b_mt_t
            for half in range(2):
                P.op("act", lambda e, half=half: e.copy(out=acc[:, half * 512:(half + 1) * 512], in_=psB[half][:]), reads=[b_psB[half]], writes=[b_acc])
            P.op("dve", lambda e: e.tensor_tensor(out=acc[:], in0=acc[:], in1=modrow[:, 5, :], op=ALU.mult), reads=[b_acc, b_mod], writes=[b_acc])
            P.op("dve", lambda e: e.tensor_tensor(out=acc[:], in0=acc[:], in1=x1[:], op=ALU.add), reads=[b_acc, b_x1], writes=[b_acc])
            P.op("pool", lambda e: e.memset(stt[:, 0:1], 0.0), reads=[b_stt], writes=[b_stt])
            P.op("act", lambda e: e.activation(out=sq[:], in_=acc[:], func=AF.Square, accum_out=stt[:, 0:1]), reads=[b_acc, b_stt], writes=[b_sq, b_stt])
            P.op("act", lambda e: e.activation(out=stt[:, 1:2], in_=stt[:, 0:1], func=AF.Sqrt, scale=1.0 / D, bias=EPS), reads=[b_stt], writes=[b_stt])
            P.op("dve", lambda e: e.reciprocal(out=stt[:, 2:3], in_=stt[:, 1:2]), reads=[b_stt], writes=[b_stt])
            P.op("dve", lambda e: e.scalar_tensor_tensor(out=mt[:], in0=acc[:], scalar=stt[:, 2:3], in1=rows[:, 128:128 + D], op0=ALU.mult, op1=ALU.mult),
                 reads=[b_acc, b_stt, b_rows], writes=[b_mt])
            P.dma("sp", lambda e, j=j: e.dma_start(out=out_d[j * 128:(j + 1) * 128, :], in_=mt[:]), reads=[b_mt], track=b_mt, is_out=True)

        mixer(0); select(0); mixer(1)
        for j in range(NOWN):
            la = P.record(lambda: select(j + 1)) if j + 1 < NOWN else []
            lb = P.record(lambda: mixer(j + 2)) if j + 2 < NOWN else []
            pa = (len(la) + 127) // 128
            pb = (len(lb) + 127) // 128
            for s_ in range(128):
                P.replay(P.record(lambda: slot(j, s_)))
                P.replay(la[s_ * pa:(s_ + 1) * pa])
                P.replay(lb[s_ * pb:(s_ + 1) * pb])
            P.replay(P.record(lambda: tail(j)))
        P.flush()
    P.stack = gst
```

```python
from contextlib import ExitStack
import numpy as np
import concourse.bass as bass
import concourse.mybir as mybir
from concourse.bass_utils import run_bass_kernel_spmd

F32 = mybir.dt.float32
BF16 = mybir.dt.bfloat16
I32 = mybir.dt.int32
U32 = mybir.dt.uint32
AF = mybir.ActivationFunctionType
ALU = mybir.AluOpType
AX = mybir.AxisListType
ENGS = ("pe", "act", "dve", "pool", "sp")
EPS = 1e-6
NEG = -30000.0

L = 8192
D = 1024
NCH = 64
NOWN = 16
DEBUG = {}


class Buf:
    __slots__ = ("name", "lw", "rd", "dsem", "dcnt")

    def __init__(self, name):
        self.name = name
        self.lw = None
        self.rd = {}
        self.dsem = None
        self.dcnt = 0


def rr(*gens):
    gens = list(gens)
    while gens:
        for g in list(gens):
            try:
                next(g)
            except StopIteration:
                gens.remove(g)


def bufs(name, n):
    return [Buf("%s%d" % (name, i)) for i in range(n)]


class Prog:
    def __init__(self, nc, stack):
        self.nc = nc
        self.gstack = stack
        self.stack = stack
        self.q = {e: [] for e in ENGS}
        self.cnt = {e: 0 for e in ENGS}
        self.sem = {e: stack.enter_context(nc.semaphore("s_" + e)) for e in ENGS}
        self.seen = {e: {} for e in ENGS}
        self.dtoks = {}
        self.out_tokens = []
        self.nid = 0
        self.rec = None

    def sb(self, name, shape, dt):
        return self.stack.enter_context(self.nc.sbuf_tensor(name, list(shape), dt))

    def ps(self, name, shape, dt):
        return self.stack.enter_context(self.nc.psum_tensor(name, list(shape), dt))

    def newsem(self, name):
        self.nid += 1
        return self.gstack.enter_context(self.nc.semaphore("%s_%d" % (name, self.nid)))

    def _need(self, eng, tok, waits):
        if tok is None:
            return
        sem, val, weng = tok
        if weng == "pe" and eng == "pe":
            return
        key = id(sem)
        if self.seen[eng].get(key, 0) >= val:
            return
        if waits.get(key, (None, 0))[1] < val:
            waits[key] = (sem, val)

    def _deps(self, eng, reads, writes):
        waits = {}
        for b in reads:
            self._need(eng, b.lw, waits)
        for b in writes:
            self._need(eng, b.lw, waits)
            for r in b.rd.values():
                self._need(eng, r, waits)
        for key, (sem, val) in waits.items():
            self.seen[eng][key] = val
        return list(waits.values())

    def _commit(self, tok, reads, writes):
        k = id(tok[0])
        for b in reads:
            o = b.rd.get(k)
            if o is None or o[1] < tok[1]:
                b.rd[k] = tok
        for b in writes:
            b.lw = tok
            b.rd = {}

    def record(self, f):
        self.rec = []
        f()
        r, self.rec = self.rec, None
        return r

    def replay(self, items):
        for kind, a, kw in items:
            (self.op if kind == "op" else self.dma)(*a, **kw)

    def op(self, eng, fn, reads=(), writes=()):
        if self.rec is not None:
            self.rec.append(("op", (eng, fn), dict(reads=list(reads), writes=list(writes))))
            return
        waits = self._deps(eng, reads, writes)
        self.cnt[eng] += 1
        sem = self.sem[eng]
        self.q[eng].append((waits, fn, sem, 1))
        self._commit((sem, self.cnt[eng], eng), reads, writes)

    def dma(self, eng, fn, reads=(), writes=(), track=None, is_out=False):
        if self.rec is not None:
            self.rec.append(("dma", (eng, fn), dict(reads=list(reads), writes=list(writes), track=track, is_out=is_out)))
            return
        waits = self._deps(eng, reads, writes)
        tb = track if track is not None else (writes[0] if writes else reads[0])
        if tb.dsem is None:
            tb.dsem = self.newsem("d")
        tb.dcnt += 1
        val = 16 * tb.dcnt
        self.q[eng].append((waits, fn, tb.dsem, 16))
        tok = (tb.dsem, val, "dma")
        self.dtoks[id(tb.dsem)] = (tb.dsem, val)
        self._commit(tok, reads, writes)
        if is_out:
            self.out_tokens.append(tok)

    def flush(self, final=False):
        q = self.q
        self.q = {e: [] for e in ENGS}
        bar = {}
        for e in ENGS:
            w = []
            for f in ENGS:
                if f != e and self.cnt[f] > 0 and self.seen[e].get(id(self.sem[f]), 0) < self.cnt[f]:
                    w.append((self.sem[f], self.cnt[f]))
                    self.seen[e][id(self.sem[f])] = self.cnt[f]
            for k, (s, v) in self.dtoks.items():
                if self.seen[e].get(k, 0) < v:
                    w.append((s, v))
                    self.seen[e][k] = v
            bar[e] = w

        def run(e, name):
            for waits, fn, sem, inc in q[name]:
                for (ws, wv) in waits:
                    e.wait_ge(ws, wv)
                fn(e).then_inc(sem, inc)
            for (ws, wv) in bar[name]:
                e.wait_ge(ws, wv)

        with self.nc.Block() as block:
            @block.sync
            def _(e):
                run(e, "sp")

            @block.scalar
            def _(e):
                run(e, "act")

            @block.vector
            def _(e):
                run(e, "dve")

            @block.gpsimd
            def _(e):
                run(e, "pool")

            @block.tensor
            def _(e):
                run(e, "pe")


C_Q, C_K, C_V, C_Z, C_B, C_A, C_QL, C_KV, C_KI, C_WI, C_GA, C_GB = (
    0, 1024, 2048, 3072, 4096, 4104, 4112, 4368, 4624, 4688, 4696, 5720)


def build(phases=("p2", "p3a", "p3b")):
    nc = bass.Bass("TRN2", target_bir_lowering=False)
    dr = {}

    def din(name, shape, dt=F32):
        dr[name] = nc.dram_tensor(name, list(shape), dt, kind="ExternalInput").ap()

    din("xf", [L, D]); din("xo", [NOWN * 128, D]); din("cT", [128, 8])
    din("sel", [128, 4]); din("cmask", [128, 512])
    din("w_ada", [D, 6 * D]); din("b_ada", [6 * D]); din("norm1_w", [D]); din("w_in", [D, 6744])
    din("dn_conv_w", [128, 24, 4]); din("dn_a_log", [8]); din("dn_dt_bias", [8]); din("dn_onorm_w", [128])
    din("q_norm_w", [256]); din("kv_norm_w", [256]); din("idx_k_norm_w", [64])
    din("w_uq", [256, 1024]); din("w_iq", [256, 512]); din("w_uk", [256, 1024]); din("w_uv", [256, 1024])
    din("w_a_out", [D, D]); din("w_b_out", [D, D]); din("w_o", [D, D]); din("norm2_w", [D])
    din("peer_w_q", [D, 2048]); din("peer_sub_keys", [16, 128, 128]); din("peer_u", [16384, D]); din("peer_v", [16384, D])
    din("final_norm_w", [D])
    out_d = nc.dram_tensor("out", [NOWN * 128, D], F32, kind="ExternalOutput").ap()
    ckv_tm_d = nc.dram_tensor("ckv_tm_d", [L, 256], BF16).ap()
    ckvT_d = nc.dram_tensor("ckvT_d", [256, L], BF16).ap()
    kidxT_d = nc.dram_tensor("kidxT_d", [64, L], BF16).ap()
    uv_d = nc.dram_tensor("uv_d", [16384, 2 * D], BF16).ap()
    wb_d = nc.dram_tensor("wb_d", [16, 128, 8, 512], BF16).ap()
    oown_d = nc.dram_tensor("oown_d", [NOWN * 128, D], F32,
                            kind=("ExternalOutput" if "dbg_o" in DEBUG else "Internal")).ap()
    yb_d = nc.dram_tensor("yb_d", [NOWN * 128, D], F32,
                          kind=("ExternalOutput" if "dbg_yb" in DEBUG else "Internal")).ap()

    with ExitStack() as gst:
        P = Prog(nc, gst)
        ident = P.sb("ident", [128, 128], BF16); b_ident = Buf("ident")
        identf = P.sb("identf", [128, 128], F32); b_identf = Buf("identf")
        onesf = P.sb("onesf", [128, 128], F32); b_onesf = Buf("onesf")
        trile = P.sb("trile", [128, 128], F32); b_trile = Buf("trile")
        slm = P.sb("slm", [128, 128], F32); b_slm = Buf("slm")
        negs = P.sb("negs", [128, 128], F32); b_negs = Buf("negs")
        neg2 = P.sb("neg2", [128, 128], F32); b_neg2 = Buf("neg2")
        bdm = P.sb("bdm", [128, 128], F32); b_bdm = Buf("bdm")
        offm = P.sb("offm", [128, 128], F32); b_offm = Buf("offm")
        offtm = P.sb("offtm", [128, 128], F32); b_offtm = Buf("offtm")
        MR = {}
        mod_d = nc.dram_tensor("mod_d", [6 * D], F32).ap()
        selt = P.sb("selt", [128, 4], F32); b_sel = Buf("selt")

        def pool_sel(out, in_, pattern, op, fill, base, cm, rd, wr):
            P.op("pool", lambda e: e.affine_select(out=out, in_=in_, pattern=pattern, compare_op=op, fill=fill,
                                                   base=base, channel_multiplier=cm), reads=rd, writes=wr)

        P.op("pool", lambda e: e.memset(onesf[:], 1.0), writes=[b_onesf])
        pool_sel(identf[:], onesf[:], [[-1, 128]], ALU.is_equal, 0.0, 0, 1, [b_onesf], [b_identf])
        P.op("dve", lambda e: e.tensor_copy(out=ident[:], in_=identf[:]), reads=[b_identf], writes=[b_ident])
        pool_sel(trile[:], onesf[:], [[1, 128]], ALU.is_ge, 0.0, 0, -1, [b_onesf], [b_trile])
        pool_sel(slm[:], onesf[:], [[-1, 128]], ALU.is_gt, 0.0, 0, 1, [b_onesf], [b_slm])
        zf = P.sb("zf", [128, 128], F32); b_zf = Buf("zf")
        P.op("pool", lambda e: e.memset(zf[:], 0.0), writes=[b_zf])
        pool_sel(negs[:], zf[:], [[-1, 128]], ALU.is_gt, NEG, 0, 1, [b_zf], [b_negs])
        pool_sel(neg2[:], zf[:], [[1, 128]], ALU.is_ge, NEG, 0, -1, [b_zf], [b_neg2])
        P.op("pool", lambda e: e.memset(bdm[:], 0.0), writes=[b_bdm])
        P.op("pool", lambda e: e.memset(bdm[0:64, 0:64], 1.0), reads=[b_bdm], writes=[b_bdm])
        P.op("pool", lambda e: e.memset(bdm[64:128, 64:128], 1.0), reads=[b_bdm], writes=[b_bdm])
        bdsl = P.sb("bdsl", [128, 128], F32); b_bdsl = Buf("bdsl")
        bdsu = P.sb("bdsu", [128, 128], F32); b_bdsu = Buf("bdsu")
        pool_sel(bdsl[:], bdm[:], [[-1, 128]], ALU.is_gt, 0.0, 0, 1, [b_bdm], [b_bdsl])
        pool_sel(bdsu[:], bdm[:], [[1, 128]], ALU.is_gt, 0.0, 0, -1, [b_bdm], [b_bdsu])
        P.op("pool", lambda e: e.memset(offm[:], 0.0), writes=[b_offm])
        P.op("pool", lambda e: e.memset(offm[64:128, 0:64], 1.0), reads=[b_offm], writes=[b_offm])
        P.op("pool", lambda e: e.memset(offtm[:], 0.0), writes=[b_offtm])
        P.op("pool", lambda e: e.memset(offtm[0:64, 64:128], 1.0), reads=[b_offtm], writes=[b_offtm])
        P.dma("sp", lambda e: e.dma_start(out=selt[:], in_=dr["sel"]), writes=[b_sel])

        with ExitStack() as st:
            P.stack = st
            modrow = P.sb("modrow_s", [128, 6, D], F32); b_mod = Buf("modrow_s")
            cTt = P.sb("cTt", [128, 8], F32); b_cT = Buf("cTt")
            scT = P.sb("scT", [128, 8], BF16); b_scT = Buf("scT")
            nrow = P.sb("nrow", [128, 2, D], F32); b_nrow = Buf("nrow")
            was = [P.sb("wa%d" % i, [128, 8, 512], BF16) for i in range(2)]; b_wa = bufs("wa", 2)
            pm = [P.ps("pm%d" % i, [128, 512], F32) for i in range(2)]; b_pm = bufs("pm", 2)
            P.dma("sp", lambda e: e.dma_start(out=cTt[:], in_=dr["cT"]), writes=[b_cT])
            P.dma("sp", lambda e: e.dma_start(out=modrow[:].rearrange("p a d -> p (a d)"),
                                              in_=dr["b_ada"].partition_broadcast(128)), writes=[b_mod])
            P.dma("sp", lambda e: e.dma_start(out=nrow[:, 0, :], in_=dr["norm1_w"].partition_broadcast(128)), writes=[b_nrow])
            P.dma("sp", lambda e: e.dma_start(out=nrow[:, 1, :], in_=dr["norm2_w"].partition_broadcast(128)), writes=[b_nrow])
            P.op("act", lambda e: e.activation(out=scT[:], in_=cTt[:], func=AF.Silu), reads=[b_cT], writes=[b_scT])
            wa_v = dr["w_ada"].rearrange("(k p) n -> p k n", p=128)
            for cc in range(12):
                s = cc % 2
                P.dma("pool", lambda e, s=s, cc=cc: e.dma_start(out=was[s][:], in_=wa_v[:, :, cc * 512:(cc + 1) * 512]),
                      writes=[b_wa[s]])
                for k in range(8):
                    P.op("pe", lambda e, s=s, k=k: e.matmul(pm[s][:], lhsT=scT[:, k:k + 1].to_broadcast([128, 128]),
                                                            rhs=was[s][:, k, :], start=(k == 0), stop=(k == 7)),
                         reads=[b_scT, b_wa[s]], writes=[b_pm[s]])
                a, o = cc // 2, (cc % 2) * 512
                P.op("dve", lambda e, s=s, a=a, o=o: e.tensor_tensor(out=modrow[:, a, o:o + 512], in0=modrow[:, a, o:o + 512],
                                                                  in1=pm[s][:], op=ALU.add),
                     reads=[b_pm[s], b_mod], writes=[b_mod])
            for (a, n) in ((1, 0), (4, 1)):
                P.op("dve", lambda e, a=a, n=n: e.scalar_tensor_tensor(out=modrow[:, a, :], in0=modrow[:, a, :], scalar=1.0,
                                                                      in1=nrow[:, n, :], op0=ALU.add, op1=ALU.mult),
                     reads=[b_mod, b_nrow], writes=[b_mod])
            P.dma("sp", lambda e: e.dma_start(out=mod_d.rearrange("(a n) -> a n", a=1), in_=modrow[0:1, :, :].rearrange("p a d -> p (a d)")),
                  reads=[b_mod], track=b_mod)
            P.flush()
        P.stack = gst

        def normmod(xt, bx, hb, bh, sq, bsq, st_, bst, arow, brow):
            P.op("pool", lambda e: e.memset(st_[:, 0:1], 0.0), writes=[bst])
            P.op("act", lambda e: e.activation(out=sq, in_=xt, func=AF.Square, accum_out=st_[:, 0:1]),
                 reads=[bx], writes=[bsq, bst])
            P.op("act", lambda e: e.activation(out=st_[:, 1:2], in_=st_[:, 0:1], func=AF.Ln, scale=1.0 / D, bias=EPS),
                 reads=[bst], writes=[bst])
            P.op("act", lambda e: e.activation(out=st_[:, 2:3], in_=st_[:, 1:2], func=AF.Exp, scale=-0.5), reads=[bst], writes=[bst])
            mr_, bmr_ = MR["t"], MR["b"]
            P.op("dve", lambda e: e.scalar_tensor_tensor(out=sq, in0=xt, scalar=st_[:, 2:3], in1=mr_[:, arow, :],
                                                          op0=ALU.mult, op1=ALU.mult),
                 reads=[bx, bst, bmr_, bsq], writes=[bsq])
            P.op("dve", lambda e: e.tensor_tensor(out=hb, in0=sq, in1=mr_[:, brow, :], op=ALU.add),
                 reads=[bsq, bmr_], writes=[bh])

        G_ = locals()
        if "p2" in phases:
            phase2(P, nc, dr, G_)
        if "p3a" in phases:
            phase3a(P, nc, dr, G_)
        if "p3b" in phases:
            phase3b(P, nc, dr, G_)
        if any(P.q[e] for e in ENGS):
            P.flush()
    return nc


def phase2(P, nc, dr, G):
    gst = P.stack
    ident, b_ident, identf, b_identf = G["ident"], G["b_ident"], G["identf"], G["b_identf"]
    onesf, b_onesf, trile, b_trile, slm, b_slm = G["onesf"], G["b_onesf"], G["trile"], G["b_trile"], G["slm"], G["b_slm"]
    negs, b_negs, neg2, b_neg2 = G["negs"], G["b_negs"], G["neg2"], G["b_neg2"]
    bdm, b_bdm, offm, b_offm, offtm, b_offtm = G["bdm"], G["b_bdm"], G["offm"], G["b_offm"], G["offtm"], G["b_offtm"]
    bdsl, b_bdsl, bdsu, b_bdsu = G["bdsl"], G["b_bdsl"], G["bdsu"], G["b_bdsu"]
    selt, b_sel, normmod = G["selt"], G["b_sel"], G["normmod"]
    ckv_tm_d, ckvT_d, kidxT_d, oown_d = G["ckv_tm_d"], G["ckvT_d"], G["kidxT_d"], G["oown_d"]
    uv_d = G["uv_d"]
    with ExitStack() as st:
        P.stack = st
        modrow = P.sb("modrow_2", [128, 2, D], F32); b_mod = Buf("modrow_2")
        P.dma("sp", lambda e: e.dma_start(out=modrow[:].rearrange("p a d -> p (a d)"), in_=G["mod_d"][0:2 * D].partition_broadcast(128)), writes=[b_mod])
        G["MR"]["t"], G["MR"]["b"] = modrow, b_mod
        wqkv = P.sb("wqkv", [128, 8, 3072], BF16); b_wqkv = Buf("wqkv")
        wkvba = P.sb("wkvba", [128, 8, 336], BF16); b_wkvba = Buf("wkvba")
        win_v = dr["w_in"].rearrange("(k p) n -> p k n", p=128)
        for k in range(8):
            P.dma("pool", lambda e, k=k: e.dma_start(out=wqkv[:, k, :], in_=win_v[:, k, 0:3072]), writes=[b_wqkv])
        P.dma("pool", lambda e: e.dma_start(out=wkvba[:, :, 0:320], in_=win_v[:, :, C_KV:C_KV + 320]), writes=[b_wkvba])
        P.dma("pool", lambda e: e.dma_start(out=wkvba[:, :, 320:336], in_=win_v[:, :, C_B:C_B + 16]), writes=[b_wkvba])
        convw = P.sb("convw", [128, 24, 4], F32); b_convw = Buf("convw")
        P.dma("sp", lambda e: e.dma_start(out=convw[:], in_=dr["dn_conv_w"]), writes=[b_convw])
        rows = P.sb("rows", [128, 16 + 256 + 64], F32); b_rows = Buf("rows")
        P.dma("sp", lambda e: e.dma_start(out=rows[:, 0:8], in_=dr["dn_a_log"].partition_broadcast(128)), writes=[b_rows])
        P.dma("sp", lambda e: e.dma_start(out=rows[:, 8:16], in_=dr["dn_dt_bias"].partition_broadcast(128)), writes=[b_rows])
        P.dma("sp", lambda e: e.dma_start(out=rows[:, 16:272], in_=dr["kv_norm_w"].partition_broadcast(128)), writes=[b_rows])
        P.dma("sp", lambda e: e.dma_start(out=rows[:, 272:336], in_=dr["idx_k_norm_w"].partition_broadcast(128)), writes=[b_rows])
        nea = P.sb("nea", [128, 8], F32); b_nea = Buf("nea")
        P.op("act", lambda e: e.activation(out=nea[:], in_=rows[:, 0:8], func=AF.Exp), reads=[b_rows], writes=[b_nea])
        P.op("dve", lambda e: e.tensor_scalar(out=nea[:], in0=nea[:], scalar1=-1.0, scalar2=None, op0=ALU.mult),
             reads=[b_nea], writes=[b_nea])

        xts = [P.sb("xt%d" % i, [128, D], F32) for i in range(1)] * 2; b_xt = bufs("xt", 1) * 2
        sqs = P.sb("sq", [128, D], F32); b_sq = Buf("sq")
        stt = [P.sb("stt%d" % i, [128, 4], F32) for i in range(1)] * 2; b_stt = bufs("stt", 1) * 2
        hbs = [P.sb("hb%d" % i, [128, D], BF16) for i in range(1)] * 2; b_hb = bufs("hb", 1) * 2
        hTg = [P.sb("hTg%d" % i, [128, 8, 512], BF16) for i in range(1)] * 2; b_hTg = bufs("hTg", 1) * 2
        kvba = [P.sb("kvba%d" % i, [128, 336], F32) for i in range(4)]; b_kvba = bufs("kvba", 4)
        hist = P.sb("hist", [128, 24, 3], F32); b_hist = bufs("hist", 24)
        pcs = [P.sb("pc%d" % i, [128, 515], F32) for i in range(2)]; b_pc = bufs("pc", 2)
        cacc = [P.sb("cacc%d" % i, [128, 512], F32) for i in range(2)]; b_cacc = bufs("cacc", 2)
        qkvs = P.sb("qkvs", [128, 24, 512], BF16); b_qkvs = bufs("qkvs", 24)
        S = P.sb("S", [128, 8, 128], F32); b_S = bufs("S", 8)
        Sb = P.sb("Sb", [128, 8, 128], BF16); b_Sb = bufs("Sb", 8)
        oacc = [P.sb("oacc%d" % i, [128, D], F32) for i in range(1)] * 2; b_oacc = bufs("oacc", 1) * 2
        tms = [P.sb("tm%d" % i, [128, 3, 8, 128], BF16) for i in range(2)]
        b_tms = [[bufs("tm%d_%d_" % (i, w), 8) for w in range(3)] for i in range(2)]
        scs2 = [P.sb("sc%d" % i, [128, 24, 8], F32) for i in range(2)]; b_scs2 = bufs("sc", 2)
        sqt = sqs[:, :].rearrange("p (a b) -> p a b", a=8); b_sqt = b_sq
        Rs = [P.sb("R%d" % i, [128, 8, 256], BF16) for i in range(2)]; b_Rs = [bufs("R%d_" % i, 8) for i in range(2)]
        Ks2s = [P.sb("Ks2_%d" % i, [128, 8, 128], BF16) for i in range(2)]; b_Ks2s = bufs("Ks2_", 2)
        Lgs = [P.sb("Lg%d" % i, [128, 8, 128], F32) for i in range(2)]; b_Qms = [bufs("Qm%d_" % i, 8) for i in range(2)]
        E = P.sb("E", [128, 8, 128], F32); b_E = bufs("E", 8); och = E; b_och = b_E
        ET = P.sb("ET", [128, 8, 128], F32); b_ET = bufs("ET", 2)
        Np = P.sb("Np", [128, 8, 128], F32); b_Np = bufs("Np", 8)
        NA = [P.sb("NA%d" % i, [128, 8, 128], F32) for i in range(2)]; b_NA = [bufs("NA%d_" % i, 8) for i in range(2)]
        NT = [P.sb("NT%d" % i, [128, 8, 128], F32) for i in range(2)]; b_NT = [bufs("NT%d_" % i, 8) for i in range(2)]
        Qb = P.sb("Qb", [128, 8, 128], BF16); b_Qb = bufs("Qb", 8)
        y1 = P.sb("y1", [128, 8, 256], BF16); b_y1 = bufs("y1", 8); R2 = y1; b_R2 = b_y1
        aT = P.sb("aT", [128, 8, 128], BF16); b_aT = bufs("aT", 8)
        o1 = Np; b_o1 = b_Np
        ckvn = P.sb("ckvn", [128, 320], BF16); b_ckvn = Buf("ckvn")
        ckvT_s = P.sb("ckvT_s", [128, 3, 128], BF16); b_ckvTs = Buf("ckvT_s")
        b_uvd = Buf("uvd")
        psA = P.ps("psA", [128, 512], F32); b_psA = Buf("psA")
        psB = P.ps("psB", [128, 512], F32); b_psB = Buf("psB")
        psT = P.ps("psT", [128, 8, 128], BF16); b_psT = Buf("psT")
        psH = [P.ps("psH%d" % i, [128, 4, 128], F32) for i in range(4)]; b_psH = bufs("psH", 4)
        psS = P.ps("psS", [128, 512], F32); b_psS = Buf("psS")

        P.op("pool", lambda e: e.memset(hist[:], 0.0), writes=b_hist)
        P.op("pool", lambda e: e.memset(S[:], 0.0), writes=b_S)
        P.op("pool", lambda e: e.memset(Sb[:], 0.0), writes=b_Sb)

        def mm(out, lhsT, rhs, start, stop, rd, wr):
            P.op("pe", lambda e: e.matmul(out, lhsT=lhsT, rhs=rhs, start=start, stop=stop), reads=rd, writes=wr)

        def tr(out, in_, idn, rd, wr):
            P.op("pe", lambda e: e.transpose(out=out, in_=in_, identity=idn), reads=rd, writes=wr)

        def front(g):
            hs = 0
            for cb in range(4):
                blk = 4 * g + cb
                xs = blk % 2
                P.dma("sp", lambda e, xs=xs, blk=blk: e.dma_start(out=xts[xs][:], in_=dr["xf"][blk * 128:(blk + 1) * 128, :]),
                      writes=[b_xt[xs]])
                normmod(xts[xs][:], b_xt[xs], hbs[xs][:], b_hb[xs], sqs[:], b_sq, stt[xs], b_stt[xs], 1, 0)
                for k in range(8):
                    tr(psT[:, k, :], hbs[xs][:, k * 128:(k + 1) * 128], ident[:], [b_hb[xs], b_ident], [b_psT])
                P.op("act", lambda e, hs=hs, cb=cb: e.copy(out=hTg[hs][:, :, cb * 128:(cb + 1) * 128], in_=psT[:]),
                     reads=[b_psT], writes=[b_hTg[hs]])
                for k in range(8):
                    mm(psS[:, 0:336], hTg[hs][:, k, cb * 128:(cb + 1) * 128], wkvba[:, k, :], k == 0, k == 7,
                       [b_hTg[hs], b_wkvba], [b_psS])
                P.op("act", lambda e, cb=cb: e.copy(out=kvba[cb][:], in_=psS[:, 0:336]), reads=[b_psS], writes=[b_kvba[cb]])

        front(0)
        for g in range(NCH // 4):
            hs = 0
            for cc in range(24):
                ps_, bps_ = (psA, b_psA) if cc % 2 == 0 else (psB, b_psB)
                p = cc % 2
                for k in range(8):
                    mm(ps_[:], wqkv[:, k, cc * 128:(cc + 1) * 128], hTg[hs][:, k, :], k == 0, k == 7,
                       [b_wqkv, b_hTg[hs]], [bps_])
                P.op("act", lambda e, p=p, ps_=ps_: e.copy(out=pcs[p][:, 3:515], in_=ps_[:]), reads=[bps_], writes=[b_pc[p]])
                P.op("dve", lambda e, p=p, cc=cc: e.tensor_copy(out=pcs[p][:, 0:3], in_=hist[:, cc, :]),
                     reads=[b_hist[cc], b_pc[p]], writes=[b_pc[p]])
                P.op("act", lambda e, ps_=ps_, cc=cc: e.copy(out=hist[:, cc, :], in_=ps_[:, 509:512]), reads=[bps_], writes=[b_hist[cc]])
                P.op("dve", lambda e, p=p, cc=cc: e.tensor_scalar(out=cacc[p][:], in0=pcs[p][:, 3:515], scalar1=convw[:, cc, 3:4],
                                                               scalar2=None, op0=ALU.mult),
                     reads=[b_pc[p], b_convw], writes=[b_cacc[p]])
                for i in range(3):
                    P.op("dve", lambda e, p=p, cc=cc, i=i: e.scalar_tensor_tensor(
                        out=cacc[p][:], in0=pcs[p][:, i:i + 512], scalar=convw[:, cc, i:i + 1], in1=cacc[p][:],
                        op0=ALU.mult, op1=ALU.add), reads=[b_pc[p], b_convw, b_cacc[p]], writes=[b_cacc[p]])
                P.op("act", lambda e, p=p, cc=cc: e.activation(out=qkvs[:, cc, :], in_=cacc[p][:], func=AF.Silu),
                     reads=[b_cacc[p]], writes=[b_qkvs[cc]])

            def S1(cb, par):
                tm, b_tm, sc, b_sc, R, b_R, Ks2, b_Ks2 = tms[par], b_tms[par], scs2[par], b_scs2[par], Rs[par], b_Rs[par], Ks2s[par], b_Ks2s[par]
                Lg, b_Qm = Lgs[par], b_Qms[par]
                Qm = Lg
                NoT, b_NoT, sol, b_sol, wT, b_wT, vn, b_vn = tm[:, 2, :, :], b_tm[2], R, b_R, tm[:, 1, :, :], b_tm[1], tm[:, 0, :, :], b_tm[0]
                blk = 4 * g + cb
                c0 = cb * 128
                kb = kvba[cb]; bkb = b_kvba[cb]
                P.op("pool", lambda e: e.memset(sc[:, 20, 0:2], 0.0), writes=[b_sc])
                P.op("act", lambda e, kb=kb: e.activation(out=sqt[:, 0:2, :].rearrange("p a b -> p (a b)"), in_=kb[:, 0:256],
                                                          func=AF.Square, accum_out=sc[:, 20, 0:1]),
                     reads=[bkb, b_sc], writes=[b_sqt, b_sc])
                P.op("act", lambda e, kb=kb: e.activation(out=sqt[:, 2, 0:64], in_=kb[:, 256:320],
                                                          func=AF.Square, accum_out=sc[:, 20, 1:2]),
                     reads=[bkb, b_sc], writes=[b_sqt, b_sc])
                P.op("act", lambda e: e.activation(out=sc[:, 20, 2:3], in_=sc[:, 20, 0:1], func=AF.Ln, scale=1.0 / 256, bias=EPS),
                     reads=[b_sc], writes=[b_sc])
                P.op("act", lambda e: e.activation(out=sc[:, 20, 3:4], in_=sc[:, 20, 1:2], func=AF.Ln, scale=1.0 / 64, bias=EPS),
                     reads=[b_sc], writes=[b_sc])
                P.op("act", lambda e: e.activation(out=sc[:, 20, 4:6], in_=sc[:, 20, 2:4], func=AF.Exp, scale=-0.5), reads=[b_sc], writes=[b_sc])
                P.op("dve", lambda e, kb=kb: e.scalar_tensor_tensor(out=ckvn[:, 0:256], in0=kb[:, 0:256], scalar=sc[:, 20, 4:5],
                                                                    in1=rows[:, 16:272], op0=ALU.mult, op1=ALU.mult),
                     reads=[bkb, b_sc, b_rows], writes=[b_ckvn])
                P.op("dve", lambda e, kb=kb: e.scalar_tensor_tensor(out=ckvn[:, 256:320], in0=kb[:, 256:320], scalar=sc[:, 20, 5:6],
                                                                    in1=rows[:, 272:336], op0=ALU.mult, op1=ALU.mult),
                     reads=[bkb, b_sc, b_rows, b_ckvn], writes=[b_ckvn])
                P.dma("sp", lambda e, blk=blk: e.dma_start(out=ckv_tm_d[blk * 128:(blk + 1) * 128, :], in_=ckvn[:, 0:256]),
                      reads=[b_ckvn], track=b_ckvn)
                for i in range(2):
                    tr(psT[:, i, :], ckvn[:, i * 128:(i + 1) * 128], ident[:], [b_ckvn, b_ident], [b_psT])
                tr(psT[0:64, 2, :], ckvn[:, 256:320], ident[:], [b_ckvn, b_ident], [b_psT])
                P.op("act", lambda e: e.copy(out=ckvT_s[:, 0:2, :], in_=psT[:, 0:2, :]), reads=[b_psT], writes=[b_ckvTs])
                P.op("act", lambda e: e.copy(out=ckvT_s[0:64, 2, :], in_=psT[0:64, 2, :]), reads=[b_psT, b_ckvTs], writes=[b_ckvTs])
                P.dma("sp", lambda e, blk=blk: e.dma_start(
                    out=ckvT_d[:, blk * 128:(blk + 1) * 128].rearrange("(a p) t -> p a t", p=128), in_=ckvT_s[:, 0:2, :]),
                    reads=[b_ckvTs], track=b_ckvTs)
                P.dma("sp", lambda e, blk=blk: e.dma_start(out=kidxT_d[:, blk * 128:(blk + 1) * 128], in_=ckvT_s[0:64, 2, :]),
                      reads=[b_ckvTs], track=b_ckvTs)

                for tb in range(2):
                    r0 = blk * 256
                    src = dr["peer_u" if tb == 0 else "peer_v"][r0:r0 + 256, :]
                    dst = uv_d[r0:r0 + 256, tb * D:(tb + 1) * D]
                    P.dma("pool", lambda e, src=src, dst=dst: e.dma_start(out=dst, in_=src), writes=[b_uvd], track=b_uvd)
                for w in range(3):
                    for h in range(8):
                        tr(psT[:, h, :], qkvs[:, 8 * w + h, c0:c0 + 128], ident[:], [b_qkvs[8 * w + h], b_ident], [b_psT])
                    P.op("act" if w != 1 else "dve",
                         (lambda e, w=w: e.copy(out=tm[:, w, :, :], in_=psT[:])) if w != 1 else
                         (lambda e, w=w: e.tensor_copy(out=tm[:, w, :, :], in_=psT[:])),
                         reads=[b_psT], writes=b_tm[w])
                def scop(eng, fn, extra_r=()):
                    P.op(eng, fn, reads=[b_sc] + list(extra_r), writes=[b_sc])
                scop("act", lambda e, kb=kb: e.activation(out=sc[:, 0, :], in_=kb[:, 320:328], func=AF.Exp, scale=-1.0), [bkb])
                scop("dve", lambda e: e.tensor_scalar(out=sc[:, 0, :], in0=sc[:, 0, :], scalar1=1.0, scalar2=None, op0=ALU.add))
                scop("dve", lambda e: e.reciprocal(out=sc[:, 0, :], in_=sc[:, 0, :]))
                scop("dve", lambda e, kb=kb: e.tensor_tensor(out=sc[:, 16, :], in0=kb[:, 328:336], in1=rows[:, 8:16], op=ALU.add),
                     [bkb, b_rows])
                scop("act", lambda e: e.activation(out=sc[:, 16, :], in_=sc[:, 16, :], func=AF.Exp))
                scop("act", lambda e: e.activation(out=sc[:, 16, :], in_=sc[:, 16, :], func=AF.Ln, bias=1.0))
                scop("dve", lambda e: e.tensor_tensor(out=sc[:, 1, :], in0=sc[:, 16, :], in1=nea[:], op=ALU.mult), [b_nea])
                mm(psS[:, 0:8], trile[:], sc[:, 1, :], True, True, [b_trile, b_sc], [b_psS])
                mm(psS[:, 8:16], onesf[:], sc[:, 1, :], True, True, [b_onesf, b_sc], [b_psS])
                scop("act", lambda e: e.copy(out=sc[:, 2:4, :].rearrange("p a b -> p (a b)"), in_=psS[:, 0:16]), [b_psS])
                scop("act", lambda e: e.activation(out=sc[:, 4, :], in_=sc[:, 2, :], func=AF.Exp))
                scop("dve", lambda e: e.tensor_tensor(out=sc[:, 16, :], in0=sc[:, 3, :], in1=sc[:, 2, :], op=ALU.subtract))
                scop("act", lambda e: e.activation(out=sc[:, 5, :], in_=sc[:, 16, :], func=AF.Exp))
                scop("act", lambda e: e.activation(out=sc[:, 6, :], in_=sc[:, 3, :], func=AF.Exp))
                for (w, row) in ((0, 7), (1, 8)):
                    P.op("act", lambda e, w=w: e.activation(out=sqt[:].rearrange("p a b -> p (a b)"),
                                                            in_=tm[:, w, :, :].rearrange("p a b -> p (a b)"), func=AF.Square),
                         reads=b_tm[w], writes=[b_sqt])
                    scop("dve", lambda e, row=row: e.reduce_sum(out=sc[:, row, :], in_=sqt[:], axis=AX.X), [b_sqt])
                scop("act", lambda e: e.activation(out=sc[:, 7:9, :], in_=sc[:, 7:9, :], func=AF.Ln, bias=EPS))
                scop("act", lambda e: e.activation(out=sc[:, 9, :], in_=sc[:, 8, :], func=AF.Exp, scale=-1.0))
                scop("act", lambda e: e.activation(out=sc[:, 10, :], in_=sc[:, 8, :], func=AF.Exp, scale=-0.5))
                scop("act", lambda e: e.activation(out=sc[:, 11, :], in_=sc[:, 7, :], func=AF.Exp, scale=-0.5))
                scop("dve", lambda e: e.tensor_scalar(out=sc[:, 11, :], in0=sc[:, 11, :], scalar1=128.0 ** -0.5, scalar2=None, op0=ALU.mult))
                scop("dve", lambda e: e.tensor_tensor(out=sc[:, 12, :], in0=sc[:, 11, :], in1=sc[:, 4, :], op=ALU.mult))
                scop("dve", lambda e: e.tensor_tensor(out=sc[:, 13, :], in0=sc[:, 10, :], in1=sc[:, 0, :], op=ALU.mult))
                scop("dve", lambda e: e.tensor_tensor(out=sc[:, 16, :], in0=sc[:, 9, :], in1=sc[:, 0, :], op=ALU.mult))
                scop("dve", lambda e: e.tensor_tensor(out=sc[:, 14, :], in0=sc[:, 16, :], in1=sc[:, 4, :], op=ALU.mult))
                scop("dve", lambda e: e.tensor_scalar(out=sc[:, 15, :], in0=sc[:, 16, :], scalar1=-1.0, scalar2=None, op0=ALU.mult))
                P.op("dve", lambda e: e.tensor_tensor(out=R[:, :, 0:128], in0=tm[:, 2, :, :],
                                                      in1=sc[:, 13, :].unsqueeze(2).to_broadcast([128, 8, 128]), op=ALU.mult),
                     reads=b_tm[2] + [b_sc], writes=b_R)
                P.op("dve", lambda e: e.tensor_tensor(out=R[:, :, 128:256], in0=tm[:, 1, :, :],
                                                      in1=sc[:, 14, :].unsqueeze(2).to_broadcast([128, 8, 128]), op=ALU.mult),
                     reads=b_tm[1] + [b_sc] + b_R, writes=b_R)
                P.op("dve", lambda e: e.tensor_tensor(out=Ks2[:], in0=tm[:, 1, :, :],
                                                       in1=sc[:, 5, :].unsqueeze(2).to_broadcast([128, 8, 128]), op=ALU.mult),
                     reads=b_tm[1] + [b_sc], writes=[b_Ks2])
                P.op("dve", lambda e: e.tensor_tensor(out=Lg[:], in0=slm[:].unsqueeze(1).to_broadcast([128, 8, 128]),
                                                       in1=sc[:, 1, :].unsqueeze(2).to_broadcast([128, 8, 128]), op=ALU.mult),
                     reads=[b_slm, b_sc], writes=b_Qm)

            def S234(cb, par):
                tm, b_tm, sc, b_sc, R, b_R, Ks2, b_Ks2 = tms[par], b_tms[par], scs2[par], b_scs2[par], Rs[par], b_Rs[par], Ks2s[par], b_Ks2s[par]
                Lg, b_Qm = Lgs[par], b_Qms[par]
                Qm = Lg
                NoT, b_NoT, sol, b_sol, wT, b_wT, vn, b_vn = tm[:, 2, :, :], b_tm[2], R, b_R, tm[:, 1, :, :], b_tm[1], tm[:, 0, :, :], b_tm[0]
                blk = 4 * g + cb
                c0 = cb * 128
                kb = kvba[cb]; bkb = b_kvba[cb]
                def b4(row, hh):
                    return sc[:, row, 4 * hh:4 * hh + 4].unsqueeze(2).to_broadcast([128, 4, 128])
                CB = (psA, psB); b_CB = (b_psA, b_psB)
                def g_(hh):
                    hs4 = slice(4 * hh, 4 * hh + 4)
                    for i in range(4):
                        h = 4 * hh + i
                        mm(psH[hh][:, i, :], trile[:], Lg[:, h, :], True, True, [b_trile, b_Qm[h]], [b_psH[hh]])
                    P.op("act", lambda e, hh=hh, hs4=hs4: e.activation(out=E[:, hs4, :], in_=psH[hh][:], func=AF.Exp),
                         reads=[b_psH[hh]], writes=b_E[hs4])
                    yield
                    for i in range(4):
                        h = 4 * hh + i
                        mm(psH[2 + hh][:, i, :], Lg[:, h, :], trile[:], True, False, [b_trile, b_Qm[h]], [b_psH[2 + hh]])
                        mm(psH[2 + hh][:, i, :], identf[:], neg2[:], False, True, [b_identf, b_neg2], [b_psH[2 + hh]])
                    P.op("act", lambda e, hh=hh, hs4=hs4: e.activation(out=ET[:, hs4, :], in_=psH[2 + hh][:], func=AF.Exp),
                         reads=[b_psH[2 + hh]], writes=[b_ET[hh]])
                    yield
                    P.op("dve", lambda e, hh=hh, hs4=hs4: e.tensor_tensor(out=E[:, hs4, :], in0=E[:, hs4, :], in1=b4(15, hh), op=ALU.mult),
                         reads=b_E[hs4] + [b_sc], writes=b_E[hs4])
                    yield
                rr(*[g_(v_) for v_ in range(2)])
                def g_(hh):
                    hs4 = slice(4 * hh, 4 * hh + 4)
                    for i in range(4):
                        h = 4 * hh + i
                        kT = qkvs[:, 8 + h, c0:c0 + 128]
                        mm(psH[hh][:, i, :], kT, kT, True, True, [b_qkvs[8 + h]], [b_psH[hh]])
                    P.op("dve", lambda e, hh=hh, hs4=hs4: e.tensor_tensor(out=Np[:, hs4, :], in0=psH[hh][:], in1=E[:, hs4, :], op=ALU.mult),
                         reads=[b_psH[hh]] + b_E[hs4], writes=b_Np[hs4])
                    yield
                    for i in range(4):
                        h = 4 * hh + i
                        tr(psH[2 + hh][:, i, :], Np[:, h, :], identf[:], [b_Np[h], b_identf], [b_psH[2 + hh]])
                    P.op("dve", lambda e, hh=hh, hs4=hs4: e.tensor_tensor(out=NT[0][:, hs4, :], in0=psH[2 + hh][:],
                                                                       in1=bdsu[:].unsqueeze(1).to_broadcast([128, 4, 128]), op=ALU.mult),
                         reads=[b_psH[2 + hh], b_bdsu], writes=b_NT[0][hs4])
                    yield
                    P.op("dve", lambda e, hh=hh, hs4=hs4: e.tensor_tensor(out=NoT[:, hs4, :], in0=psH[2 + hh][:],
                                                                       in1=offtm[:].unsqueeze(1).to_broadcast([128, 4, 128]), op=ALU.mult),
                         reads=[b_psH[2 + hh], b_offtm], writes=b_NoT[hs4])
                    yield
                    P.op("dve", lambda e, hs4=hs4: e.tensor_tensor(out=NA[0][:, hs4, :], in0=Np[:, hs4, :],
                                                                    in1=bdsl[:].unsqueeze(1).to_broadcast([128, 4, 128]), op=ALU.mult),
                         reads=b_Np[hs4] + [b_bdsl], writes=b_NA[0][hs4])
                    yield
                    P.op("dve", lambda e, hs4=hs4: e.tensor_tensor(out=Qm[:, hs4, :], in0=NT[0][:, hs4, :],
                                                                    in1=identf[:].unsqueeze(1).to_broadcast([128, 4, 128]), op=ALU.add),
                         reads=b_NT[0][hs4] + [b_identf], writes=b_Qm[hs4])
                    yield
                rr(*[g_(v_) for v_ in range(2)])
                cur = 0
                for lev in range(5):
                    nxt = 1 - cur
                    last = (lev == 4)
                    def g_(hh):
                        hs4 = slice(4 * hh, 4 * hh + 4)
                        for i in range(4):
                            h = 4 * hh + i
                            mm(psH[hh][:, i, :], NT[cur][:, h, :], NA[cur][:, h, :], True, True,
                               [b_NT[cur][h], b_NA[cur][h]], [b_psH[hh]])
                        P.op("act", lambda e, hh=hh, hs4=hs4, nxt=nxt: e.copy(out=NA[nxt][:, hs4, :], in_=psH[hh][:]),
                             reads=[b_psH[hh]], writes=b_NA[nxt][hs4])
                        yield
                        if not last:
                            for i in range(4):
                                h = 4 * hh + i
                                mm(psH[2 + hh][:, i, :], NA[cur][:, h, :], NT[cur][:, h, :], True, True,
                                   [b_NT[cur][h], b_NA[cur][h]], [b_psH[2 + hh]])
                            P.op("act", lambda e, hh=hh, hs4=hs4, nxt=nxt: e.copy(out=NT[nxt][:, hs4, :], in_=psH[2 + hh][:]),
                                 reads=[b_psH[2 + hh]], writes=b_NT[nxt][hs4])
                        for i in range(4):
                            h = 4 * hh + i
                            mm(CB[hh][:, i * 128:(i + 1) * 128], NA[nxt][:, h, :], Qm[:, h, :], True, True,
                               [b_NA[nxt][h], b_Qm[h]], [b_CB[hh]])
                        P.op("dve", lambda e, hh=hh, hs4=hs4: e.tensor_tensor(out=Qm[:, hs4, :], in0=CB[hh][:].rearrange("p (a b) -> p a b", a=4),
                                                                           in1=Qm[:, hs4, :], op=ALU.add),
                             reads=[b_CB[hh]] + b_Qm[hs4], writes=b_Qm[hs4])
                        yield
                    rr(*[g_(v_) for v_ in range(2)])
                    cur = nxt
                P.op("act", lambda e: e.copy(out=Qb[:], in_=Qm[:]), reads=b_Qm, writes=b_Qb)
                def g_(q):
                    hs2 = slice(2 * q, 2 * q + 2)
                    pb, bpb = psH[q], b_psH[q]
                    pbv = pb[:].rearrange("p a b -> p (a b)").rearrange("p (a b) -> p a b", a=2)
                    for i in range(2):
                        h = 2 * q + i
                        mm(pbv[:, i, :], Qb[:, h, :], R[:, h, :], True, True, [b_Qb[h], b_R[h]], [bpb])
                    P.op("act", lambda e, hs2=hs2, pbv=pbv: e.copy(out=y1[:, hs2, :], in_=pbv), reads=[bpb], writes=b_y1[hs2])
                    yield
                    for i in range(2):
                        h = 2 * q + i
                        mm(pbv[:, i, :], NoT[:, h, :], y1[:, h, :], True, True, [b_NoT[h], b_y1[h]], [bpb])
                    P.op("dve", lambda e, hs2=hs2, pbv=pbv: e.tensor_tensor(out=R2[:, hs2, :], in0=pbv, in1=R[:, hs2, :], op=ALU.add),
                         reads=[bpb] + b_R[hs2], writes=b_R2[hs2])
                    yield
                    for i in range(2):
                        h = 2 * q + i
                        mm(pbv[:, i, :], Qb[:, h, :], R2[:, h, :], True, True, [b_Qb[h], b_R2[h]], [bpb])
                    P.op("act", lambda e, hs2=hs2, pbv=pbv: e.copy(out=sol[:, hs2, :], in_=pbv), reads=[bpb], writes=b_sol[hs2])
                    yield
                rr(*[g_(v_) for v_ in range(4)])
                for h in range(8):
                    tr(psT[:, h, :], sol[:, h, 128:256], ident[:], [b_sol[h], b_ident], [b_psT])
                P.op("act", lambda e: e.copy(out=wT, in_=psT[:]), reads=[b_psT], writes=b_wT)
                def g_(hh):
                    hs4 = slice(4 * hh, 4 * hh + 4)
                    for i in range(4):
                        h = 4 * hh + i
                        mm(psH[hh][:, i, :], qkvs[:, 8 + h, c0:c0 + 128], qkvs[:, h, c0:c0 + 128], True, True,
                           [b_qkvs[8 + h], b_qkvs[h]], [b_psH[hh]])
                    P.op("dve", lambda e, hh=hh, hs4=hs4: e.tensor_tensor(out=aT[:, hs4, :], in0=psH[hh][:], in1=ET[:, hs4, :], op=ALU.mult),
                         reads=[b_psH[hh], b_ET[hh]], writes=b_aT[hs4])
                    yield
                rr(*[g_(v_) for v_ in range(2)])
                def g_(hh):
                    hs4 = slice(4 * hh, 4 * hh + 4)
                    for i in range(4):
                        h = 4 * hh + i
                        mm(psH[2 + hh][:, i, :], wT[:, h, :], Sb[:, h, :], True, True, [b_wT[h], b_Sb[h]], [b_psH[2 + hh]])
                    P.op("dve", lambda e, hh=hh, hs4=hs4: e.tensor_tensor(out=vn[:, hs4, :], in0=sol[:, hs4, 0:128], in1=psH[2 + hh][:],
                                                                       op=ALU.subtract),
                         reads=b_sol[hs4] + [b_psH[2 + hh]], writes=b_vn[hs4])
                    yield
                    for i in range(4):
                        h = 4 * hh + i
                        mm(CB[hh][:, i * 128:(i + 1) * 128], qkvs[:, h, c0:c0 + 128], Sb[:, h, :], True, True, [b_qkvs[h], b_Sb[h]], [b_CB[hh]])
                    P.op("dve", lambda e, hh=hh, hs4=hs4: e.tensor_tensor(out=o1[:, hs4, :], in0=CB[hh][:].rearrange("p (a b) -> p a b", a=4),
                                                                       in1=b4(12, hh), op=ALU.mult),
                         reads=[b_CB[hh], b_sc], writes=b_o1[hs4])
                    yield
                    for i in range(4):
                        h = 4 * hh + i
                        mm(psH[hh][:, i, :], aT[:, h, :], vn[:, h, :], True, True, [b_aT[h], b_vn[h]], [b_psH[hh]])
                    P.op("dve", lambda e, hh=hh, hs4=hs4: e.tensor_tensor(out=och[:, hs4, :], in0=psH[hh][:], in1=b4(11, hh), op=ALU.mult),
                         reads=[b_psH[hh], b_sc], writes=b_och[hs4])
                    yield
                    P.op("dve", lambda e, hs4=hs4: e.tensor_tensor(out=och[:, hs4, :], in0=och[:, hs4, :], in1=o1[:, hs4, :], op=ALU.add),
                         reads=b_och[hs4] + b_o1[hs4], writes=b_och[hs4])
                    yield
                    for i in range(4):
                        h = 4 * hh + i
                        mm(psH[2 + hh][:, i, :], Ks2[:, h, :], vn[:, h, :], True, True, [b_Ks2, b_vn[h]], [b_psH[2 + hh]])
                    P.op("dve", lambda e, hh=hh, hs4=hs4: e.tensor_tensor(out=S[:, hs4, :], in0=S[:, hs4, :], in1=b4(6, hh), op=ALU.mult),
                         reads=b_S[hs4] + [b_sc], writes=b_S[hs4])
                    yield
                    P.op("dve", lambda e, hh=hh, hs4=hs4: e.tensor_tensor(out=S[:, hs4, :], in0=S[:, hs4, :], in1=psH[2 + hh][:], op=ALU.add),
                         reads=b_S[hs4] + [b_psH[2 + hh]], writes=b_S[hs4])
                    yield
                    P.op("act", lambda e, hs4=hs4: e.copy(out=Sb[:, hs4, :], in_=S[:, hs4, :]), reads=b_S[hs4], writes=b_Sb[hs4])
                    yield
                rr(*[g_(v_) for v_ in range(2)])
                oa = 0
                och_f = och[:].rearrange("p a b -> p (a b)")
                if cb == 0:
                    P.op("dve", lambda e, oa=oa: e.tensor_scalar(out=oacc[oa][:], in0=och_f, scalar1=selt[:, 0:1], scalar2=None, op0=ALU.mult),
                         reads=b_och + [b_sel], writes=[b_oacc[oa]])
                else:
                    P.op("dve", lambda e, oa=oa, cb=cb: e.scalar_tensor_tensor(out=oacc[oa][:], in0=och_f, scalar=selt[:, cb:cb + 1],
                                                                              in1=oacc[oa][:], op0=ALU.mult, op1=ALU.add),
                         reads=b_och + [b_sel, b_oacc[oa]], writes=[b_oacc[oa]])
                if cb == 3:
                    P.dma("sp", lambda e, oa=oa, g=g: e.dma_start(out=oown_d[g * 128:(g + 1) * 128, :], in_=oacc[oa][:]),
                          reads=[b_oacc[oa]], track=b_oacc[oa], is_out=("dbg_o" in DEBUG))

            S1(0, (4 * g) % 2)
            for cb in range(4):
                par = (4 * g + cb) % 2
                la = P.record(lambda: S234(cb, par))
                if cb < 3:
                    lb = P.record(lambda: S1(cb + 1, 1 - par))
                else:
                    lb = P.record(lambda: front(g + 1)) if g + 1 < NCH // 4 else []
                na, nb = len(la), len(lb)
                ia = ib = 0
                cur = 0
                while ia < na or ib < nb:
                    want = 0 if (ib >= nb or (ia < na and ia * nb <= ib * na)) else 1
                    if cur == 0 and ia > 0 and ia < na and la[ia - 1][1][0] == "pe":
                        want = 0
                    elif cur == 1 and ib > 0 and ib < nb and lb[ib - 1][1][0] == "pe":
                        want = 1
                    if want == 0:
                        P.replay(la[ia:ia + 1]); ia += 1
                    else:
                        P.replay(lb[ib:ib + 1]); ib += 1
                    cur = want
        P.flush()
    P.stack = gst


def make_in_maps(inputs):
    f = lambda a: np.ascontiguousarray(np.asarray(a, dtype=np.float32))
    x = f(inputs["x"]); c = f(inputs["c"])
    shared = {
        "w_ada": f(inputs["w_ada"][0]), "b_ada": f(inputs["b_ada"][0]), "norm1_w": f(inputs["norm1_w"][0]),
        "w_in": f(inputs["w_in"][0]), "dn_conv_w": f(inputs["dn_conv_w"][0].reshape(4, 24, 128).transpose(2, 1, 0)), "dn_a_log": f(inputs["dn_a_log"][0]),
        "dn_dt_bias": f(inputs["dn_dt_bias"][0]), "dn_onorm_w": f(inputs["dn_onorm_w"][0]),
        "q_norm_w": f(inputs["q_norm_w"][0]), "kv_norm_w": f(inputs["kv_norm_w"][0]),
        "idx_k_norm_w": f(inputs["idx_k_norm_w"][0]), "w_uq": f(inputs["w_uq"][0]), "w_iq": f(inputs["w_iq"][0]),
        "w_uk": f(inputs["w_uk"][0].reshape(256, 1024)), "w_uv": f(inputs["w_uv"][0].reshape(256, 1024)),
        "w_a_out": f(inputs["w_a_out"][0]), "w_b_out": f(inputs["w_b_out"][0]), "w_o": f(inputs["w_o"][0]),
        "norm2_w": f(inputs["norm2_w"][0]), "peer_w_q": f(inputs["peer_w_q"][0]),
        "peer_sub_keys": f(inputs["peer_sub_keys"][0].reshape(16, 128, 128)),
        "peer_u": f(inputs["peer_u"][0]), "peer_v": f(inputs["peer_v"][0]), "final_norm_w": f(inputs["final_norm_w"]),
    }
    maps = []
    for k in range(8):
        b, r = k // 4, k % 4
        xb = x[b]
        xo = np.ascontiguousarray(xb.reshape(16, 4, 128, D)[:, r].reshape(NOWN * 128, D))
        sel = np.zeros((128, 4), np.float32); sel[:, r] = 1.0
        qp = np.arange(128)[:, None] + 128 * r
        kp = np.arange(512)[None, :]
        cm = np.where(kp <= qp, 0.0, -1e30).astype(np.float32)
        m = dict(shared)
        m.update({"xf": xb, "xo": xo, "cT": np.ascontiguousarray(c[b].reshape(8, 128).T), "sel": sel, "cmask": cm})
        maps.append(m)
    return maps


def kernel(**inputs):
    nc = build()
    maps = make_in_maps(inputs)
    res = run_bass_kernel_spmd(nc, maps, core_ids=list(range(8)))
    out = np.zeros((2, L, D), np.float32)
    for k in range(8):
        b, r = k // 4, k % 4
        o = np.asarray(res.results[k]["out"]).reshape(16, 128, D)
        out[b].reshape(16, 4, 128, D)[:, r] = o
    return out


def phase3a(P, nc, dr, G):
    gst = P.stack
    ident, b_ident, onesf, b_onesf = G["ident"], G["b_ident"], G["onesf"], G["b_onesf"]
    normmod = G["normmod"]
    ckv_tm_d, ckvT_d, kidxT_d, yb_d = G["ckv_tm_d"], G["ckvT_d"], G["kidxT_d"], G["yb_d"]
    with ExitStack() as st:
        P.stack = st
        modrow = P.sb("modrow_3", [128, 2, D], F32); b_mod = Buf("modrow_3")
        P.dma("sp", lambda e: e.dma_start(out=modrow[:].rearrange("p a d -> p (a d)"), in_=G["mod_d"][0:2 * D].partition_broadcast(128)), writes=[b_mod])
        G["MR"]["t"], G["MR"]["b"] = modrow, b_mod
        win_v = dr["w_in"].rearrange("(k p) n -> p k n", p=128)
        wql = P.sb("wql", [128, 8, 264], BF16); b_wql = Buf("wql")
        P.dma("pool", lambda e: e.dma_start(out=wql[:, :, 0:256], in_=win_v[:, :, C_QL:C_QL + 256]), writes=[b_wql])
        P.dma("pool", lambda e: e.dma_start(out=wql[:, :, 256:264], in_=win_v[:, :, C_WI:C_WI + 8]), writes=[b_wql])
        wuq = P.sb("wuq", [128, 2, 1024], BF16); b_wuq = Buf("wuq")
        wiq = P.sb("wiq", [128, 2, 512], BF16); b_wiq = Buf("wiq")
        wuk = P.sb("wuk", [128, 2, 1024], BF16); b_wuk = Buf("wuk")
        wuv = P.sb("wuv", [128, 2, 1024], BF16); b_wuv = Buf("wuv")
        wbs = P.sb("wbs", [128, 8, 512], BF16); b_wbs = Buf("wbs")
        wukT = P.sb("wukT", [128, 8, 256], BF16); b_wukT = Buf("wukT")
        for (t_, b_, nm) in ((wuq, b_wuq, "w_uq"), (wiq, b_wiq, "w_iq"), (wuk, b_wuk, "w_uk"), (wuv, b_wuv, "w_uv")):
            P.dma("pool", lambda e, t_=t_, nm=nm: e.dma_start(out=t_[:], in_=dr[nm].rearrange("(k p) n -> p k n", p=128)), writes=[b_])
        wb_d = G["wb_d"]
        b_wbd = Buf("wbd")
        v_ = dr["w_b_out"].rearrange("(k p) n -> p k n", p=128)
        for ci in (14, 15):
            P.dma("pool", lambda e, ci=ci, v_=v_: e.dma_start(out=wb_d[ci], in_=v_[:, :, (ci - 14) * 512:(ci - 13) * 512]), writes=[b_wbd], track=b_wbd)
        wsrc = [(win_v, C_Z), (win_v, C_Z + 512), (win_v, C_GA), (win_v, C_GA + 512), (win_v, C_GB), (win_v, C_GB + 512)]
        for nm_ in ("w_a_out", "w_o"):
            v_ = dr[nm_].rearrange("(k p) n -> p k n", p=128)
            wsrc += [(v_, 0), (v_, 512)]
        v_ = dr["peer_w_q"].rearrange("(k p) n -> p k n", p=128)
        wsrc += [(v_, 0), (v_, 512), (v_, 1024), (v_, 1536)]
        for ci, (v_, c0_) in enumerate(wsrc):
            P.dma("pool", lambda e, ci=ci, v_=v_, c0_=c0_: e.dma_start(out=wb_d[ci], in_=v_[:, :, c0_:c0_ + 512]), writes=[b_wbd], track=b_wbd)
        kidxT = P.sb("kidxT", [64, L], BF16); b_kidxT = Buf("kidxT")
        for i in range(4):
            P.dma("sp", lambda e, i=i: e.dma_start(out=kidxT[:, i * 2048:(i + 1) * 2048], in_=kidxT_d[:, i * 2048:(i + 1) * 2048]),
                  writes=[b_kidxT])
        qnrow = P.sb("qnrow", [128, 256], F32); b_qnrow = Buf("qnrow")
        P.dma("sp", lambda e: e.dma_start(out=qnrow[:], in_=dr["q_norm_w"].partition_broadcast(128)), writes=[b_qnrow])
        cmask = P.sb("cmaskt", [128, 512], F32); b_cmask = Buf("cmask")
        P.dma("sp", lambda e: e.dma_start(out=cmask[:], in_=dr["cmask"]), writes=[b_cmask])
        onesb = P.sb("onesb", [128, 128], BF16); b_onesb = Buf("onesb")
        P.op("dve", lambda e: e.tensor_copy(out=onesb[:], in_=onesf[:]), reads=[b_onesf], writes=[b_onesb])

        xt = P.sb("xt3", [128, D], F32); b_xt = Buf("xt3")
        sq = P.sb("sq3", [128, D], F32); b_sq = Buf("sq3")
        stt = P.sb("stt3", [128, 8], F32); b_stt = Buf("stt3")
        hb = P.sb("hb3", [128, D], BF16); b_hb = Buf("hb3")
        hT = P.sb("hT3", [128, 8, 128], BF16); b_hT = Buf("hT3")
        ql = P.sb("ql", [128, 264], F32); b_ql = Buf("ql")
        qln = P.sb("qln", [128, 256], BF16); b_qln = Buf("qln")
        wi = P.sb("wi", [128, 8], F32); b_wi = Buf("wi")
        qlT = P.sb("qlT", [128, 2, 128], BF16); b_qlT = Buf("qlT")
        qT = P.sb("qT", [128, 8, 128], BF16); b_qT = Buf("qT")
        qaTs = [P.sb("qaT%d" % i, [128, 2, 8, 128], BF16) for i in range(2)]; b_qaTs = bufs("qaT", 2)
        qiT = P.sb("qiT", [64, 8, 128], BF16); b_qiT = Buf("qiT")
        score = P.sb("score", [128, L], F32); b_score = Buf("score")
        wk = P.sb("wk", [128, L], F32); b_wk = Buf("wk")
        mk = wk[:, :].bitcast(BF16)
        rl = [P.sb("rl%d" % i, [128, 512], F32) for i in range(2)]; b_rl = bufs("rl", 2)
        KIT = 32
        pw = P.sb("pw", [128, KIT], F32); b_pw = Buf("pw")
        for k in range(KIT):
            P.op("pool", lambda e, k=k: e.memset(pw[:, k:k + 1], 2.0 ** -(k + 1)), reads=[b_pw], writes=[b_pw])
        bs = P.sb("bs", [128, 8], F32); b_bs = Buf("bs")
        steps = P.sb("steps", [128, 2, KIT], F32); b_steps = Buf("steps")
        cnt = P.sb("cnt", [128, KIT], F32); b_cnt = Buf("cnt")
        thr = P.sb("thr", [128, 1], F32); b_thr = Buf("thr")
        maskT = P.sb("maskT", [128, 64, 128], BF16); b_maskT = Buf("maskT")
        ckT = [P.sb("ckT%d" % i, [128, 2, 512], BF16) for i in range(2)]; b_ckT = bufs("ckT", 2)
        ckM = [P.sb("ckM%d" % i, [128, 4, 256], BF16) for i in range(2)]; b_ckM = bufs("ckM", 2)
        pe_ = [P.sb("pe%d" % i, [128, 512], BF16) for i in range(2)]; b_pe = bufs("pe", 2)
        pm_ = [P.sb("pmk%d" % i, [128, 4, 128], BF16) for i in range(2)]; b_pm = bufs("pmm", 2)
        rs_t = P.sb("rs_t", [128, 512], F32); rs = rs_t[:, :]; b_rs = Buf("rs_t")
        rlB = [P.sb("rlB%d" % i, [128, 512], F32) for i in range(2)]; b_rlB = bufs("rlB", 2)
        olT = wuk[:, :, :].rearrange("p a (h b) -> p a h b", h=8); b_olT = b_wuk
        oT = qT; b_oT = b_qT
        yb = xt; b_yb = b_xt
        psS = P.ps("ps3S", [128, 512], F32); b_psS = Buf("ps3S")
        psT = P.ps("ps3T", [128, 8, 128], BF16); b_psT = Buf("ps3T")
        psL = [P.ps("ps3L%d" % i, [128, 512], F32) for i in range(2)]; b_psL = bufs("ps3L", 2)
        psO = [P.ps("ps3O%d" % i, [128, 512], F32) for i in range(2)]; b_psO = bufs("ps3O", 2)
        psM = P.ps("ps3M", [128, 512], F32); b_psM = Buf("ps3M")
        psX = P.ps("ps3X", [128, 512], F32); b_psX = Buf("ps3X")
        psQ = [psS, psX]; b_psQ = [b_psS, b_psX]

        def mm(out, lhsT, rhs, start, stop, rd, wr):
            P.op("pe", lambda e: e.matmul(out, lhsT=lhsT, rhs=rhs, start=start, stop=stop), reads=rd, writes=wr)

        def tr(out, in_, idn, rd, wr):
            P.op("pe", lambda e: e.transpose(out=out, in_=in_, identity=idn), reads=rd, writes=wr)

        for h in range(8):
            for cc in range(2):
                tr(psT[:, cc, :], wuk[:, cc, h * 128:(h + 1) * 128], ident[:], [b_wuk, b_ident], [b_psT])
            P.op("act", lambda e, h=h: e.copy(out=wukT[:, h, :].rearrange("p (a b) -> p a b", a=2), in_=psT[:, 0:2, :]),
                 reads=[b_psT], writes=[b_wukT])

        def A1(j):
            NK = 4 * j + 4
            NKC = j + 1
            qaT, b_qaT = qaTs[j % 2], b_qaTs[j % 2]
            P.dma("sp", lambda e, j=j: e.dma_start(out=xt[:], in_=dr["xo"][j * 128:(j + 1) * 128, :]), writes=[b_xt])
            normmod(xt[:], b_xt, hb[:], b_hb, sq[:], b_sq, stt, b_stt, 1, 0)
            for k in range(8):
                tr(psT[:, k, :], hb[:, k * 128:(k + 1) * 128], ident[:], [b_hb, b_ident], [b_psT])
            P.op("act", lambda e: e.copy(out=hT[:], in_=psT[:]), reads=[b_psT], writes=[b_hT])
            for k in range(8):
                mm(psS[:, 0:264], hT[:, k, :], wql[:, k, :], k == 0, k == 7, [b_hT, b_wql], [b_psS])
            P.op("act", lambda e: e.copy(out=ql[:], in_=psS[:, 0:264]), reads=[b_psS], writes=[b_ql])
            P.op("pool", lambda e: e.memset(stt[:, 4:5], 0.0), writes=[b_stt])
            P.op("act", lambda e: e.activation(out=sq[:, 0:256], in_=ql[:, 0:256], func=AF.Square, accum_out=stt[:, 4:5]),
                 reads=[b_ql, b_stt], writes=[b_sq, b_stt])
            P.op("act", lambda e: e.activation(out=stt[:, 5:6], in_=stt[:, 4:5], func=AF.Ln, scale=1.0 / 256, bias=EPS),
                 reads=[b_stt], writes=[b_stt])
            P.op("act", lambda e: e.activation(out=stt[:, 6:7], in_=stt[:, 5:6], func=AF.Exp, scale=-0.5), reads=[b_stt], writes=[b_stt])
            P.op("dve", lambda e: e.scalar_tensor_tensor(out=qln[:], in0=ql[:, 0:256], scalar=stt[:, 6:7], in1=qnrow[:],
                                                          op0=ALU.mult, op1=ALU.mult), reads=[b_ql, b_stt, b_qnrow], writes=[b_qln])
            P.op("dve", lambda e: e.tensor_scalar(out=wi[:], in0=ql[:, 256:264], scalar1=(8.0 ** -0.5) * (64.0 ** -0.5), scalar2=None,
                                                   op0=ALU.mult), reads=[b_ql], writes=[b_wi])
            for cc in range(2):
                tr(psT[:, cc, :], qln[:, cc * 128:(cc + 1) * 128], ident[:], [b_qln, b_ident], [b_psT])
            P.op("act", lambda e: e.copy(out=qlT[:], in_=psT[:, 0:2, :]), reads=[b_psT], writes=[b_qlT])
            for h in range(8):
                po, bpo = psQ[h // 4], b_psQ[h // 4]
                for cc in range(2):
                    mm(po[:, (h % 4) * 128:(h % 4 + 1) * 128], wuq[:, cc, h * 128:(h + 1) * 128], qlT[:, cc, :], cc == 0, cc == 1,
                       [b_wuq, b_qlT], [bpo])
            for hh in range(2):
                P.op("act", lambda e, hh=hh: e.activation(out=qT[:, 4 * hh:4 * hh + 4, :].rearrange("p a b -> p (a b)"), in_=psQ[hh][:],
                                                          func=AF.Copy, scale=128.0 ** -0.5), reads=[b_psQ[hh]], writes=[b_qT])
            for cc in range(2):
                for h in range(8):
                    po, bpo = psQ[h // 4], b_psQ[h // 4]
                    mm(po[:, (h % 4) * 128:(h % 4 + 1) * 128], wukT[:, h, cc * 128:(cc + 1) * 128], qT[:, h, :], True, True,
                       [b_wukT, b_qT], [bpo])
                for hh in range(2):
                    P.op("act", lambda e, hh=hh, cc=cc: e.copy(out=qaT[:, cc, 4 * hh:4 * hh + 4, :].rearrange("p a b -> p (a b)"),
                                                               in_=psQ[hh][:]), reads=[b_psQ[hh]], writes=[b_qaT])
            for h in range(8):
                po, bpo = psQ[h // 4], b_psQ[h // 4]
                for cc in range(2):
                    mm(po[0:64, (h % 4) * 128:(h % 4 + 1) * 128], wiq[:, cc, h * 64:(h + 1) * 64], qlT[:, cc, :], cc == 0, cc == 1,
                       [b_wiq, b_qlT], [bpo])
            for hh in range(2):
                P.op("act", lambda e, hh=hh: e.copy(out=qiT[:, 4 * hh:4 * hh + 4, :].rearrange("p a b -> p (a b)"), in_=psQ[hh][0:64, :]),
                     reads=[b_psQ[hh]], writes=[b_qiT])
            n = 0
            for kc in range(NKC):
                sl = score[:, kc * 512:(kc + 1) * 512]
                for h in range(8):
                    p = n % 2; n += 1
                    mm(psQ[p][:], qiT[:, h, :], kidxT[:, kc * 512:(kc + 1) * 512], True, True, [b_qiT, b_kidxT], [b_psQ[p]])
                    P.op("act", lambda e, p=p: e.activation(out=rl[p][:], in_=psQ[p][:], func=AF.Relu), reads=[b_psQ[p]], writes=[b_rl[p]])
                    if h == 0:
                        P.op("dve", lambda e, p=p, sl=sl: e.tensor_scalar(out=sl, in0=rl[p][:], scalar1=wi[:, 0:1], scalar2=None, op0=ALU.mult),
                             reads=[b_rl[p], b_wi], writes=[b_score])
                    else:
                        P.op("dve", lambda e, p=p, sl=sl, h=h: e.scalar_tensor_tensor(out=sl, in0=rl[p][:], scalar=wi[:, h:h + 1], in1=sl,
                                                                                   op0=ALU.mult, op1=ALU.add),
                             reads=[b_rl[p], b_wi, b_score], writes=[b_score])
                if kc == NKC - 1:
                    P.op("dve", lambda e, NK=NK: e.reduce_max(out=bs[:, 0:1], in_=score[:, 0:NK * 128], axis=AX.X, apply_absolute_value=True),
                         reads=[b_score, b_bs], writes=[b_bs])
                    P.op("dve", lambda e, sl=sl: e.tensor_tensor(out=sl, in0=sl, in1=cmask[:], op=ALU.add),
                         reads=[b_score, b_cmask], writes=[b_score])
        def A2(j):
            NK = 4 * j + 4
            W = NK * 128
            P.op("dve", lambda e: e.tensor_scalar(out=bs[:, 1:2], in0=bs[:, 0:1], scalar1=2.0, scalar2=2.0, op0=ALU.mult, op1=ALU.add),
                 reads=[b_bs], writes=[b_bs])
            P.op("dve", lambda e: e.tensor_scalar(out=bs[:, 2:3], in0=bs[:, 0:1], scalar1=-1.0, scalar2=-1.0, op0=ALU.mult, op1=ALU.add),
                 reads=[b_bs], writes=[b_bs])
            P.op("dve", lambda e: e.tensor_scalar(out=steps[:, 0, :], in0=pw[:], scalar1=bs[:, 1:2], scalar2=None, op0=ALU.mult),
                 reads=[b_pw, b_bs], writes=[b_steps])
            P.op("dve", lambda e: e.memset(cnt[:], 0.0), reads=[b_cnt], writes=[b_cnt])
            P.op("dve", lambda e: e.tensor_tensor(out=bs[:, 3:4], in0=bs[:, 2:3], in1=steps[:, 0, 0:1], op=ALU.add),
                 reads=[b_bs, b_steps], writes=[b_bs])
            for k in range(KIT):
                P.op("dve", lambda e, k=k, W=W: e.tensor_scalar(out=mk[:, 0:W], in0=score[:, 0:W], scalar1=bs[:, 3:4], scalar2=0.0,
                                                               op0=ALU.is_gt, op1=ALU.add, accum_out=cnt[:, k:k + 1]),
                     reads=[b_score, b_bs, b_cnt], writes=[b_wk, b_cnt])
                P.op("dve", lambda e, k=k: e.tensor_scalar(out=bs[:, 5:6], in0=cnt[:, k:k + 1], scalar1=255.5, scalar2=None, op0=ALU.is_gt),
                     reads=[b_cnt, b_bs], writes=[b_bs])
                P.op("dve", lambda e, k=k: e.scalar_tensor_tensor(out=bs[:, 2:3], in0=bs[:, 5:6], scalar=steps[:, 0, k:k + 1], in1=bs[:, 2:3],
                                                                  op0=ALU.mult, op1=ALU.add), reads=[b_bs, b_steps], writes=[b_bs])
                if k + 1 < KIT:
                    P.op("dve", lambda e, k=k: e.tensor_tensor(out=bs[:, 3:4], in0=bs[:, 2:3], in1=steps[:, 0, k + 1:k + 2], op=ALU.add),
                         reads=[b_bs, b_steps], writes=[b_bs])
        def A3(j):
            NK = 4 * j + 4
            W = NK * 128
            P.op("dve", lambda e, W=W: e.tensor_scalar(out=mk[:, 0:W], in0=score[:, 0:W], scalar1=bs[:, 2:3], scalar2=None, op0=ALU.is_gt),
                 reads=[b_score, b_bs], writes=[b_wk])
            for kb0 in range(0, NK, 8):
                nb = min(8, NK - kb0)
                for i in range(nb):
                    tr(psT[:, i, :], mk[:, (kb0 + i) * 128:(kb0 + i + 1) * 128], ident[:], [b_wk, b_ident], [b_psT])
                P.op("act", lambda e, kb0=kb0, nb=nb: e.activation(out=maskT[:, kb0:kb0 + nb, :], in_=psT[:, 0:nb, :], func=AF.Identity,
                                                                  scale=30000.0, bias=-30000.0), reads=[b_psT], writes=[b_maskT])
        def B(j):
            NK = 4 * j + 4
            NKC = j + 1
            qaT, b_qaT = qaTs[j % 2], b_qaTs[j % 2]
            ld = 0
            for hh in range(2):
                for kc in range(NKC):
                    s_ = ld % 2; ld += 1
                    P.dma("sp", lambda e, s_=s_, kc=kc: e.dma_start(
                        out=ckT[s_][:], in_=ckvT_d[:, kc * 512:(kc + 1) * 512].rearrange("(a p) t -> p a t", p=128)), writes=[b_ckT[s_]])
                    P.dma("sp", lambda e, s_=s_, kc=kc: e.dma_start(
                        out=ckM[s_][:], in_=ckv_tm_d[kc * 512:(kc + 1) * 512, :].rearrange("(a p) c -> p a c", p=128)), writes=[b_ckM[s_]])
                    for i in range(4):
                        kb = kc * 4 + i
                        p = kb % 2
                        for cc in range(2):
                            mm(psL[p][:], ckT[s_][:, cc, i * 128:(i + 1) * 128], qaT[:, cc, 4 * hh:4 * hh + 4, :].rearrange("p a b -> p (a b)"),
                               cc == 0, False, [b_ckT[s_], b_qaT], [b_psL[p]])
                        mm(psL[p][:].rearrange("p (a b) -> p a b", a=4), ident[:], maskT[:, kb, :].unsqueeze(1).to_broadcast([128, 4, 128]),
                           False, True, [b_ident, b_maskT], [b_psL[p]])
                        P.op("act", lambda e, p=p: e.activation(out=pm_[p][:].rearrange("p a b -> p (a b)"), in_=psL[p][:], func=AF.Exp),
                             reads=[b_psL[p]], writes=[b_pm[p]])
                        pmf = pm_[p][:].rearrange("p a b -> p (a b)")
                        for cc in range(2):
                            mm(psO[cc][:], ckM[s_][:, i, cc * 128:(cc + 1) * 128], pmf, kb == 0, kb == NK - 1, [b_ckM[s_], b_pm[p]], [b_psO[cc]])
                        mm(psM[:], onesb[:], pmf, kb == 0, kb == NK - 1, [b_onesb, b_pm[p]], [b_psM])
                P.op("act", lambda e: e.activation(out=rs, in_=psM[:], func=AF.Ln), reads=[b_psM], writes=[b_rs])
                P.op("act", lambda e: e.activation(out=rs, in_=rs, func=AF.Exp, scale=-1.0), reads=[b_rs], writes=[b_rs])
                for cc in range(2):
                    P.op("act", lambda e, cc=cc: e.copy(out=rlB[cc][:], in_=psO[cc][:]), reads=[b_psO[cc]], writes=[b_rlB[cc]])
                    P.op("pool", lambda e, cc=cc, hh=hh: e.tensor_tensor(out=olT[:, cc, 4 * hh:4 * hh + 4, :].rearrange("p a b -> p (a b)"),
                                                                      in0=rlB[cc][:], in1=rs, op=ALU.mult),
                         reads=[b_rlB[cc], b_rs], writes=[b_olT])
            for h in range(8):
                po, bpo = psO[h // 4], b_psO[h // 4]
                for cc in range(2):
                    mm(po[:, (h % 4) * 128:(h % 4 + 1) * 128], wuv[:, cc, h * 128:(h + 1) * 128], olT[:, cc, h, :], cc == 0, cc == 1,
                       [b_wuv, b_olT], [bpo])
            for hh in range(2):
                P.op("act", lambda e, hh=hh: e.copy(out=oT[:, 4 * hh:4 * hh + 4, :].rearrange("p a b -> p (a b)"), in_=psO[hh][:]),
                     reads=[b_psO[hh]], writes=[b_oT])
            for half in range(2):
                P.dma("sp", lambda e, half=half: e.dma_start(out=wbs[:], in_=wb_d[14 + half]), reads=[b_wbd], writes=[b_wbs])
                for h in range(8):
                    mm(psL[half][:], oT[:, h, :], wbs[:, h, :], h == 0, h == 7, [b_oT, b_wbs], [b_psL[half]])
                P.op("act", lambda e, half=half: e.copy(out=yb[:, half * 512:(half + 1) * 512], in_=psL[half][:]),
                     reads=[b_psL[half]], writes=[b_yb])
            P.dma("sp", lambda e, j=j: e.dma_start(out=yb_d[j * 128:(j + 1) * 128, :], in_=yb[:]), reads=[b_yb], track=b_yb,
                  is_out=("dbg_yb" in DEBUG))
        def merge(la, lb):
            na, nb = len(la), len(lb)
            ia = ib = 0
            while ia < na or ib < nb:
                if ib >= nb or (ia < na and ia * nb <= ib * na):
                    P.replay(la[ia:ia + 1]); ia += 1
                else:
                    P.replay(lb[ib:ib + 1]); ib += 1

        A1(0); A2(0); A3(0)
        for j in range(NOWN):
            if j + 1 < NOWN:
                A1(j + 1); A2(j + 1)
            B(j)
            if j + 1 < NOWN:
                A3(j + 1)
        P.flush()
    P.stack = gst


def phase3b(P, nc, dr, G):
    gst = P.stack
    ident, b_ident, onesf, b_onesf = G["ident"], G["b_ident"], G["onesf"], G["b_onesf"]
    normmod = G["normmod"]
    oown_d, yb_d, out_d, uv_d = G["oown_d"], G["yb_d"], G["out_d"], G["uv_d"]
    with ExitStack() as st:
        P.stack = st
        modrow = P.sb("modrow_4", [128, 6, D], F32); b_mod = Buf("modrow_4")
        P.dma("sp", lambda e: e.dma_start(out=modrow[:].rearrange("p a d -> p (a d)"), in_=G["mod_d"][0:6 * D].partition_broadcast(128)), writes=[b_mod])
        G["MR"]["t"], G["MR"]["b"] = modrow, b_mod
        win_v = dr["w_in"].rearrange("(k p) n -> p k n", p=128)
        wb_d = G["wb_d"]
        widx = {"z": 0, "ga": 2, "gb": 4, "ao": 6, "wo": 8, "pq": 10}
        wst = [P.sb("wst%d" % i, [128, 8, 512], BF16) for i in range(3)]; b_wst = bufs("wst", 3)
        wctr = [0]

        def wload(name, half):
            s_ = wctr[0] % 3; wctr[0] += 1
            ci = widx[name] + half
            P.dma("sp", lambda e: e.dma_start(out=wst[s_][:], in_=wb_d[ci]), writes=[b_wst[s_]])
            return wst[s_], b_wst[s_]

        skn = P.sb("skn", [128, 16, 128], BF16); b_skn = Buf("skn")
        P.dma("pool", lambda e: e.dma_start(out=skn[:], in_=dr["peer_sub_keys"].rearrange("a n d -> n a d")), writes=[b_skn])
        skT = P.sb("skT", [128, 16, 128], BF16); b_skT = Buf("skT")
        rows = P.sb("rows3", [128, 128 + D], F32); b_rows = Buf("rows3")
        P.dma("sp", lambda e: e.dma_start(out=rows[:, 0:128], in_=dr["dn_onorm_w"].partition_broadcast(128)), writes=[b_rows])
        P.dma("sp", lambda e: e.dma_start(out=rows[:, 128:128 + D], in_=dr["final_norm_w"].partition_broadcast(128)), writes=[b_rows])
        iota = P.sb("iota", [128, 16], F32); b_iota = Buf("iota")
        thr16 = P.sb("thr16", [128, 16], F32); b_thr16 = Buf("thr16")
        for i in range(16):
            P.op("pool", lambda e, i=i: e.memset(iota[:, i:i + 1], float(i)), reads=[b_iota], writes=[b_iota])
            P.op("pool", lambda e, i=i: e.memset(thr16[:, i:i + 1], float(16 * (i + 1)) if i < 15 else 1.0e9), reads=[b_thr16], writes=[b_thr16])

        xt = P.sb("xt4", [128, D], F32); b_xt = Buf("xt4")
        sq = P.sb("sq4", [128, D], F32); b_sq = Buf("sq4")
        stt = P.sb("stt4", [128, 24], F32); b_stt = Buf("stt4")
        hb = P.sb("hb4", [128, D], BF16); b_hb = Buf("hb4")
        hT = P.sb("hT4", [128, 8, 128], BF16); b_hT = Buf("hT4")
        zs = P.sb("zs", [128, D], F32); b_zs = Buf("zs")
        ot = P.sb("ot", [128, D], F32); b_ot = Buf("ot")
        og = P.sb("og", [128, D], BF16); b_og = Buf("og")
        ogT = P.sb("ogT", [128, 8, 128], BF16); b_ogT = Buf("ogT")
        sg = P.sb("sg", [128, 512], F32); b_sg = Buf("sg")
        ybt = P.sb("ybt", [128, D], F32); b_ybt = Buf("ybt")
        mt = P.sb("mt", [128, D], F32); b_mt = Buf("mt")
        mb = P.sb("mb", [128, D], BF16); b_mb = Buf("mb")
        x1s = [P.sb("x1_%d" % i, [128, D], F32) for i in range(3)]; b_x1s = bufs("x1_", 3)
        h2f = P.sb("h2f", [128, D], F32); b_h2f = Buf("h2f")
        qpT = P.sb("qpT", [128, 16, 128], BF16); b_qpT = Buf("qpT")
        scss = [P.sb("scs%d" % i, [128, 16, 128], F32) for i in range(2)]; b_scss = bufs("scs", 2)
        scw = P.sb("scw", [128, 256], F32); b_scw = Buf("scw")
        sv = P.sb("sv", [128, 16, 16], F32); b_sv = Buf("sv")
        si = P.sb("si", [128, 16, 16], U32); b_si = Buf("si")
        sif = P.sb("sif", [128, 16, 16], F32); b_sif = Buf("sif")
        cand = P.sb("cand", [128, 8, 256], F32); b_cand = Buf("cand")
        cv = P.sb("cv", [128, 8, 16], F32); b_cv = Buf("cv")
        cp = P.sb("cp", [128, 8, 16], U32); b_cp = Buf("cp")
        ca = P.sb("ca", [128, 2, 8, 16], U32); b_ca = Buf("ca")
        caf = P.sb("caf", [128, 2, 8, 16], F32); b_caf = Buf("caf")
        oh = P.sb("oh", [128, 16, 16], F32); b_oh = Buf("oh")
        ij = P.sb("ij", [128, 2, 8, 16], F32); b_ij = Buf("ij")
        eidf = P.sb("eidf", [128, 128], F32); b_eidf = Buf("eidf")
        eids = [P.sb("eid%d" % i, [128, 128], I32) for i in range(2)]; b_eids = bufs("eid", 2)
        gtss = [P.sb("gts%d" % i, [128, 8, 16], F32) for i in range(2)]; b_gtss = bufs("gts", 2)
        act = P.sb("actp", [128, 128], F32); b_act = Buf("actp")
        coef = P.sb("coef", [128, 128], F32); b_coef = Buf("coef")
        NUG = 6
        ug = [P.sb("ug%d" % i, [128, 2 * D], BF16) for i in range(NUG)]; b_ug = bufs("ug", NUG)
        dg = [P.sb("dg%d" % i, [128, 128], BF16) for i in range(4)]; b_dg = bufs("dg", 4)
        jb = P.sb("jb", [128, D], BF16); b_jb = Buf("jb")
        h2bs = [P.sb("h2b%d" % i, [128, D], BF16) for i in range(3)]; b_h2bs = bufs("h2b", 3)
        sq_t = P.sb("sq_t", [128, D], F32); b_sq_t = Buf("sq_t")
        stts = P.sb("stts", [128, 24], F32); b_stts = Buf("stts")
        stt_t = P.sb("stt_t", [128, 4], F32); b_stt_t = Buf("stt_t")
        mt_t = P.sb("mt_t", [128, D], F32); b_mt_t = Buf("mt_t")
        acc = P.sb("acc", [128, D], F32); b_acc = Buf("acc")
        psA = [P.ps("ps4A%d" % i, [128, 512], F32) for i in range(4)]; b_psA = bufs("ps4A", 4)
        psB = [P.ps("ps4B%d" % i, [128, 512], F32) for i in range(2)]; b_psB = bufs("ps4B", 2)
        psT = P.ps("ps4T", [128, 8, 128], BF16); b_psT = Buf("ps4T")
        psS = P.ps("ps4S", [128, 512], F32); b_psS = Buf("ps4S")

        def mm(out, lhsT, rhs, start, stop, rd, wr):
            P.op("pe", lambda e: e.matmul(out, lhsT=lhsT, rhs=rhs, start=start, stop=stop), reads=rd, writes=wr)

        def tr(out, in_, idn, rd, wr):
            P.op("pe", lambda e: e.transpose(out=out, in_=in_, identity=idn), reads=rd, writes=wr)

        def transpose8(src, bsrc, dst, bdst):
            for k in range(8):
                tr(psT[:, k, :], src[:, k * 128:(k + 1) * 128], ident[:], [bsrc, b_ident], [b_psT])
            P.op("act", lambda e: e.copy(out=dst[:], in_=psT[:]), reads=[b_psT], writes=[bdst])

        for a in range(0, 16, 8):
            for i in range(8):
                tr(psT[:, i, :], skn[:, a + i, :], ident[:], [b_skn, b_ident], [b_psT])
            P.op("act", lambda e, a=a: e.copy(out=skT[:, a:a + 8, :], in_=psT[:]), reads=[b_psT], writes=[b_skT])

        def top16(src_ap, bsrc, vals, idxs, work, extra_w):
            P.op("dve", lambda e: e.max(out=vals[:, 0:8], in_=src_ap), reads=[bsrc] + extra_w, writes=extra_w)
            P.op("dve", lambda e: e.max_index(out=idxs[:, 0:8], in_max=vals[:, 0:8], in_values=src_ap), reads=[bsrc] + extra_w, writes=extra_w)
            P.op("dve", lambda e: e.match_replace(out=work, in_to_replace=vals[:, 0:8], in_values=src_ap, imm_value=-3.0e38),
                 reads=[bsrc] + extra_w, writes=[b_scw])
            P.op("dve", lambda e: e.max(out=vals[:, 8:16], in_=work), reads=[b_scw] + extra_w, writes=extra_w)
            P.op("dve", lambda e: e.max_index(out=idxs[:, 8:16], in_max=vals[:, 8:16], in_values=work), reads=[b_scw] + extra_w, writes=extra_w)

        def mixer(j):
            x1, b_x1, h2b, b_h2b = x1s[j % 3], b_x1s[j % 3], h2bs[j % 3], b_h2bs[j % 3]
            scs, b_scs = scss[j % 2], b_scss[j % 2]
            P.dma("sp", lambda e, j=j: e.dma_start(out=xt[:], in_=dr["xo"][j * 128:(j + 1) * 128, :]), writes=[b_xt])
            P.dma("sp", lambda e, j=j: e.dma_start(out=ot[:], in_=oown_d[j * 128:(j + 1) * 128, :]), writes=[b_ot])
            P.dma("sp", lambda e, j=j: e.dma_start(out=ybt[:], in_=yb_d[j * 128:(j + 1) * 128, :]), writes=[b_ybt])
            normmod(xt[:], b_xt, hb[:], b_hb, sq[:], b_sq, stt, b_stt, 1, 0)
            transpose8(hb, b_hb, hT, b_hT)
            for half in range(2):
                w_, bw_ = wload("z", half)
                for k in range(8):
                    mm(psA[half][:], hT[:, k, :], w_[:, k, :], k == 0, k == 7, [b_hT, bw_], [b_psA[half]])
                P.op("act", lambda e, half=half: e.activation(out=zs[:, half * 512:(half + 1) * 512], in_=psA[half][:], func=AF.Silu),
                     reads=[b_psA[half]], writes=[b_zs])
            P.op("act", lambda e: e.activation(out=sq[:], in_=ot[:], func=AF.Square), reads=[b_ot], writes=[b_sq])
            P.op("dve", lambda e: e.reduce_sum(out=stt[:, 8:16], in_=sq[:, :].rearrange("p (a b) -> p a b", a=8), axis=AX.X),
                 reads=[b_sq, b_stt], writes=[b_stt])
            P.op("act", lambda e: e.activation(out=stt[:, 8:16], in_=stt[:, 8:16], func=AF.Sqrt, scale=1.0 / 128, bias=EPS),
                 reads=[b_stt], writes=[b_stt])
            P.op("dve", lambda e: e.reciprocal(out=stt[:, 16:24], in_=stt[:, 8:16]), reads=[b_stt], writes=[b_stt])
            o3 = ot[:, :].rearrange("p (a b) -> p a b", a=8)
            P.op("dve", lambda e: e.tensor_tensor(out=o3, in0=o3, in1=stt[:, 16:24].unsqueeze(2).to_broadcast([128, 8, 128]), op=ALU.mult),
                 reads=[b_ot, b_stt], writes=[b_ot])
            P.op("dve", lambda e: e.tensor_tensor(out=o3, in0=o3, in1=rows[:, 0:128].unsqueeze(1).to_broadcast([128, 8, 128]), op=ALU.mult),
                 reads=[b_ot, b_rows], writes=[b_ot])
            P.op("dve", lambda e: e.tensor_tensor(out=og[:], in0=ot[:], in1=zs[:], op=ALU.mult), reads=[b_ot, b_zs], writes=[b_og])
            transpose8(og, b_og, ogT, b_ogT)
            for half in range(2):
                hs_ = slice(half * 512, (half + 1) * 512)
                w_, bw_ = wload("ao", half)
                for k in range(8):
                    mm(psA[0][:], ogT[:, k, :], w_[:, k, :], k == 0, k == 7, [b_ogT, bw_], [b_psA[0]])
                w_, bw_ = wload("ga", half)
                for k in range(8):
                    mm(psA[1][:], hT[:, k, :], w_[:, k, :], k == 0, k == 7, [b_hT, bw_], [b_psA[1]])
                P.op("act", lambda e: e.activation(out=sg[:], in_=psA[1][:], func=AF.Sigmoid), reads=[b_psA[1]], writes=[b_sg])
                P.op("dve", lambda e, hs_=hs_: e.tensor_tensor(out=mt[:, hs_], in0=psA[0][:], in1=sg[:], op=ALU.mult),
                     reads=[b_psA[0], b_sg], writes=[b_mt])
                w_, bw_ = wload("gb", half)
                for k in range(8):
                    mm(psA[2][:], hT[:, k, :], w_[:, k, :], k == 0, k == 7, [b_hT, bw_], [b_psA[2]])
                P.op("act", lambda e: e.activation(out=sg[:], in_=psA[2][:], func=AF.Sigmoid), reads=[b_psA[2]], writes=[b_sg])
                P.op("dve", lambda e, hs_=hs_: e.tensor_tensor(out=ybt[:, hs_], in0=ybt[:, hs_], in1=sg[:], op=ALU.mult),
                     reads=[b_ybt, b_sg], writes=[b_ybt])
                P.op("dve", lambda e, hs_=hs_: e.tensor_tensor(out=mb[:, hs_], in0=mt[:, hs_], in1=ybt[:, hs_], op=ALU.add),
                     reads=[b_mt, b_ybt], writes=[b_mb])
            transpose8(mb, b_mb, ogT, b_ogT)
            for half in range(2):
                hs_ = slice(half * 512, (half + 1) * 512)
                w_, bw_ = wload("wo", half)
                for k in range(8):
                    mm(psA[half][:], ogT[:, k, :], w_[:, k, :], k == 0, k == 7, [b_ogT, bw_], [b_psA[half]])
                P.op("dve", lambda e, half=half, hs_=hs_: e.tensor_tensor(out=mt[:, hs_], in0=psA[half][:], in1=modrow[:, 2, hs_], op=ALU.mult),
                     reads=[b_psA[half], b_mod], writes=[b_mt])
                P.op("dve", lambda e, hs_=hs_: e.tensor_tensor(out=x1[:, hs_], in0=mt[:, hs_], in1=xt[:, hs_], op=ALU.add),
                     reads=[b_mt, b_xt], writes=[b_x1])
            normmod(x1[:], b_x1, h2f[:], b_h2f, sq[:], b_sq, stt, b_stt, 4, 3)
            P.op("act", lambda e: e.copy(out=hb[:], in_=h2f[:]), reads=[b_h2f], writes=[b_hb])
            P.op("act", lambda e: e.copy(out=h2b[:], in_=h2f[:]), reads=[b_h2f], writes=[b_h2b])
            transpose8(hb, b_hb, hT, b_hT)
            for q4 in range(4):
                w_, bw_ = wload("pq", q4)
                for i in range(4):
                    for k in range(8):
                        mm(psA[i][:, 0:128], w_[:, k, i * 128:(i + 1) * 128], hT[:, k, :], k == 0, k == 7, [b_hT, bw_], [b_psA[i]])
                    P.op("act", lambda e, i=i, q4=q4: e.copy(out=qpT[:, 4 * q4 + i, :], in_=psA[i][:, 0:128]), reads=[b_psA[i]], writes=[b_qpT])
            for q4 in range(4):
                for i in range(4):
                    hp = 4 * q4 + i
                    mm(psA[q4][:, i * 128:(i + 1) * 128], qpT[:, hp, :], skT[:, hp, :], True, True, [b_qpT, b_skT], [b_psA[q4]])
                P.op("act", lambda e, q4=q4: e.copy(out=scs[:, 4 * q4:4 * q4 + 4, :].rearrange("p a b -> p (a b)"), in_=psA[q4][:]),
                     reads=[b_psA[q4]], writes=[b_scs])

        def select(j):
            par = j % 2
            eid, b_eid, gts, b_gts = eids[par], b_eids[par], gtss[par], b_gtss[par]
            scs, b_scs = scss[par], b_scss[par]
            for hp in range(16):
                top16(scs[:, hp, :], b_scs, sv[:, hp, :], si[:, hp, :], scw[:, 0:128], [b_sv, b_si])
            P.op("dve", lambda e: e.tensor_copy(out=sif[:], in_=si[:]), reads=[b_si], writes=[b_sif])
            for h in range(8):
                P.op("dve", lambda e, h=h: e.tensor_tensor(
                    out=cand[:, h, :].rearrange("p (a b) -> p a b", a=16),
                    in0=sv[:, 2 * h, :].unsqueeze(2).to_broadcast([128, 16, 16]),
                    in1=sv[:, 2 * h + 1, :].unsqueeze(1).to_broadcast([128, 16, 16]), op=ALU.add),
                    reads=[b_sv, b_cand], writes=[b_cand])
            for h in range(8):
                top16(cand[:, h, :], b_cand, cv[:, h, :], cp[:, h, :], scw[:, 0:256], [b_cv, b_cp])
            P.op("dve", lambda e: e.tensor_scalar(out=stts[:, 8:16], in0=cv[:, :, 0], scalar1=-1.0, scalar2=None, op0=ALU.mult),
                 reads=[b_cv, b_stts], writes=[b_stts])
            P.op("pool", lambda e: e.memset(stts[:, 16:24], 0.0), reads=[b_stts], writes=[b_stts])
            for h in range(8):
                P.op("act", lambda e, h=h: e.activation(out=gts[:, h, :], in_=cv[:, h, :], func=AF.Exp, bias=stts[:, 8 + h:9 + h],
                                                        accum_out=stts[:, 16 + h:17 + h]), reads=[b_cv, b_stts, b_gts], writes=[b_gts, b_stts])
            P.op("dve", lambda e: e.reciprocal(out=stts[:, 16:24], in_=stts[:, 16:24]), reads=[b_stts], writes=[b_stts])
            P.op("dve", lambda e: e.tensor_tensor(out=gts[:], in0=gts[:], in1=stts[:, 16:24].unsqueeze(2).to_broadcast([128, 8, 16]), op=ALU.mult),
                 reads=[b_gts, b_stts], writes=[b_gts])
            P.op("dve", lambda e: e.tensor_copy(out=caf[:, 0, :, :], in_=cp[:]), reads=[b_cp, b_caf], writes=[b_caf])
            tmp3 = cand[:, :, :].rearrange("p h (r k) -> p (h r) k", k=16)
            P.op("dve", lambda e: e.tensor_tensor(out=tmp3, in0=caf[:, 0, :, :].rearrange("p a b -> p (a b)").unsqueeze(2).to_broadcast([128, 128, 16]),
                                                  in1=thr16[:].unsqueeze(1).to_broadcast([128, 128, 16]), op=ALU.is_ge),
                 reads=[b_caf, b_thr16, b_cand], writes=[b_cand])
            P.op("dve", lambda e: e.reduce_sum(out=caf[:, 1, :, :].rearrange("p a b -> p (a b)"), in_=tmp3, axis=AX.X),
                 reads=[b_cand, b_caf], writes=[b_caf])
            P.op("dve", lambda e: e.scalar_tensor_tensor(out=caf[:, 0, :, :], in0=caf[:, 1, :, :], scalar=-16.0, in1=caf[:, 0, :, :],
                                                          op0=ALU.mult, op1=ALU.add), reads=[b_caf], writes=[b_caf])
            for t_ in range(2):
                for h in range(8):
                    P.op("dve", lambda e, t_=t_, h=h: e.tensor_tensor(
                        out=oh[:], in0=caf[:, 1 - t_, h, :].unsqueeze(2).to_broadcast([128, 16, 16]),
                        in1=iota[:].unsqueeze(1).to_broadcast([128, 16, 16]), op=ALU.is_equal), reads=[b_caf, b_iota, b_oh], writes=[b_oh])
                    P.op("dve", lambda e, t_=t_, h=h: e.tensor_tensor(
                        out=oh[:], in0=oh[:], in1=sif[:, 2 * h + t_, :].unsqueeze(1).to_broadcast([128, 16, 16]), op=ALU.mult),
                        reads=[b_oh, b_sif], writes=[b_oh])
                    P.op("dve", lambda e, t_=t_, h=h: e.reduce_sum(out=ij[:, t_, h, :], in_=oh[:], axis=AX.X), reads=[b_oh, b_ij], writes=[b_ij])
            P.op("dve", lambda e: e.scalar_tensor_tensor(out=eidf[:], in0=ij[:, 0, :, :].rearrange("p a b -> p (a b)"), scalar=128.0,
                                                          in1=ij[:, 1, :, :].rearrange("p a b -> p (a b)"), op0=ALU.mult, op1=ALU.add),
                 reads=[b_ij], writes=[b_eidf])
            P.op("dve", lambda e: e.tensor_copy(out=eid[:], in_=eidf[:]), reads=[b_eidf], writes=[b_eid])

        def slot(j, s):
            par = j % 2
            eid, b_eid, gts, b_gts, h2b, b_h2b = eids[par], b_eids[par], gtss[par], b_gtss[par], h2bs[j % 3], b_h2bs[j % 3]
            gflat = gts[:].rearrange("p a b -> p (a b)")
            if s == 0:
                P.op("pool", lambda e: e.memset(act[:], 0.0), writes=[b_act])
            if True:
                u_ = s % NUG
                d_ = s % 4
                P.dma("pool", lambda e, u_=u_, s=s: e.indirect_dma_start(
                    out=ug[u_][:], out_offset=None, in_=uv_d, in_offset=bass.IndirectOffsetOnAxis(ap=eid[:, s:s + 1], axis=0)),
                    reads=[b_eid], writes=[b_ug[u_]])
                P.op("dve", lambda e, u_=u_, s=s: e.scalar_tensor_tensor(out=jb[:], in0=ug[u_][:, 0:D], scalar=1.0, in1=h2b[:], op0=ALU.mult, op1=ALU.mult,
                                                                      accum_out=act[:, s:s + 1]), reads=[b_ug[u_], b_h2b, b_act, b_jb], writes=[b_jb, b_act])
                P.op("act", lambda e, s=s: e.activation(out=coef[:, s:s + 1], in_=act[:, s:s + 1], func=AF.Gelu), reads=[b_act, b_coef], writes=[b_coef])
                P.op("act", lambda e, s=s: e.activation(out=coef[:, s:s + 1], in_=coef[:, s:s + 1], func=AF.Copy, scale=gflat[:, s:s + 1]),
                     reads=[b_coef, b_gts], writes=[b_coef])
                P.op("act", lambda e, s=s, d_=d_: e.activation(out=dg[d_][:], in_=ident[:], func=AF.Copy, scale=coef[:, s:s + 1]),
                     reads=[b_coef, b_ident], writes=[b_dg[d_]])
                for half in range(2):
                    mm(psB[half][:], dg[d_][:], ug[u_][:, D + half * 512:D + (half + 1) * 512], s == 0, s == 127,
                       [b_dg[d_], b_ug[u_]], [b_psB[half]])

        def tail(j):
            x1, b_x1 = x1s[j % 3], b_x1s[j % 3]
            sq, b_sq, stt, b_stt, mt, b_mt = sq_t, b_sq_t, stt_t, b_stt_t, mt_t, b_mt_t
            for half in range(2):
                P.op("act", lambda e, half=half: e.copy(out=acc[:, half * 512:(half + 1) * 512], in_=psB[half][:]), reads=[b_psB[half]], writes=[b_acc])
            P.op("dve", lambda e: e.tensor_tensor(out=acc[:], in0=acc[:], in1=modrow[:, 5, :], op=ALU.mult), reads=[b_acc, b_mod], writes=[b_acc])
            P.op("dve", lambda e: e.tensor_tensor(out=acc[:], in0=acc[:], in1=x1[:], op=ALU.add), reads=[b_acc, b_x1], writes=[b_acc])
            P.op("pool", lambda e: e.memset(stt[:, 0:1], 0.0), reads=[b_stt], writes=[b_stt])
            P.op("act", lambda e: e.activation(out=sq[:], in_=acc[:], func=AF.Square, accum_out=stt[:, 0:1]), reads=[b_acc, b_stt], writes=[b_sq, b_stt])
            P.op("act", lambda e: e.activation(out=stt[:, 1:2], in_=stt[:, 0:1], func=AF.Sqrt, scale=1.0 / D, bias=EPS), reads=[b_stt], writes=[b_stt])
            P.op("dve", lambda e: e.reciprocal(out=stt[:, 2:3], in_=stt[:, 1:2]), reads=[b_stt], writes=[b_stt])
            P.op("dve", lambda e: e.scalar_tensor_tensor(out=mt[:], in0=acc[:], scalar=stt[:, 2:3], in1=rows[:, 128:128 + D], op0=ALU.mult, op1=ALU.mult),
                 reads=[b_acc, b_stt, b_rows], writes=[b_mt])
            P.dma("sp", lambda e, j=j: e.dma_start(out=out_d[j * 128:(j + 1) * 128, :], in_=mt[:]), reads=[b_mt], track=b_mt, is_out=True)

        mixer(0); select(0); mixer(1)
        for j in range(NOWN):
            la = P.record(lambda: select(j + 1)) if j + 1 < NOWN else []
            lb = P.record(lambda: mixer(j + 2)) if j + 2 < NOWN else []
            pa = (len(la) + 127) // 128
            pb = (len(lb) + 127) // 128
            for s_ in range(128):
                P.replay(P.record(lambda: slot(j, s_)))
                P.replay(la[s_ * pa:(s_ + 1) * pa])
                P.replay(lb[s_ * pb:(s_ + 1) * pb])
            P.replay(P.record(lambda: tail(j)))
        P.flush()
    P.stack = gst
```
